# Optimizing a Trainium2 kernel written in Bass

```python
import functools
import math
import jax
import jax.numpy as jnp
from jax import lax
import numpy as np

D_MODEL = 1024
BATCH = 4
SEQ = 4096
DEPTH = 1
DEC_BATCH = 16
DEC_SEQ = 64
PAST_LEN = 4096

CHUNK = 64
N_FOX_HEADS = 8
FOX_HEAD_DIM = 64
FOX_WIDTH = N_FOX_HEADS * FOX_HEAD_DIM
Q_BLOCK = 128
N_MEM = 256
N_MEM_HEADS = 4
MEM_HEAD_DIM = 128
MEM_WIDTH = N_MEM_HEADS * MEM_HEAD_DIM
SSM_GROUP = 16
SSM_WIDTH = 512
N_SSM_GROUPS = SSM_WIDTH // SSM_GROUP
SSM_STATE = 64
N_BRANCHES = 3
IN_SPLITS = (FOX_WIDTH, FOX_WIDTH, FOX_WIDTH, N_FOX_HEADS, MEM_WIDTH, SSM_WIDTH, D_MODEL, D_MODEL, D_MODEL)
IN_WIDTH = 3 * FOX_WIDTH + N_FOX_HEADS + MEM_WIDTH + SSM_WIDTH + N_BRANCHES * D_MODEL
N_EXPERT_GROUPS = 4
EXPERTS_PER_GROUP = 8
N_EXPERTS = N_EXPERT_GROUPS * EXPERTS_PER_GROUP
TOP_K_IN_GROUP = 2
D_EXPERT = 256
RMS_EPS = 1e-6
NEG_INF = -1e30

kernel_name = 'hybrid_fox_s5_hmoe_stream_step'


def _rmsnorm(x, g):
    xf = x.astype(jnp.float32)
    y = xf * lax.rsqrt(jnp.mean(xf * xf, axis=-1, keepdims=True) + RMS_EPS)
    return (y * g.astype(jnp.float32)).astype(x.dtype)


def _split_last(z, sizes):
    offsets = []
    acc = 0
    for s in sizes[:-1]:
        acc += s
        offsets.append(acc)
    return jnp.split(z, offsets, axis=-1)


def _mixer_inputs(x, norm_mix, w_in, b_forget, qn_fox, kn_fox, qn_mem):
    bsz, s, _ = x.shape
    z = _rmsnorm(x, norm_mix) @ w_in
    q_f, k_f, v_f, f_lg, q_m, u, g_f, g_s, g_m = _split_last(z, IN_SPLITS)
    q_f = _rmsnorm(q_f.reshape(bsz, s, N_FOX_HEADS, FOX_HEAD_DIM), qn_fox)
    k_f = _rmsnorm(k_f.reshape(bsz, s, N_FOX_HEADS, FOX_HEAD_DIM), kn_fox)
    v_f = v_f.reshape(bsz, s, N_FOX_HEADS, FOX_HEAD_DIM)
    logf = jax.nn.log_sigmoid((f_lg + b_forget).astype(jnp.float32))
    q_m = _rmsnorm(q_m.reshape(bsz, s, N_MEM_HEADS, MEM_HEAD_DIM), qn_mem)
    return (q_f, k_f, v_f, logf, q_m, u,
            jax.nn.sigmoid(g_f), jax.nn.sigmoid(g_s), jax.nn.sigmoid(g_m))


def _fox_block(q, c_q, q_pos, k, v, c_k, k_pos):
    s = jnp.einsum('bqhd,bkhd->bhqk', q.astype(jnp.float32), k.astype(jnp.float32)) * (FOX_HEAD_DIM ** -0.5)
    bias = jnp.swapaxes(c_q, 1, 2)[:, :, :, None] - jnp.swapaxes(c_k, 1, 2)[:, :, None, :]
    mask = k_pos[None, :] <= q_pos[:, None]
    s = jnp.where(mask, s + bias, NEG_INF)
    p = jax.nn.softmax(s, axis=-1)
    return jnp.einsum('bhqk,bkhd->bqhd', p, v.astype(jnp.float32)).astype(v.dtype)


def _fox_prompt(q, k, v, logf):
    bsz, s, h, dh = q.shape
    nblk = s // Q_BLOCK
    c = jnp.cumsum(logf, axis=1)
    pos = jnp.arange(s)
    qb = jnp.swapaxes(q.reshape(bsz, nblk, Q_BLOCK, h, dh), 0, 1)
    cb = jnp.swapaxes(c.reshape(bsz, nblk, Q_BLOCK, h), 0, 1)
    pb = pos.reshape(nblk, Q_BLOCK)
    o = lax.map(lambda blk: _fox_block(blk[0], blk[1], blk[2], k, v, c, pos), (qb, cb, pb))
    return jnp.swapaxes(o, 0, 1).reshape(bsz, s, h * dh)


def _fox_sample(cache_k, cache_v, cache_logf, q, k_new, v_new, logf_new):
    bsz, n, h, dh = q.shape
    past = cache_k.shape[1]
    k_all = jnp.concatenate([cache_k, k_new.astype(cache_k.dtype)], axis=1)
    v_all = jnp.concatenate([cache_v, v_new.astype(cache_v.dtype)], axis=1)
    c_all = jnp.cumsum(jnp.concatenate([cache_logf.astype(jnp.float32), logf_new], axis=1), axis=1)
    k_pos = jnp.arange(past + n)
    q_pos = past + jnp.arange(n)
    o = _fox_block(q, c_all[:, past:], q_pos, k_all, v_all, c_all, k_pos)
    return o.reshape(bsz, n, h * dh)


def _complex_affine_combine(earlier, later):
    a1r, a1i, b1r, b1i = earlier
    a2r, a2i, b2r, b2i = later
    return (a2r * a1r - a2i * a1i,
            a2r * a1i + a2i * a1r,
            a2r * b1r - a2i * b1i + b2r,
            a2r * b1i + a2i * b1r + b2i)


def _ssm_branch(u, h0_re, h0_im, a_re, a_im, log_dt, b_re, b_im, c_re, c_im, d, w_glu):
    bsz, s, _ = u.shape
    f32 = jnp.float32
    a_re, a_im, b_re, b_im = a_re.astype(f32), a_im.astype(f32), b_re.astype(f32), b_im.astype(f32)
    uf = u.astype(f32)
    ug = uf.reshape(bsz, s, N_SSM_GROUPS, SSM_GROUP)
    dt = jnp.exp(log_dt.astype(f32))[:, None]
    mag = jnp.exp(dt * a_re)
    ab_re = mag * jnp.cos(dt * a_im)
    ab_im = mag * jnp.sin(dt * a_im)
    den = a_re * a_re + a_im * a_im
    nr, ni = ab_re - 1.0, ab_im
    coef_re = (nr * a_re + ni * a_im) / den
    coef_im = (ni * a_re - nr * a_im) / den
    bb_re = coef_re[..., None] * b_re - coef_im[..., None] * b_im
    bb_im = coef_re[..., None] * b_im + coef_im[..., None] * b_re
    bu_re = jnp.einsum('bsgh,gph->bsgp', ug, bb_re)
    bu_im = jnp.einsum('bsgh,gph->bsgp', ug, bb_im)
    bu_re = bu_re.at[:, 0].add(ab_re * h0_re - ab_im * h0_im)
    bu_im = bu_im.at[:, 0].add(ab_re * h0_im + ab_im * h0_re)
    a_r = jnp.broadcast_to(ab_re, bu_re.shape)
    a_i = jnp.broadcast_to(ab_im, bu_im.shape)
    _, _, h_re, h_im = lax.associative_scan(_complex_affine_combine, (a_r, a_i, bu_re, bu_im), axis=1)
    y = (jnp.einsum('bsgp,ghp->bsgh', h_re, c_re.astype(f32))
         - jnp.einsum('bsgp,ghp->bsgh', h_im, c_im.astype(f32)))
    y = y.reshape(bsz, s, SSM_WIDTH) + d.astype(f32) * uf
    z = jax.nn.gelu(y) @ w_glu.astype(f32)
    za, zg = jnp.split(z, 2, axis=-1)
    return (za * jax.nn.sigmoid(zg)).astype(u.dtype), h_re[:, -1], h_im[:, -1]


def _mem_kv(mem, norm_mem, w_mem_kv, kn_mem):
    bsz, n, _ = mem.shape
    kv = _rmsnorm(mem, norm_mem) @ w_mem_kv
    k, v = jnp.split(kv, 2, axis=-1)
    k = _rmsnorm(k.reshape(bsz, n, N_MEM_HEADS, MEM_HEAD_DIM), kn_mem)
    return k, v.reshape(bsz, n, N_MEM_HEADS, MEM_HEAD_DIM)


def _mem_attend(q, k, v):
    bsz, s = q.shape[0], q.shape[1]
    sc = jnp.einsum('bqhd,bkhd->bhqk', q.astype(jnp.float32), k.astype(jnp.float32)) * (MEM_HEAD_DIM ** -0.5)
    p = jax.nn.softmax(sc, axis=-1)
    o = jnp.einsum('bhqk,bkhd->bqhd', p, v.astype(jnp.float32))
    return o.reshape(bsz, s, MEM_WIDTH).astype(q.dtype)


def _hmoe(x, norm_ffn, w_router_group, w_router_expert, w_gate, w_up, w_down):
    h = _rmsnorm(x, norm_ffn)
    g_logits = (h @ w_router_group).astype(jnp.float32)
    g_prob = jax.nn.softmax(g_logits, axis=-1)
    grp = jnp.argmax(g_logits, axis=-1)
    g_w = jnp.max(g_prob, axis=-1, keepdims=True)
    e_logits = (h @ w_router_expert).astype(jnp.float32)
    e_logits = e_logits.reshape(x.shape[0], x.shape[1], N_EXPERT_GROUPS, EXPERTS_PER_GROUP)
    e_sel = jnp.einsum('bsge,bsg->bse', e_logits, jax.nn.one_hot(grp, N_EXPERT_GROUPS, dtype=jnp.float32))
    top_v, top_i = lax.top_k(e_sel, TOP_K_IN_GROUP)
    w_sel = jax.nn.softmax(top_v, axis=-1) * g_w
    expert_id = grp[..., None] * EXPERTS_PER_GROUP + top_i
    combine = jnp.sum(jax.nn.one_hot(expert_id, N_EXPERTS, dtype=jnp.float32) * w_sel[..., None], axis=-2)
    a = jnp.einsum('bsd,edf->bsef', h, w_gate)
    up = jnp.einsum('bsd,edf->bsef', h, w_up)
    act = jax.nn.silu(a) * up * combine[..., None].astype(h.dtype)
    return x + jnp.einsum('bsef,efd->bsd', act, w_down)


def _layer(x, attend_fox, h0_re, h0_im, mem_k, mem_v, p):
    q_f, k_f, v_f, logf, q_m, u, g_f, g_s, g_m = _mixer_inputs(
        x, p['norm_mix'], p['w_in'], p['b_forget'], p['qn_fox'], p['kn_fox'], p['qn_mem'])
    o_fox = attend_fox(q_f, k_f, v_f, logf)
    y_ssm, h_re, h_im = _ssm_branch(u, h0_re, h0_im, p['ssm_a_re'], p['ssm_a_im'], p['ssm_log_dt'],
                                    p['ssm_b_re'], p['ssm_b_im'], p['ssm_c_re'], p['ssm_c_im'],
                                    p['ssm_d'], p['w_glu'])
    o_mem = _mem_attend(q_m, mem_k, mem_v)
    merged = (g_f * (o_fox @ p['w_br_fox']) + g_s * (y_ssm @ p['w_br_ssm'])
              + g_m * (o_mem @ p['w_br_mem']))
    x = x + merged @ p['w_out']
    x = _hmoe(x, p['norm_ffn'], p['w_router_group'], p['w_router_expert'],
              p['moe_w_gate'], p['moe_w_up'], p['moe_w_down'])
    return x, k_f, v_f, logf, h_re, h_im


def setup_inputs(seed: int = 0) -> dict:
    key = jax.random.key(seed)
    it = iter(list(jax.random.split(key, 48)))
    f32 = jnp.float32

    def nrm(shape, scale=1.0):
        return scale * jax.random.normal(next(it), shape, f32)

    L = DEPTH
    G, P, H = N_SSM_GROUPS, SSM_STATE, SSM_GROUP
    return {
        'x_prompt': nrm((BATCH, SEQ, D_MODEL)),
        'x_sample': nrm((DEC_BATCH, DEC_SEQ, D_MODEL)),
        'mem_prompt': nrm((BATCH, N_MEM, D_MODEL)),
        'cache_fox_k': nrm((L, DEC_BATCH, PAST_LEN, N_FOX_HEADS, FOX_HEAD_DIM)),
        'cache_fox_v': nrm((L, DEC_BATCH, PAST_LEN, N_FOX_HEADS, FOX_HEAD_DIM)),
        'cache_fox_logf': jax.nn.log_sigmoid(4.0 + nrm((L, DEC_BATCH, PAST_LEN, N_FOX_HEADS))),
        'state_ssm_re': nrm((L, DEC_BATCH, G, P), 0.5),
        'state_ssm_im': nrm((L, DEC_BATCH, G, P), 0.5),
        'cache_mem_k': nrm((L, DEC_BATCH, N_MEM, N_MEM_HEADS, MEM_HEAD_DIM)),
        'cache_mem_v': nrm((L, DEC_BATCH, N_MEM, N_MEM_HEADS, MEM_HEAD_DIM)),
        'norm_mix': 1.0 + nrm((L, D_MODEL), 0.02),
        'w_in': nrm((L, D_MODEL, IN_WIDTH), D_MODEL ** -0.5),
        'b_forget': 4.0 + nrm((L, N_FOX_HEADS), 0.5),
        'qn_fox': 1.0 + nrm((L, FOX_HEAD_DIM), 0.02),
        'kn_fox': 1.0 + nrm((L, FOX_HEAD_DIM), 0.02),
        'qn_mem': 1.0 + nrm((L, MEM_HEAD_DIM), 0.02),
        'kn_mem': 1.0 + nrm((L, MEM_HEAD_DIM), 0.02),
        'norm_mem': 1.0 + nrm((L, D_MODEL), 0.02),
        'w_mem_kv': nrm((L, D_MODEL, 2 * MEM_WIDTH), D_MODEL ** -0.5),
        'ssm_a_re': -0.5 + nrm((L, G, P), 0.01),
        'ssm_a_im': math.pi * jnp.arange(P, dtype=f32) + nrm((L, G, P), 0.01),
        'ssm_log_dt': jax.random.uniform(next(it), (L, G), f32, math.log(1e-3), math.log(1e-1)),
        'ssm_b_re': nrm((L, G, P, H), (2 * H) ** -0.5),
        'ssm_b_im': nrm((L, G, P, H), (2 * H) ** -0.5),
        'ssm_c_re': nrm((L, G, H, P), P ** -0.5),
        'ssm_c_im': nrm((L, G, H, P), P ** -0.5),
        'ssm_d': nrm((L, SSM_WIDTH), 0.5),
        'w_glu': nrm((L, SSM_WIDTH, 2 * SSM_WIDTH), SSM_WIDTH ** -0.5),
        'w_br_fox': nrm((L, FOX_WIDTH, D_MODEL), FOX_WIDTH ** -0.5),
        'w_br_ssm': nrm((L, SSM_WIDTH, D_MODEL), SSM_WIDTH ** -0.5),
        'w_br_mem': nrm((L, MEM_WIDTH, D_MODEL), MEM_WIDTH ** -0.5),
        'w_out': nrm((L, D_MODEL, D_MODEL), D_MODEL ** -0.5),
        'norm_ffn': 1.0 + nrm((L, D_MODEL), 0.02),
        'w_router_group': nrm((L, D_MODEL, N_EXPERT_GROUPS), D_MODEL ** -0.5),
        'w_router_expert': nrm((L, D_MODEL, N_EXPERTS), D_MODEL ** -0.5),
        'moe_w_gate': nrm((L, N_EXPERTS, D_MODEL, D_EXPERT), D_MODEL ** -0.5),
        'moe_w_up': nrm((L, N_EXPERTS, D_MODEL, D_EXPERT), D_MODEL ** -0.5),
        'moe_w_down': nrm((L, N_EXPERTS, D_EXPERT, D_MODEL), D_EXPERT ** -0.5),
    }


def reference(x_prompt, x_sample, mem_prompt, cache_fox_k, cache_fox_v, cache_fox_logf,
              state_ssm_re, state_ssm_im, cache_mem_k, cache_mem_v,
              norm_mix, w_in, b_forget, qn_fox, kn_fox, qn_mem, kn_mem, norm_mem, w_mem_kv,
              ssm_a_re, ssm_a_im, ssm_log_dt, ssm_b_re, ssm_b_im, ssm_c_re, ssm_c_im, ssm_d, w_glu,
              w_br_fox, w_br_ssm, w_br_mem, w_out, norm_ffn, w_router_group, w_router_expert,
              moe_w_gate, moe_w_up, moe_w_down):
    p_k, p_v, p_lf, p_re, p_im, p_mk, p_mv = [], [], [], [], [], [], []
    s_k, s_v, s_lf, s_re, s_im = [], [], [], [], []
    xp, xs = x_prompt, x_sample
    for l in range(DEPTH):
        p = dict(norm_mix=norm_mix[l], w_in=w_in[l], b_forget=b_forget[l], qn_fox=qn_fox[l],
                 kn_fox=kn_fox[l], qn_mem=qn_mem[l], ssm_a_re=ssm_a_re[l], ssm_a_im=ssm_a_im[l],
                 ssm_log_dt=ssm_log_dt[l], ssm_b_re=ssm_b_re[l], ssm_b_im=ssm_b_im[l],
                 ssm_c_re=ssm_c_re[l], ssm_c_im=ssm_c_im[l], ssm_d=ssm_d[l], w_glu=w_glu[l],
                 w_br_fox=w_br_fox[l], w_br_ssm=w_br_ssm[l], w_br_mem=w_br_mem[l], w_out=w_out[l],
                 norm_ffn=norm_ffn[l], w_router_group=w_router_group[l],
                 w_router_expert=w_router_expert[l], moe_w_gate=moe_w_gate[l],
                 moe_w_up=moe_w_up[l], moe_w_down=moe_w_down[l])
        mk, mv = _mem_kv(mem_prompt, norm_mem[l], w_mem_kv[l], kn_mem[l])
        h0 = jnp.zeros((xp.shape[0], N_SSM_GROUPS, SSM_STATE), jnp.float32)
        xp, k_f, v_f, lf, h_re, h_im = _layer(xp, _fox_prompt, h0, h0, mk, mv, p)
        p_k.append(k_f); p_v.append(v_f); p_lf.append(lf)
        p_re.append(h_re); p_im.append(h_im); p_mk.append(mk); p_mv.append(mv)
        fox_cached = functools.partial(_fox_sample, cache_fox_k[l], cache_fox_v[l], cache_fox_logf[l])
        xs, k_f, v_f, lf, h_re, h_im = _layer(xs, fox_cached, state_ssm_re[l].astype(jnp.float32),
                                              state_ssm_im[l].astype(jnp.float32),
                                              cache_mem_k[l], cache_mem_v[l], p)
        s_k.append(k_f); s_v.append(v_f); s_lf.append(lf); s_re.append(h_re); s_im.append(h_im)
    prompt_fox_k = jnp.stack(p_k)
    prompt_fox_v = jnp.stack(p_v)
    prompt_fox_logf = jnp.stack(p_lf)
    prompt_ssm_re = jnp.stack(p_re)
    prompt_ssm_im = jnp.stack(p_im)
    prompt_mem_k = jnp.stack(p_mk)
    prompt_mem_v = jnp.stack(p_mv)
    sample_fox_k = jnp.stack(s_k)
    sample_fox_v = jnp.stack(s_v)
    sample_fox_logf = jnp.stack(s_lf)
    sample_ssm_re = jnp.stack(s_re)
    sample_ssm_im = jnp.stack(s_im)
    return (xp, xs, prompt_fox_k, prompt_fox_v, prompt_fox_logf, prompt_ssm_re, prompt_ssm_im,
            prompt_mem_k, prompt_mem_v, sample_fox_k, sample_fox_v, sample_fox_logf,
            sample_ssm_re, sample_ssm_im)
```

```python
import os
import numpy as np
from contextlib import ExitStack, contextmanager
import concourse.bass as bass
import concourse.mybir as mybir
from concourse.bass_utils import run_bass_kernel_spmd

F32 = mybir.dt.float32
BF16 = mybir.dt.bfloat16
I32 = mybir.dt.int32
AF = mybir.ActivationFunctionType
ALU = mybir.AluOpType
AX = mybir.AxisListType

NCORES = 8
D = 1024
SEQ = 4096
HALF = 2048
NSMP = 128
NOWN = HALF + NSMP
NT = NOWN // 128
EPS = 1e-6
NEG = -30000.0
ARENA_BYTES = 212480
STOP = ""
_DSZ = {F32: 4, BF16: 2, I32: 4}


class Buf:
    def __init__(self, t, psum=False):
        self.t = t
        self.w = None
        self.r = []
        self.psum = psum

    def __getitem__(self, idx):
        return self.t[idx]


class KB:
    NDMA = 24

    def __init__(self, nc, es):
        self.nc = nc
        self.es = es
        self.eng = {"pe": nc.tensor, "act": nc.scalar, "dve": nc.vector, "pool": nc.gpsimd, "sp": nc.sync}
        self.sem = {k: es.enter_context(nc.semaphore("s_" + k)) for k in self.eng}
        self.cnt = {k: 0 for k in self.eng}
        self.dsem = [es.enter_context(nc.semaphore("d%d" % i)) for i in range(self.NDMA)]
        self.dcnt = [0] * self.NDMA
        self.dnext = 0
        self.dnext_q = {}
        self.waited = {}
        self.uid = 0
        self.arena = es.enter_context(nc.sbuf_tensor("arena", [128, ARENA_BYTES // 2], BF16))
        self.regions = []

    def at(self, off, shape, dt=F32):
        n = 1
        for s in shape[1:]:
            n *= s
        nb = n * _DSZ[dt]
        assert off % 4 == 0 and off + nb <= ARENA_BYTES, (off, nb)
        v = self.arena[0:shape[0], off // 2:(off + nb) // 2]
        if dt != BF16:
            v = v.bitcast(dt)
        if len(shape) == 3:
            v = v.rearrange("p (a b) -> p a b", a=shape[1])
        elif len(shape) == 4:
            v = v.rearrange("p (a b c) -> p a b c", a=shape[1], b=shape[2])
        return Buf(v)

    def sb(self, name, shape, dt=F32):
        n = 1
        for s in shape[1:]:
            n *= s
        nb = (n * _DSZ[dt] + 63) // 64 * 64
        for reg in self.regions:
            if reg[0] + nb <= reg[1]:
                off = reg[0]
                reg[0] += nb
                return self.at(off, shape, dt)
        raise AssertionError("arena regions exhausted for %s %s (%d B): %s" % (name, shape, nb, self.regions))

    def ps(self, name, shape, dt=F32):
        self.uid += 1
        return Buf(self.es.enter_context(self.nc.psum_tensor("%s_%d" % (name, self.uid), list(shape), dt)), psum=True)

    @contextmanager
    def scope(self, regions):
        old, oldr = self.es, self.regions
        with ExitStack() as es2:
            self.es = es2
            self.regions = [[a, b] for a, b in regions]
            yield
            self.barrier()
        self.es, self.regions = old, oldr

    def _semof(self, key):
        return self.sem[key] if isinstance(key, str) else self.dsem[key]

    def _wait(self, eng, ev):
        key, val = ev
        if self.waited.get((eng, key), 0) >= val:
            return
        self.eng[eng].wait_ge(self._semof(key), val)
        self.waited[(eng, key)] = val

    def barrier(self):
        for e in self.eng:
            for k in self.eng:
                if k != e and self.cnt[k]:
                    self._wait(e, (k, self.cnt[k]))
            for s in range(self.NDMA):
                if self.dcnt[s]:
                    self._wait(e, (s, self.dcnt[s]))

    def _deps(self, eng, reads, writes):
        deps = []
        for b in reads:
            if b.w is not None:
                deps.append(b.w)
            if b.psum:
                deps.extend(ev for ev in b.r if ev[0] != eng)
        for b in writes:
            if b.w is not None:
                deps.append(b.w)
            deps.extend(b.r)
        for ev in deps:
            if ev[0] == eng and eng == "pe":
                continue
            self._wait(eng, ev)

    def _mark(self, ev, reads, writes):
        for b in reads:
            b.r.append(ev)
            if len(b.r) > 48:
                last = {}
                for k, v in b.r:
                    last[k] = max(last.get(k, 0), v)
                b.r = list(last.items())
        for b in writes:
            b.w = ev
            b.r = []

    def op(self, eng, fn, reads=(), writes=()):
        self._deps(eng, reads, writes)
        inst = fn(self.eng[eng])
        self.cnt[eng] += 1
        inst.then_inc(self.sem[eng], 1)
        self._mark((eng, self.cnt[eng]), reads, writes)

    def mm(self, out, lhsT, rhs, start, stop, reads, writes, **kw):
        self.op("pe", lambda e: e.matmul(out, lhsT, rhs, start=start, stop=stop, **kw), reads, writes)

    def tr(self, out, in_, ident, reads, writes):
        self.op("pe", lambda e: e.transpose(out=out, in_=in_, identity=ident), reads, writes)

    def dma(self, q, out, in_, reads=(), writes=(), **kw):
        lo, n = (0, 16) if q == "sp" else (16, self.NDMA - 16)
        cur = self.dnext_q.get(q, 0)
        slot = lo + cur
        self.dnext_q[q] = (cur + 1) % n
        if self.dcnt[slot] > 0:
            self._wait(q, (slot, self.dcnt[slot]))
        self._deps(q, reads, writes)
        inst = self.eng[q].dma_start(out=out, in_=in_, **kw)
        self.dcnt[slot] += 16
        inst.then_inc(self.dsem[slot], 16)
        self._mark((slot, self.dcnt[slot]), reads, writes)

    def finish(self):
        for slot in range(self.NDMA):
            if self.dcnt[slot]:
                self._wait("sp", (slot, self.dcnt[slot]))


class _View:
    def __init__(self, parent, ap):
        self.__dict__["p"] = parent
        self.__dict__["ap"] = ap

    def __getitem__(self, idx):
        return self.ap if idx == slice(None) else self.ap[idx]

    def __getattr__(self, k):
        return getattr(self.p, k)

    def __setattr__(self, k, v):
        setattr(self.p, k, v)


_SH = 2048
O_CONST = (0, 14336 + _SH)
O_OFT = 14336 + _SH
O_KT = 49152 + _SH
O_VE = 81920 + _SH
O_CK = 115200 + _SH
O_QT = 116224 + _SH
O_QMT = 133632 + _SH
O_UTO = 151040 + _SH
O_HI = 168448 + _SH
O_OMT = 49152 + _SH
O_UTF = 66560 + _SH
O_YS2 = 83968 + _SH
O_FREE2 = 101376 + _SH


def build():
    nc = bass.Bass("TRN2", target_bir_lowering=False)
    din = lambda n, s, dt=F32: nc.dram_tensor(n, list(s), dt, kind="ExternalInput").ap()
    dout = lambda n, s, dt=F32: nc.dram_tensor(n, list(s), dt, kind="ExternalOutput").ap()

    x_all = din("x_all", [SEQ, D])
    x_own = din("x_own", [NOWN, D])
    gmix = din("gmix", [128, D])
    w_a = din("w_a", [D, 1544])
    w_b = din("w_b", [D, 1024])
    kn_rep = din("kn_rep", [128, 512])
    qn_rep = din("qn_rep", [128, 512])
    qnm_rep = din("qnm_rep", [128, 512])
    bf_rep = din("bf_rep", [128, 8])
    ident_in = din("ident", [128, 128])
    triu_in = din("triu", [128, 128])
    ma_in = din("mask_a", [128, 4, 512])
    mb_in = din("mask_b", [128, 4, 512])
    addm_in = din("addm", [128, 4, 36])
    rflag_in = din("rflag", [128, 1])
    c_k = din("c_k", [2, SEQ, 512])
    c_v = din("c_v", [2, SEQ, 512])
    c_lf = din("c_lf", [2, SEQ, 8])
    mem_in = din("mem_in", [256, D])
    gmem = din("gmem", [128, D])
    w_mkv = din("w_mkv", [D, 1024])
    knm_rep = din("knm_rep", [128, 512])
    c_mk = din("c_mk", [2, 256, 512])
    c_mv = din("c_mv", [2, 256, 512])
    sp_are = din("sp_are", [128, 32])
    sp_aim = din("sp_aim", [128, 32])
    sp_ldt = din("sp_ldt", [128, 32])
    sp_bre = din("sp_bre", [128, 32, 16])
    sp_bim = din("sp_bim", [128, 32, 16])
    sp_cre = din("sp_cre", [128, 32, 16])
    sp_cim = din("sp_cim", [128, 32, 16])
    sp_dcol = din("sp_dcol", [128, 32])
    sp_et = din("sp_et", [128, 41])
    sp_caus = din("sp_caus", [128, 128])
    sp_esel = din("sp_esel", [128, 64, 128])
    sp_eselT = din("sp_eselT", [128, 64, 128])
    sp_swap = din("sp_swap", [128, 128])
    sp_sg = din("sp_sg", [128, 1])
    sp_h0 = din("sp_h0", [128, 2, 32])
    sp_h0s = din("sp_h0s", [128, 2, 32])
    w_glu = din("w_glu", [512, 1024])
    w_g = din("w_g", [D, 3072])
    w_brf = din("w_brf", [512, D])
    w_brs = din("w_brs", [512, D])
    w_brm = din("w_brm", [512, D])
    w_out = din("w_out", [D, D])
    gffn = din("gffn", [128, D])
    w_r = din("w_r", [D, 36])
    moe_wg = din("moe_wg", [32, D, 256])
    moe_wu = din("moe_wu", [32, D, 256])
    moe_wd = din("moe_wd", [32, 256, D])
    sele = din("sele", [32, 32, 128])

    o_k = dout("o_k", [SEQ, 512])
    o_v = dout("o_v", [SEQ, 512])
    o_lf = dout("o_lf", [SEQ, 8])
    o_sk = dout("o_sk", [NSMP, 512])
    o_sv = dout("o_sv", [NSMP, 512])
    o_slf = dout("o_slf", [NSMP, 8])
    o_mk = dout("o_mk", [256, 512])
    o_mv = dout("o_mv", [256, 512])
    o_fin = dout("o_fin", [96, 128])
    o_y = dout("o_y", [NOWN, D])

    with ExitStack() as es:
        kb = KB(nc, es)
        kb.regions = [list(O_CONST)]
        G = kb.sb("G", [128, D])
        KN = kb.sb("KN", [128, 512])
        QN = kb.sb("QN", [128, 512])
        QNM = kb.sb("QNM", [128, 512])
        BFr = kb.sb("BFr", [128, 8])
        IDf = kb.sb("IDf", [128, 128])
        IDb = kb.sb("IDb", [128, 128], BF16)
        TRIU = kb.sb("TRIU", [128, 128])
        ONES = kb.sb("ONES", [128, 128])
        RFL = kb.sb("RFL", [128, 1])
        ADDM = kb.sb("ADDM", [128, 4, 36])
        RUN = kb.sb("RUN", [128, 8])
        RUN16 = kb.sb("RUN16", [128, 8])
        RUNO = kb.sb("RUNO", [128, 8])
        RUNOJ = kb.sb("RUNOJ", [128, 4, 8])
        CREF = kb.sb("CREF", [128, 4, 8])
        KTN = kb.sb("KTN", [128, 4, 128], BF16)
        VEN = kb.sb("VEN", [128, 8, 65], BF16)
        LFN = kb.sb("LFN", [128, 8])
        for dst, src in ((G, gmix), (KN, kn_rep), (QN, qn_rep), (QNM, qnm_rep), (BFr, bf_rep), (IDf, ident_in),
                         (TRIU, triu_in), (RFL, rflag_in), (ADDM, addm_in)):
            kb.dma("sp", dst[:], src, writes=[dst])
        kb.op("dve", lambda e: e.tensor_copy(out=IDb[:], in_=IDf[:]), [IDf], [IDb])
        kb.op("dve", lambda e: e.memset(ONES[:], 1.0), [], [ONES])
        kb.op("dve", lambda e: e.memset(RUN[:], 0.0), [], [RUN])
        kb.op("dve", lambda e: e.memset(RUNO[:], 0.0), [], [RUNO])
        kb.op("dve", lambda e: e.memset(VEN[:, :, 64:65], 1.0), [], [VEN])

        OFT = kb.at(O_OFT, [64, 8, NOWN], BF16)
        KT = kb.at(O_KT, [128, 4, SEQ], BF16)
        VE = kb.at(O_VE, [128, 32, 8, 65], BF16)
        CK = kb.at(O_CK, [128, 32, 8])
        QT = kb.at(O_QT, [128, 4, NOWN], BF16)
        QMT = kb.at(O_QMT, [128, 4, NOWN], BF16)
        UTO = kb.at(O_UTO, [128, 4, NOWN], BF16)
        kb.op("dve", lambda e: e.memset(VE[:, :, :, 64:65], 1.0), [], [VE])

        def norm_a(x, gain, ss, rs, hn, sq):
            kb.op("act", lambda e: e.activation(out=sq[:], in_=x[:], func=AF.Square, accum_out=ss[:]), [x], [sq, ss])
            kb.op("act", lambda e: e.activation(out=rs[:], in_=ss[:], func=AF.Ln, scale=1.0 / D, bias=EPS),
                  [ss], [rs])
            kb.op("act", lambda e: e.activation(out=rs[:], in_=rs[:], func=AF.Exp, scale=-0.5), [rs], [rs])
            kb.op("dve", lambda e: e.scalar_tensor_tensor(out=hn[:], in0=x[:], scalar=rs[:, 0:1], in1=gain[:],
                                                          op0=ALU.mult, op1=ALU.mult), [x, rs, gain], [hn])

        def norm_b(hn, pt, ht_dst):
            for k in range(8):
                kb.tr(pt[:, k, :], hn[:, k * 128:(k + 1) * 128], IDb[:], [hn, IDb], [pt])
            kb.op("act", lambda e: e.copy(out=ht_dst[0], in_=pt[:]), [pt], [ht_dst[1]])

        def norm_tile(x, gain, ss, rs, hn, sq, pt, ht_dst):
            norm_a(x, gain, ss, rs, hn, sq)
            norm_b(hn, pt, ht_dst)

        def head_norm(src, nh, dh, gain, scale, sq, ss, dst_f32, dst_bf):
            sap, sbuf = src
            v3 = lambda ap: ap.rearrange("p (h d) -> p h d", d=dh)
            kb.op("act", lambda e: e.activation(out=sq[:], in_=sap, func=AF.Square), [sbuf], [sq])
            kb.op("dve", lambda e: e.tensor_reduce(out=ss[:, 0:nh], in_=v3(sq[:]), axis=AX.X, op=ALU.add), [sq], [ss])
            kb.op("act", lambda e: e.activation(out=ss[:, 0:nh], in_=ss[:, 0:nh], func=AF.Ln, scale=1.0 / dh,
                                                bias=EPS), [ss], [ss])
            kb.op("act", lambda e: e.activation(out=ss[:, 0:nh], in_=ss[:, 0:nh], func=AF.Exp, scale=-0.5),
                  [ss], [ss])
            kb.op("dve", lambda e: e.tensor_tensor(out=v3(sq[:]), in0=v3(sap),
                                                   in1=ss[:, 0:nh, None].to_broadcast([128, nh, dh]), op=ALU.mult),
                  [sbuf, ss], [sq])
            if dst_f32 is not None:
                kb.op("dve", lambda e: e.tensor_tensor(out=dst_f32[:], in0=sq[:], in1=gain[:], op=ALU.mult),
                      [sq, gain], [dst_f32])
            kb.op("dve", lambda e: e.scalar_tensor_tensor(out=dst_bf[:], in0=sq[:], scalar=float(scale), in1=gain[:],
                                                          op0=ALU.mult, op1=ALU.mult), [sq, gain], [dst_bf])

        def logsig(lf, src, sbuf):
            kb.op("dve", lambda e: e.tensor_tensor(out=lf[:], in0=src, in1=BFr[:], op=ALU.add), [sbuf, BFr], [lf])
            kb.op("act", lambda e: e.activation(out=lf[:], in_=lf[:], func=AF.Exp, scale=-1.0), [lf], [lf])
            kb.op("act", lambda e: e.activation(out=lf[:], in_=lf[:], func=AF.Ln, bias=1.0), [lf], [lf])
            kb.op("dve", lambda e: e.tensor_scalar(out=lf[:], in0=lf[:], scalar1=-1.0, scalar2=None, op0=ALU.mult),
                  [lf], [lf])

        def kv_tile_a(kv, ko, kob, ksq, kss, ok_ap, ov_ap, ve_dst):
            kb.dma("sp", ov_ap, kv[:, 512:1024], reads=[kv])
            kb.op("pool", lambda e: e.tensor_copy(out=ve_dst[0],
                                                  in_=kv[:, 512:1024].rearrange("p (h d) -> p h d", d=64)),
                  [kv], [ve_dst[1]])
            head_norm((kv[:, 0:512], kv), 8, 64, KN, 1.0, ksq, kss, ko, kob)
            kb.dma("sp", ok_ap, ko[:], reads=[ko])

        def kv_tile_b(kv, kob, lf, olf_ap, kt_ptk, kt_dst):
            for hp in range(4):
                kb.tr(kt_ptk[:, hp, :], kob[:, hp * 128:(hp + 1) * 128], IDb[:], [kob, IDb], [kt_ptk])
            kb.op("act", lambda e: e.copy(out=kt_dst[0], in_=kt_ptk[:, 0:4, :]), [kt_ptk], [kt_dst[1]])
            logsig(lf, kv[:, 1024:1032], kv)
            kb.dma("sp", olf_ap, lf[:], reads=[lf])

        def kv_tile(kv, ko, kob, ksq, kss, lf, ok_ap, ov_ap, olf_ap, ve_dst, kt_ptk, kt_dst):
            kv_tile_a(kv, ko, kob, ksq, kss, ok_ap, ov_ap, ve_dst)
            kv_tile_b(kv, kob, lf, olf_ap, kt_ptk, kt_dst)

        LOHI = [[O_HI, ARENA_BYTES], [O_OFT, O_KT]]

        with kb.scope(LOHI):
            W = kb.sb("WA", [128, 8, 1032], BF16)
            wv = w_a.rearrange("(k p) n -> p k n", p=128)
            STGW = [kb.sb("STGW", [128, 1032]) for i in range(2)]
            for k in range(8):
                stg = STGW[k % 2]
                kb.dma("sp", stg[:], wv[:, k, 0:1032], writes=[stg])
                kb.op(("dve", "pool")[k % 2], lambda e: e.tensor_copy(out=W[:, k, :], in_=stg[:]), [stg], [W])
            NB = 2
            X = [kb.sb("X", [128, D]) for i in range(NB)]
            SQ = kb.sb("SQ", [128, D], BF16)
            SS = [kb.sb("SS", [128, 1]) for i in range(NB)]
            RS = [kb.sb("RS", [128, 1]) for i in range(NB)]
            HN = [kb.sb("HN", [128, D], BF16) for i in range(NB)]
            HT = [kb.sb("HT", [128, 8, 128], BF16) for i in range(NB)]
            PT = [kb.ps("PT", [128, 8, 128], BF16) for i in range(NB)]
            PKV = kb.ps("PKV", [128, 3, 512])
            PTK = kb.ps("PTK", [128, 4, 128], BF16)
            PC = kb.ps("PC", [128, 8])
            KV = [kb.sb("KV", [128, 1032]) for i in range(NB)]
            KSQ = kb.sb("KSQ", [128, 512])
            KSS = [kb.sb("KSS", [128, 8]) for i in range(NB)]
            KO = [kb.sb("KO", [128, 512]) for i in range(NB)]
            KOB = [kb.sb("KOB", [128, 512], BF16) for i in range(NB)]
            LF = [kb.sb("LF", [128, 8]) for i in range(NB)]
            HT.append(kb.sb("HT", [128, 8, 128], BF16))
            KV.append(kb.sb("KV", [128, 1032]))

            X.append(kb.sb("X", [128, D]))
            X.append(kb.sb("X", [128, D]))

            def a0(i):
                kb.dma("sp", X[i % 4][:], x_all[i * 128:(i + 1) * 128, :], writes=[X[i % 4]])

            def a1a(i):
                b = i % NB
                norm_a(X[i % 4], G, SS[b], RS[b], HN[b], SQ)

            def a1b(i):
                b = i % NB
                norm_b(HN[b], PT[b], (HT[i % 3][:], HT[i % 3]))

            def a2(i):
                ht, kv = HT[i % 3], KV[i % 3]
                for ci, (c0, c1) in enumerate(((0, 512), (512, 1024), (1024, 1032))):
                    for k in range(8):
                        kb.mm(PKV[:, ci, 0:c1 - c0], ht[:, k, :], W[:, k, c0:c1], k == 0, k == 7, [ht, W], [PKV])
                kb.op("dve", lambda e: e.tensor_copy(out=kv[:, 0:1024].rearrange("p (c n) -> p c n", c=2),
                                                     in_=PKV[:, 0:2, :]), [PKV], [kv])
                kb.op("dve", lambda e: e.tensor_copy(out=kv[:, 1024:1032], in_=PKV[:, 2, 0:8]), [PKV], [kv])

            def a3a(i):
                b = i % NB
                rows = slice(i * 128, (i + 1) * 128)
                kv_tile_a(KV[i % 3], KO[b], KOB[b], KSQ, KSS[b], o_k[rows, :], o_v[rows, :], (VE[:, i, :, 0:64], VE))

            def a3b(i):
                b = i % NB
                kv, lf = KV[i % 3], LF[b]
                rows = slice(i * 128, (i + 1) * 128)
                kv_tile_b(kv, KOB[b], lf, o_lf[rows, :], PTK, (KT[:, :, rows], KT))
                kb.mm(PC[:], TRIU[:], lf[:], True, False, [TRIU, lf], [PC])
                kb.mm(PC[:], ONES[:], RUN[:], False, True, [ONES, RUN], [PC])
                kb.op("dve", lambda e: e.tensor_copy(out=CK[:, i, :], in_=PC[:]), [PC], [CK])
                kb.op("dve", lambda e: e.tensor_tensor(out=RUN[:], in0=RUN[:], in1=lf[:], op=ALU.add),
                      [RUN, lf], [RUN])
                if i == 15:
                    kb.op("dve", lambda e: e.tensor_copy(out=RUN16[:], in_=RUN[:]), [RUN], [RUN16])

            nA = SEQ // 128
            a0(0)
            a0(1)
            ok = lambda i: 0 <= i < nA
            for t in range(nA + 2):
                if t + 2 < nA:
                    a0(t + 2)
                if ok(t - 2):
                    a3a(t - 2)
                if ok(t):
                    a1a(t)
                if ok(t - 1):
                    a2(t - 1)
                if ok(t):
                    a1b(t)
                if ok(t - 2):
                    a3b(t - 2)

        with kb.scope(LOHI):
            W = kb.sb("WB", [128, 8, 1024], BF16)
            WFU = kb.sb("WFU", [128, 8, 520], BF16)
            wv = w_b.rearrange("(k p) n -> p k n", p=128)
            wv2 = w_a.rearrange("(k p) n -> p k n", p=128)
            for k in range(8):
                kb.dma("pool", W[:, k, :], wv[:, k, :], writes=[W])
                kb.dma("pool", WFU[:, k, :], wv2[:, k, 1024:1544], writes=[WFU])
            NB = 2
            X = [kb.sb("X", [128, D]) for i in range(NB)]
            SQ = kb.sb("SQ", [128, D], BF16)
            SS = [kb.sb("SS", [128, 1]) for i in range(NB)]
            RS = [kb.sb("RS", [128, 1]) for i in range(NB)]
            HN = [kb.sb("HN", [128, D], BF16) for i in range(NB)]
            PT = [kb.ps("PT", [128, 8, 128], BF16) for i in range(NB)]
            PQ = kb.ps("PQ", [128, 2, 512])
            PTQ = kb.ps("PTQ", [128, 8, 128], BF16)
            PF = kb.ps("PF", [128, 8])
            PU = kb.ps("PU", [128, 512])
            QSQ = kb.sb("QSQ", [128, 512])
            QSS = [kb.sb("QSS", [128, 8]) for i in range(NB)]
            QB = [kb.sb("QB", [128, 1024], BF16) for i in range(NB)]
            LF = [kb.sb("LF", [128, 8]) for i in range(NB)]
            KVS = kb.sb("KVS", [128, 1032])
            HT4 = kb.sb("HT4", [128, 8, 512], BF16)
            HT4b = [HT4, kb.sb("HT4", [128, 8, 512], BF16)]
            X.append(KVS)
            QF = [kb.sb("QF", [128, 1024]) for i in range(2)]
            FF = [kb.sb("FF", [128, 8]) for i in range(2)]

            def tile_pos(i):
                return HT4b[(i // 4) % 2], slice((i % 4) * 128, (i % 4 + 1) * 128)

            def b0(i):
                kb.dma("sp", X[i % 3][:, 0:D], x_own[i * 128:(i + 1) * 128, :], writes=[X[i % 3]])

            def b1a(i):
                xb = X[i % 3]
                norm_a(_View(xb, xb[:, 0:D]), G, SS[i % 2], RS[i % 2], HN[i % 2], SQ)

            def b1b(i):
                ht4, ltk = tile_pos(i)
                norm_b(HN[i % 2], PT[i % 2], (ht4[:, :, ltk], ht4))
                if i % 4 == 3 or i == NT - 1:
                    grp = i // 4
                    t0 = grp * 512
                    nt = i % 4 + 1
                    for c in range(4):
                        for k in range(8):
                            kb.mm(PU[:, 0:nt * 128], WFU[:, k, 8 + c * 128:8 + (c + 1) * 128],
                                  ht4[:, k, 0:nt * 128], k == 0, k == 7, [WFU, ht4], [PU])
                        kb.op("act", lambda e: e.copy(out=UTO[:, c, t0:t0 + nt * 128], in_=PU[:, 0:nt * 128]),
                              [PU], [UTO])

            def b2(i):
                ht4, ltk = tile_pos(i)
                qf, ff = QF[i % 2], FF[i % 2]
                for ci in range(2):
                    for k in range(8):
                        kb.mm(PQ[:, ci, :], ht4[:, k, ltk], W[:, k, ci * 512:(ci + 1) * 512],
                              k == 0, k == 7, [ht4, W], [PQ])
                for k in range(8):
                    kb.mm(PF[:], ht4[:, k, ltk], WFU[:, k, 0:8], k == 0, k == 7, [ht4, WFU], [PF])
                kb.op("dve", lambda e: e.tensor_copy(out=qf[:].rearrange("p (c n) -> p c n", c=2), in_=PQ[:]),
                      [PQ], [qf])
                kb.op("dve", lambda e: e.tensor_copy(out=ff[:], in_=PF[:]), [PF], [ff])

            def b3(i):
                b = i % 2
                tok = slice(i * 128, (i + 1) * 128)
                qf, ff, qb = QF[b], FF[b], QB[b]
                head_norm((qf[:, 0:512], qf), 8, 64, QN, 0.125, QSQ, QSS[b], None, _View(qb, qb[:, 0:512]))
                head_norm((qf[:, 512:1024], qf), 4, 128, QNM, 128.0 ** -0.5, QSQ, QSS[b], None,
                          _View(qb, qb[:, 512:1024]))
                for c in range(8):
                    kb.tr(PTQ[:, c, :], qb[:, c * 128:(c + 1) * 128], IDb[:], [qb, IDb], [PTQ])
                kb.op("act", lambda e: e.copy(out=QT[:, :, tok], in_=PTQ[:, 0:4, :]), [PTQ], [QT])
                kb.op("act", lambda e: e.copy(out=QMT[:, :, tok], in_=PTQ[:, 4:8, :]), [PTQ], [QMT])
                if i < 16:
                    lf = LF[b]
                    logsig(lf, ff[:], ff)
                    kb.op("dve", lambda e: e.tensor_tensor(out=RUNO[:], in0=RUNO[:], in1=lf[:], op=ALU.add),
                          [RUNO, lf], [RUNO])
                    if i % 4 == 3:
                        kb.op("dve", lambda e: e.tensor_copy(out=RUNOJ[:, i // 4, :], in_=RUNO[:]),
                              [RUNO], [RUNOJ])

            okb = lambda i: 0 <= i < NT
            b0(0)
            b0(1)
            for t in range(NT + 2):
                if t + 2 < NT:
                    b0(t + 2)
                if okb(t):
                    b1a(t)
                if okb(t - 1):
                    b2(t - 1)
                if okb(t):
                    b1b(t)
                if okb(t - 2):
                    b3(t - 2)
            ht4, ltk = tile_pos(NT - 1)
            for k in range(8):
                kb.dma("pool", W[:, k, :], wv2[:, k, 0:1024], reads=[], writes=[W])
            for ci, (c0, c1) in enumerate(((0, 512), (512, 1024))):
                for k in range(8):
                    kb.mm(PQ[:, ci, :], ht4[:, k, ltk], W[:, k, c0:c1], k == 0, k == 7, [ht4, W], [PQ])
            kb.op("dve", lambda e: e.tensor_copy(out=KVS[:, 0:1024].rearrange("p (c n) -> p c n", c=2),
                                                 in_=PQ[:]), [PQ], [KVS])
            kb.op("dve", lambda e: e.tensor_copy(out=KVS[:, 1024:1032], in_=FF[(NT - 1) % 2][:]),
                  [FF[(NT - 1) % 2]], [KVS])
            KOS = _View(QF[1], QF[1][:, 0:512])
            KOBS = _View(QB[1], QB[1][:, 0:512])
            kv_tile(KVS, KOS, KOBS, QSQ, QSS[0], LFN, o_sk[:, :], o_sv[:, :], o_slf[:, :],
                    (VEN[:, :, 0:64], VEN), PTQ, (KTN[:], KTN))

        def normalize_out(pso, nq, OS, RR, PS_R, SEL, dst, dbuf):
            kb.op("dve", lambda e: e.tensor_copy(out=OS[:, 0:nq], in_=pso[:, 0:nq]), [pso], [OS])
            kb.mm(PS_R[:, 0:nq], SEL[:], OS[:, 0:nq], True, True, [SEL, OS], [PS_R])
            kb.op("act", lambda e: e.activation(out=RR[:, 0:nq], in_=PS_R[:, 0:nq], func=AF.Ln), [PS_R], [RR])
            kb.op("act", lambda e: e.activation(out=RR[:, 0:nq], in_=RR[:, 0:nq], func=AF.Exp, scale=-1.0),
                  [RR], [RR])
            kb.op("dve", lambda e: e.tensor_tensor(out=dst, in0=OS[0:64, 0:nq], in1=RR[:, 0:nq], op=ALU.mult),
                  [OS, RR], [dbuf])

        with kb.scope([[O_HI, ARENA_BYTES]]):
            MA = kb.sb("MA", [128, 4, 512], BF16)
            MB = kb.sb("MB", [128, 4, 512], BF16)
            kb.dma("pool", MA[:], ma_in, writes=[MA])
            kb.dma("pool", MB[:], mb_in, writes=[MB])
            SEL = kb.sb("SEL", [65, 64])
            kb.op("dve", lambda e: e.memset(SEL[:], 0.0), [], [SEL])
            kb.op("dve", lambda e: e.memset(SEL[64:65, :], 1.0), [], [SEL])
            BIAS = kb.sb("BIAS", [128, 4, 36, 8])
            TMP8 = kb.sb("TMP8", [128, 8])
            PCR = kb.ps("PCR", [128, 8])
            for J in range(4):
                nkb = 20 + 4 * J
                kb.op("dve", lambda e: e.scalar_tensor_tensor(
                    out=TMP8[:], in0=RUN16[:], scalar=RFL[:, 0:1], in1=RUNOJ[:, J, :], op0=ALU.mult,
                    op1=ALU.add), [RUN16, RFL, RUNOJ], [TMP8])
                kb.mm(PCR[:], ONES[:], TMP8[:], True, True, [ONES, TMP8], [PCR])
                kb.op("dve", lambda e: e.tensor_copy(out=CREF[:, J, :], in_=PCR[:]), [PCR], [CREF])
                kb.op("dve", lambda e: e.tensor_tensor(
                    out=BIAS[:, J, 0:nkb, :], in0=ADDM[:, J, 0:nkb, None].to_broadcast([128, nkb, 8]),
                    in1=CK[:, 0:nkb, :], op=ALU.subtract), [ADDM, CK], [BIAS])
                kb.op("dve", lambda e: e.tensor_tensor(
                    out=BIAS[:, J, 0:nkb, :], in0=BIAS[:, J, 0:nkb, :],
                    in1=CREF[:, J, None, :].to_broadcast([128, nkb, 8]), op=ALU.add), [BIAS, CREF], [BIAS])
            PS_S = [kb.ps("PS_S", [128, 512]) for i in range(3)]
            PS_O = [kb.ps("PS_O", [65, 512]) for i in range(2)]
            PS_R = kb.ps("PS_R", [64, 512])
            PTs = [kb.sb("PTs", [128, 512], BF16) for i in range(3)]
            OS = kb.sb("OS", [65, 512])
            RR = kb.sb("RR", [64, 512])
            LA = 2
            it = 0

            def run_pipe(items, tail):
                n = len(items)
                pend = []
                for idx in range(n + LA):
                    if idx < n:
                        items[idx][0]()
                    if idx >= LA:
                        items[idx - LA][1]()
                        ep = items[idx - LA][2]
                        if ep is not None:
                            pend.append([LA + 1, ep])
                    for pe_ in pend:
                        pe_[0] -= 1
                    for pe_ in [p_ for p_ in pend if p_[0] <= 0]:
                        pe_[1]()
                        pend.remove(pe_)
                for pe_ in pend:
                    pe_[1]()

            items = []
            for J in range(4):
                nkb = 20 + 4 * J
                qs = slice(J * 512, (J + 1) * 512)
                for h in range(8):
                    hp, po = h // 2, 64 * (h % 2)
                    pso = PS_O[(J * 8 + h) % 2]
                    for m in range(nkb):
                        pss, pts = PS_S[it % 3], PTs[it % 3]
                        it += 1

                        def qk(pss=pss, po=po, hp=hp, m=m, qs=qs):
                            kb.mm(pss[:], KT[po:po + 64, hp, m * 128:(m + 1) * 128], QT[po:po + 64, hp, qs],
                                  True, True, [KT, QT], [pss])

                        def post(pss=pss, pts=pts, pso=pso, J=J, m=m, h=h, nkb=nkb):
                            kb.op("act", lambda e: e.activation(out=pts[:], in_=pss[:], func=AF.Exp,
                                                                bias=BIAS[:, J, m, h:h + 1], scale=1.0),
                                  [pss, BIAS], [pts])
                            if 4 * J <= m < 4 * J + 4:
                                kb.op("dve", lambda e: e.tensor_tensor(out=pts[:], in0=pts[:], in1=MA[:, m - 4 * J, :],
                                                                       op=ALU.mult), [pts, MA], [pts])
                            if 16 + 4 * J <= m:
                                kb.op("dve", lambda e: e.tensor_tensor(out=pts[:], in0=pts[:],
                                                                       in1=MB[:, m - 16 - 4 * J, :], op=ALU.mult),
                                      [pts, MB], [pts])
                            kb.mm(pso[:], VE[:, m, h, :], pts[:], m == 0, m == nkb - 1, [VE, pts], [pso])

                        ep = None
                        if m == nkb - 1:
                            ep = (lambda pso=pso, h=h, qs=qs: normalize_out(pso, 512, OS, RR, PS_R, SEL,
                                                                            OFT[:, h, qs], OFT))
                        items.append((qk, post, ep))
            run_pipe(items, None)

            CKS = [kb.sb("CKS", [128, 4, 512], BF16) for i in range(2)]
            STGC = [kb.sb("STGC", [128, 2, 512]) for i in range(3)]
            stg_i = [0]
            LFS = kb.sb("LFS", [128, 32, 8])
            CKN = kb.sb("CKN", [128, 8])
            BIASN = kb.sb("BIASN", [128, 8])
            CRS = kb.sb("CRS", [128, 8])
            PTK = kb.ps("PTK2", [128, 2, 4, 128], BF16)
            for sbi in range(2):
                r0 = 64 * sbi
                rs_ = slice(r0, r0 + 64)
                qcol = slice(HALF + r0, HALF + r0 + 64)
                ckv = c_k[sbi].rearrange("(n p) f -> p n f", p=128)
                cvf = c_v[sbi].rearrange("(n p) f -> p n f", p=128)
                kb.dma("sp", LFS[:], c_lf[sbi].rearrange("(n p) h -> p n h", p=128), writes=[LFS])
                for g4 in range(8):
                    cks = CKS[g4 % 2]
                    for hf2 in range(2):
                        t0_ = g4 * 4 + hf2 * 2
                        stg = STGC[stg_i[0] % 3]
                        stg_i[0] += 1
                        kb.dma("sp", stg[:], ckv[:, t0_:t0_ + 2, :], writes=[stg])
                        kb.op("dve", lambda e: e.tensor_copy(out=cks[:, hf2 * 2:hf2 * 2 + 2, :], in_=stg[:]),
                              [stg], [cks])
                        stg = STGC[stg_i[0] % 3]
                        stg_i[0] += 1
                        kb.dma("sp", stg[:], cvf[:, t0_:t0_ + 2, :], writes=[stg])
                        kb.op("pool", lambda e: e.tensor_copy(
                            out=VE[:, t0_:t0_ + 2, :, 0:64],
                            in_=stg[:].rearrange("p n (h d) -> p n h d", d=64)), [stg], [VE])
                    for j2 in range(2):
                        for j in range(2):
                            for hp in range(4):
                                kb.tr(PTK[:, j, hp, :], cks[:, j2 * 2 + j, hp * 128:(hp + 1) * 128], IDb[:],
                                      [cks, IDb], [PTK])
                        c0 = g4 * 512 + j2 * 256
                        kb.op("act", lambda e: e.copy(
                            out=KT[:, :, c0:c0 + 256].rearrange("p h (j t) -> p j h t", j=2),
                            in_=PTK[:]), [PTK], [KT])
                kb.op("dve", lambda e: e.memset(RUN[:], 0.0), [], [RUN])
                for i in range(32):
                    kb.mm(PCR[:], TRIU[:], LFS[:, i, :], True, False, [TRIU, LFS], [PCR])
                    kb.mm(PCR[:], ONES[:], RUN[:], False, True, [ONES, RUN], [PCR])
                    kb.op("dve", lambda e: e.tensor_copy(out=CK[:, i, :], in_=PCR[:]), [PCR], [CK])
                    kb.op("dve", lambda e: e.tensor_tensor(out=RUN[:], in0=RUN[:], in1=LFS[:, i, :], op=ALU.add),
                          [RUN, LFS], [RUN])
                kb.mm(PCR[rs_, :], TRIU[rs_, rs_], LFN[rs_, :], True, False, [TRIU, LFN], [PCR])
                kb.mm(PCR[rs_, :], ONES[:, rs_], RUN[:], False, True, [ONES, RUN], [PCR])
                kb.op("dve", lambda e: e.tensor_copy(out=CKN[rs_, :], in_=PCR[rs_, :]), [PCR], [CKN])
                kb.mm(PCR[:], ONES[:], RUN[:], True, False, [ONES, RUN], [PCR])
                kb.mm(PCR[:], ONES[rs_, :], LFN[rs_, :], False, True, [ONES, LFN], [PCR])
                kb.op("dve", lambda e: e.tensor_copy(out=CRS[:], in_=PCR[:]), [PCR], [CRS])
                kb.op("dve", lambda e: e.tensor_tensor(out=BIAS[:, 0, 0:32, :],
                                                       in0=CRS[:, None, :].to_broadcast([128, 32, 8]),
                                                       in1=CK[:, 0:32, :], op=ALU.subtract), [CRS, CK], [BIAS])
                kb.op("dve", lambda e: e.tensor_tensor(out=BIASN[rs_, :], in0=CRS[rs_, :], in1=CKN[rs_, :],
                                                       op=ALU.subtract), [CRS, CKN], [BIASN])
                items = []
                for h in range(8):
                    hp, po = h // 2, 64 * (h % 2)
                    pso = PS_O[h % 2]
                    for m in range(33):
                        pss, pts = PS_S[it % 3], PTs[it % 3]
                        it += 1
                        if m < 32:
                            def qk(pss=pss, po=po, hp=hp, m=m):
                                kb.mm(pss[:, 0:64], KT[po:po + 64, hp, m * 128:(m + 1) * 128],
                                      QT[po:po + 64, hp, qcol], True, True, [KT, QT], [pss])

                            def post(pss=pss, pts=pts, pso=pso, m=m, h=h):
                                kb.op("act", lambda e: e.activation(out=pts[:, 0:64], in_=pss[:, 0:64], func=AF.Exp,
                                                                    bias=BIAS[:, 0, m, h:h + 1], scale=1.0),
                                      [pss, BIAS], [pts])
                                kb.mm(pso[:, 0:64], VE[:, m, h, :], pts[:, 0:64], m == 0, False, [VE, pts], [pso])
                            ep = None
                        else:
                            def qk(pss=pss, po=po, hp=hp):
                                kb.mm(pss[rs_, 0:64], KTN[po:po + 64, hp, rs_], QT[po:po + 64, hp, qcol],
                                      True, True, [KTN, QT], [pss])

                            def post(pss=pss, pts=pts, pso=pso, h=h):
                                kb.op("act", lambda e: e.activation(out=pts[rs_, 0:64], in_=pss[rs_, 0:64],
                                                                    func=AF.Exp, bias=BIASN[rs_, h:h + 1], scale=1.0),
                                      [pss, BIASN], [pts])
                                kb.op("dve", lambda e: e.tensor_tensor(out=pts[rs_, 0:64], in0=pts[rs_, 0:64],
                                                                       in1=TRIU[rs_, rs_], op=ALU.mult),
                                      [pts, TRIU], [pts])
                                kb.mm(pso[:, 0:64], VEN[rs_, h, :], pts[rs_, 0:64], False, True, [VEN, pts], [pso])
                            ep = (lambda pso=pso, h=h: normalize_out(pso, 64, OS, RR, PS_R, SEL, OFT[:, h, qcol], OFT))
                        items.append((qk, post, ep))
                run_pipe(items, None)


        OMT = kb.at(O_OMT, [128, 4, NOWN], BF16)
        with kb.scope([[O_FREE2, O_QMT], [O_HI, ARENA_BYTES]]):
            WM = kb.sb("WM", [128, 8, 1024], BF16)
            wv = w_mkv.rearrange("(k p) n -> p k n", p=128)
            for k in range(8):
                kb.dma("pool", WM[:, k, :], wv[:, k, :], writes=[WM])
            GM = kb.sb("GM", [128, D])
            KNM = kb.sb("KNM", [128, 512])
            kb.dma("sp", GM[:], gmem, writes=[GM])
            kb.dma("sp", KNM[:], knm_rep, writes=[KNM])
            ONESb = kb.sb("ONESb", [128, 128], BF16)
            kb.op("dve", lambda e: e.memset(ONESb[:], 1.0), [], [ONESb])
            X = [kb.sb("X", [128, D]) for i in range(2)]
            SQ = kb.sb("SQ", [128, D], BF16)
            SS = [kb.sb("SS", [128, 1]) for i in range(2)]
            RS = [kb.sb("RS", [128, 1]) for i in range(2)]
            HN = [kb.sb("HN", [128, D], BF16) for i in range(2)]
            HT = [kb.sb("HT", [128, 8, 128], BF16) for i in range(2)]
            KVm = [kb.sb("KVm", [128, 1024]) for i in range(2)]
            KSQ = kb.sb("KSQ", [128, 512])
            KSS = kb.sb("KSS", [128, 8])
            KO = [kb.sb("KO", [128, 512]) for i in range(2)]
            KOB = [kb.sb("KOB", [128, 512], BF16) for i in range(2)]
            MKT = kb.sb("MKT", [128, 4, 256], BF16)
            MV = kb.sb("MV", [128, 2, 512], BF16)
            CST = [kb.sb("CST", [128, 2, 512], BF16) for i in range(2)]
            PTs = [kb.sb("PTs", [128, 512], BF16) for i in range(2)]
            RR = kb.sb("RR", [128, 512])
            PT = [kb.ps("PT", [128, 8, 128], BF16) for i in range(2)]
            PKV = kb.ps("PKV", [128, 2, 512])
            PS_S = [kb.ps("PS_S", [128, 512]) for i in range(2)]
            PS_O = kb.ps("PS_O", [128, 512])
            PS_D = kb.ps("PS_D", [128, 512])
            for i in range(2):
                x, ht, kv = X[i], HT[i], KVm[i]
                kb.dma("sp", x[:], mem_in[i * 128:(i + 1) * 128, :], writes=[x])
                norm_tile(x, GM, SS[i], RS[i], HN[i], SQ, PT[i], (ht[:], ht))
                for ci in range(2):
                    for k in range(8):
                        kb.mm(PKV[:, ci, :], ht[:, k, :], WM[:, k, ci * 512:(ci + 1) * 512], k == 0, k == 7,
                              [ht, WM], [PKV])
                kb.op("dve", lambda e: e.tensor_copy(out=kv[:].rearrange("p (c n) -> p c n", c=2), in_=PKV[:]),
                      [PKV], [kv])
                rows = slice(i * 128, (i + 1) * 128)
                kb.dma("sp", o_mv[rows, :], kv[:, 512:1024], reads=[kv])
                kb.op("pool", lambda e: e.tensor_copy(out=MV[:, i, :], in_=kv[:, 512:1024]), [kv], [MV])
                head_norm((kv[:, 0:512], kv), 4, 128, KNM, 1.0, KSQ, KSS, KO[i], KOB[i])
                kb.dma("sp", o_mk[rows, :], KO[i][:], reads=[KO[i]])
                for h in range(4):
                    kb.tr(PT[i][:, h, :], KOB[i][:, h * 128:(h + 1) * 128], IDb[:], [KOB[i], IDb], [PT[i]])
                kb.op("act", lambda e: e.copy(out=MKT[:, :, rows], in_=PT[i][:, 0:4, :]), [PT[i]], [MKT])

            def mem_attend(qs, nq, it0):
                it = it0
                for h in range(4):
                    for mb in range(2):
                        pss, pts = PS_S[it % 2], PTs[it % 2]
                        it += 1
                        kb.mm(pss[:, 0:nq], MKT[:, h, mb * 128:(mb + 1) * 128], QMT[:, h, qs], True, True,
                              [MKT, QMT], [pss])
                        kb.op("act", lambda e: e.activation(out=pts[:, 0:nq], in_=pss[:, 0:nq], func=AF.Exp),
                              [pss], [pts])
                        kb.mm(PS_O[:, 0:nq], MV[:, mb, h * 128:(h + 1) * 128], pts[:, 0:nq], mb == 0, mb == 1,
                              [MV, pts], [PS_O])
                        kb.mm(PS_D[:, 0:nq], ONESb[:], pts[:, 0:nq], mb == 0, mb == 1, [ONESb, pts], [PS_D])
                    kb.op("act", lambda e: e.activation(out=RR[:, 0:nq], in_=PS_D[:, 0:nq], func=AF.Ln), [PS_D], [RR])
                    kb.op("act", lambda e: e.activation(out=RR[:, 0:nq], in_=RR[:, 0:nq], func=AF.Exp, scale=-1.0),
                          [RR], [RR])
                    kb.op("dve", lambda e: e.tensor_tensor(out=OMT[:, h, qs], in0=PS_O[:, 0:nq], in1=RR[:, 0:nq],
                                                           op=ALU.mult), [PS_O, RR], [OMT])
                return it

            it = 0
            for J in range(4):
                it = mem_attend(slice(J * 512, (J + 1) * 512), 512, it)
            for sbi in range(2):
                ck, cv = CST[0], CST[1]
                kb.dma("pool", ck[:], c_mk[sbi].rearrange("(n p) f -> p n f", p=128), writes=[ck])
                kb.dma("pool", MV[:], c_mv[sbi].rearrange("(n p) f -> p n f", p=128), writes=[MV])
                for i in range(2):
                    for h in range(4):
                        kb.tr(PT[i][:, h, :], ck[:, i, h * 128:(h + 1) * 128], IDb[:], [ck, IDb], [PT[i]])
                    kb.op("act", lambda e: e.copy(out=MKT[:, :, i * 128:(i + 1) * 128], in_=PT[i][:, 0:4, :]),
                          [PT[i]], [MKT])
                it = mem_attend(slice(HALF + 64 * sbi, HALF + 64 * sbi + 64), 64, it)

        UTF = kb.at(O_UTF, [128, 4, HALF], BF16)
        YGALL = kb.at(O_UTF, [128, 32, 272], BF16)
        YS2 = kb.at(O_YS2, [128, 4, NOWN], BF16)
        with kb.scope([[O_FREE2, O_UTO], [O_HI, ARENA_BYTES]]):
            WU = kb.sb("WU", [128, 8, 512], BF16)
            wv2 = w_a.rearrange("(k p) n -> p k n", p=128)
            STGW = [kb.sb("STGW", [128, 2, 512]) for i in range(2)]
            for k2 in range(4):
                stg = STGW[k2 % 2]
                kb.dma("sp", stg[:], wv2[:, 2 * k2:2 * k2 + 2, 1032:1544], writes=[stg])
                kb.op(("dve", "pool")[k2 % 2], lambda e: e.tensor_copy(out=WU[:, 2 * k2:2 * k2 + 2, :], in_=stg[:]),
                      [stg], [WU])
            X = [kb.sb("X", [128, D]) for i in range(2)]
            SQ = kb.sb("SQ", [128, D], BF16)
            SS = [kb.sb("SS", [128, 1]) for i in range(2)]
            RS = [kb.sb("RS", [128, 1]) for i in range(2)]
            HN = [kb.sb("HN", [128, D], BF16) for i in range(2)]
            HT4 = [kb.sb("HT4", [128, 8, 512], BF16) for i in range(2)]
            PT = [kb.ps("PT", [128, 8, 128], BF16) for i in range(2)]
            PU = [kb.ps("PU", [128, 512]) for i in range(2)]
            for grp in range(4):
                ht4 = HT4[grp % 2]
                for j in range(4):
                    i = grp * 4 + j
                    b = i % 2
                    kb.dma("sp", X[b][:], x_all[i * 128:(i + 1) * 128, :], writes=[X[b]])
                    norm_tile(X[b], G, SS[b], RS[b], HN[b], SQ, PT[b], (ht4[:, :, j * 128:(j + 1) * 128], ht4))
                for c in range(4):
                    pu = PU[c % 2]
                    for k in range(8):
                        kb.mm(pu[:], WU[:, k, c * 128:(c + 1) * 128], ht4[:, k, :], k == 0, k == 7, [WU, ht4], [pu])
                    kb.op("act", lambda e: e.copy(out=UTF[:, c, grp * 512:(grp + 1) * 512], in_=pu[:]), [pu], [UTF])

        R_A = [O_FREE2, O_UTO]
        R_B = [O_HI, ARENA_BYTES]
        with kb.scope([R_B]):
            T0 = kb.sb("T0", [128, 32, 128], BF16)
            WST = kb.sb("WST", [128, 32, 128], BF16)
            VVB = kb.sb("VVB", [128, 32, 128], BF16)
            RC = kb.sb("RC", [128, 32, 9, 2])
            CRc = kb.sb("CRc", [128, 32])
            CIs = kb.sb("CIs", [128, 32])
            EALL = kb.sb("EALL", [128, 32])
            INITA = kb.sb("INITA", [128, 3, 32])
            FINS = kb.sb("FINS", [128, 3, 32])
            SG = kb.sb("SG", [128, 1])
            kb.dma("sp", SG[:], sp_sg, writes=[SG])
            with kb.scope([R_A]):
                P1 = kb.sb("P1", [128, 32, 41])
                P2 = kb.sb("P2", [128, 32, 41])
                CA = kb.sb("CA", [128, 32, 16])
                CB = kb.sb("CB", [128, 32, 16])
                BA = kb.sb("BA", [128, 32, 16])
                BB = kb.sb("BB", [128, 32, 16])
                SCR0 = kb.regions[0][0]
                ARE = kb.sb("ARE", [128, 32])
                AIM = kb.sb("AIM", [128, 32])
                LDT = kb.sb("LDT", [128, 32])
                ET = kb.sb("ET", [128, 41])
                CRE = kb.sb("CRE", [128, 32, 16])
                CIM = kb.sb("CIM", [128, 32, 16])
                DAR = kb.sb("DAR", [128, 32])
                DAI = kb.sb("DAI", [128, 32])
                Y = kb.sb("Y", [128, 32, 41])
                TF = kb.sb("TF", [128, 32, 41])
                TI = kb.sb("TI", [128, 32, 41], I32)
                MAG = kb.sb("MAG", [128, 32, 41])
                SIN, COS = P2, P1
                t32 = [kb.sb("t32", [128, 32]) for i in range(6)]
                for dst, src in ((ARE, sp_are), (AIM, sp_aim), (LDT, sp_ldt), (ET, sp_et), (BA, sp_bre), (BB, sp_bim),
                                 (CRE, sp_cre), (CIM, sp_cim)):
                    kb.dma("sp", dst[:], src, writes=[dst])
                TT = lambda o, a, b_, op, rd, wr: kb.op("dve", lambda e: e.tensor_tensor(out=o, in0=a, in1=b_, op=op),
                                                        rd, wr)
                TS = lambda o, a, s1, s2, op0, op1, rd, wr: kb.op(
                    "dve", lambda e: e.tensor_scalar(out=o, in0=a, scalar1=s1, scalar2=s2, op0=op0,
                                                     **({"op1": op1} if op1 is not None else {})), rd, wr)
                kb.op("act", lambda e: e.activation(out=LDT[:], in_=LDT[:], func=AF.Exp), [LDT], [LDT])
                TT(DAR[:], LDT[:], ARE[:], ALU.mult, [LDT, ARE], [DAR])
                TT(DAI[:], LDT[:], AIM[:], ALU.mult, [LDT, AIM], [DAI])
                bc_g = lambda t: t[:, :, None].to_broadcast([128, 32, 41])
                bc_e = lambda t: t[:, None, :].to_broadcast([128, 32, 41])
                TT(MAG[:], bc_g(DAR), bc_e(ET), ALU.mult, [DAR, ET], [MAG])
                kb.op("act", lambda e: e.activation(out=MAG[:], in_=MAG[:], func=AF.Exp), [MAG], [MAG])
                TT(Y[:], bc_g(DAI), bc_e(ET), ALU.mult, [DAI, ET], [Y])
                TS(Y[:], Y[:], 1.0 / (2.0 * np.pi), None, ALU.mult, None, [Y], [Y])

                def sin_of(dst, shift):
                    TS(TF[:], Y[:], float(shift), None, ALU.add, None, [Y], [TF])
                    kb.op("dve", lambda e: e.tensor_copy(out=TI[:], in_=TF[:]), [TF], [TI])
                    kb.op("dve", lambda e: e.tensor_copy(out=dst[:], in_=TI[:]), [TI], [dst])
                    TT(TF[:], TF[:], dst[:], ALU.subtract, [TF, dst], [TF])
                    TS(dst[:], TF[:], 0.5, None, ALU.is_gt, None, [TF], [dst])
                    TT(TF[:], TF[:], dst[:], ALU.subtract, [TF, dst], [TF])
                    TS(dst[:], TF[:], -0.5, None, ALU.is_lt, None, [TF], [dst])
                    TT(TF[:], TF[:], dst[:], ALU.add, [TF, dst], [TF])
                    kb.op("act", lambda e: e.activation(out=dst[:], in_=TF[:], func=AF.Sin, scale=6.283185),
                          [TF], [dst])

                sin_of(SIN, 0.0)
                sin_of(COS, 0.25)
                TT(COS[:], COS[:], MAG[:], ALU.mult, [COS, MAG], [COS])
                TT(SIN[:], SIN[:], MAG[:], ALU.mult, [SIN, MAG], [SIN])
                top, bot = slice(0, 64), slice(64, 128)
                cp = lambda o, a, rd, wr: kb.op("dve", lambda e: e.tensor_copy(out=o, in_=a), rd, wr)
                NR, DEN, C1, C2, C3, C4 = t32
                abr, abi = COS[:, :, 16], SIN[:, :, 16]
                TS(NR[:], abr, -1.0, None, ALU.add, None, [COS], [NR])
                TT(DEN[:], ARE[:], ARE[:], ALU.mult, [ARE], [DEN])
                TT(C1[:], AIM[:], AIM[:], ALU.mult, [AIM], [C1])
                TT(DEN[:], DEN[:], C1[:], ALU.add, [DEN, C1], [DEN])
                kb.op("dve", lambda e: e.reciprocal(out=DEN[:], in_=DEN[:]), [DEN], [DEN])
                TT(C1[:], NR[:], ARE[:], ALU.mult, [NR, ARE], [C1])
                TT(C2[:], abi, AIM[:], ALU.mult, [SIN, AIM], [C2])
                TT(C1[:], C1[:], C2[:], ALU.add, [C1, C2], [C1])
                TT(CRc[:], C1[:], DEN[:], ALU.mult, [C1, DEN], [CRc])
                TT(C1[:], abi, ARE[:], ALU.mult, [SIN, ARE], [C1])
                TT(C2[:], NR[:], AIM[:], ALU.mult, [NR, AIM], [C2])
                TT(C1[:], C1[:], C2[:], ALU.subtract, [C1, C2], [C1])
                TT(C3[:], C1[:], DEN[:], ALU.mult, [C1, DEN], [C3])
                TS(CIs[:], C3[:], SG[:, 0:1], None, ALU.mult, None, [C3, SG], [CIs])
                bc_h = lambda t: t[:, :, None].to_broadcast([128, 32, 16])
                TT(CA[:], CRE[:], bc_h(CRc), ALU.mult, [CRE, CRc], [CA])
                TT(CB[:], CIM[:], bc_h(C3), ALU.mult, [CIM, C3], [CB])
                TT(CA[:], CA[:], CB[:], ALU.subtract, [CA, CB], [CA])
                TT(CB[:], CRE[:], bc_h(C3), ALU.mult, [CRE, C3], [CB])
                TT(CRE[:], CIM[:], bc_h(CRc), ALU.mult, [CIM, CRc], [CRE])
                TT(CB[:], CB[:], CRE[:], ALU.add, [CB, CRE], [CB])
                TS(CB[:], CB[:], -1.0, None, ALU.mult, None, [CB], [CB])
                TS(CA[bot], CA[bot], -1.0, None, ALU.mult, None, [CA], [CA])
                TS(BB[top], BB[top], -1.0, None, ALU.mult, None, [BB], [BB])
                cp(TF[bot], P1[bot], [P1], [TF])
                cp(P1[bot], P2[bot], [P2], [P1])
                cp(P2[bot], TF[bot], [TF], [P2])
                cp(RC[top, :, :, 0], P1[top, :, 32:41], [P1], [RC])
                cp(RC[top, :, :, 1], P2[top, :, 32:41], [P2], [RC])
                TS(RC[bot, :, :, 0], P1[bot, :, 32:41], -1.0, None, ALU.mult, None, [P1], [RC])
                cp(RC[bot, :, :, 1], P2[bot, :, 32:41], [P2], [RC])
                H0 = kb.sb("H0", [128, 2, 32])
                H0S = kb.sb("H0S", [128, 2, 32])
                kb.dma("sp", H0[:], sp_h0, writes=[H0])
                kb.dma("sp", H0S[:], sp_h0s, writes=[H0S])
                TT(C1[:], CRc[:], CRc[:], ALU.mult, [CRc], [C1])
                TT(C2[:], C3[:], C3[:], ALU.mult, [C3], [C2])
                TT(C1[:], C1[:], C2[:], ALU.add, [C1, C2], [C1])
                kb.op("dve", lambda e: e.reciprocal(out=C1[:], in_=C1[:]), [C1], [C1])
                b2 = lambda t: t[:, None, :].to_broadcast([128, 2, 32])
                TT(H0[:], H0[:], b2(CRc), ALU.mult, [H0, CRc], [H0])
                TT(H0S[:], H0S[:], b2(CIs), ALU.mult, [H0S, CIs], [H0S])
                TT(H0[:], H0[:], H0S[:], ALU.add, [H0, H0S], [H0])
                TT(INITA[:, 1:3, :], H0[:], b2(C1), ALU.mult, [H0, C1], [INITA])
                kb.barrier()
                kb.regions = [[SCR0, O_UTO]]
                BIGA = kb.sb("BIGA", [128, 16, 8, 16])
                BIGB = kb.sb("BIGB", [128, 16, 8, 16])
                BIGT = kb.sb("BIGT", [128, 16, 8, 16])
                CAUS = kb.sb("CAUS", [128, 128])
                DCOL = kb.sb("DCOL", [128, 32])
                TMPM = kb.sb("TMPM", [128, 128])
                kb.dma("sp", CAUS[:], sp_caus, writes=[CAUS])
                kb.dma("sp", DCOL[:], sp_dcol, writes=[DCOL])
                PSM = [kb.ps("PSM", [128, 128]) for i in range(2)]

                def big(dst, xa, xb, c0, gs, wr_extra=None):
                    bx = lambda t: t[:, gs, None, :].to_broadcast([128, 16, 8, 16])
                    bp = lambda t: t[:, gs, c0:c0 + 8, None].to_broadcast([128, 16, 8, 16])
                    TT(BIGT[:], bx(xb), bp(P2), ALU.mult, [xb, P2], [BIGT])
                    TT(dst[:], bx(xa), bp(P1), ALU.mult, [xa, P1], [dst])
                    TT(dst[:], dst[:], BIGT[:], ALU.add, [dst, BIGT], [dst])

                for gh in range(2):
                    gs = slice(gh * 16, (gh + 1) * 16)
                    big(BIGA, BA, BB, 8, gs)
                    big(BIGB, CA, CB, 24, gs)
                    for gl in range(16):
                        g = gh * 16 + gl
                        ps = PSM[g % 2]
                        kb.mm(ps[:], BIGA[:, gl].rearrange("p a b -> p (a b)"),
                              BIGB[:, gl].rearrange("p a b -> p (a b)"), True, True, [BIGA, BIGB], [ps])
                        TT(TMPM[:], ps[:], CAUS[:], ALU.mult, [ps, CAUS], [TMPM])
                        kb.op("dve", lambda e: e.scalar_tensor_tensor(out=T0[:, g, :], in0=IDf[:],
                                                                      scalar=DCOL[:, g:g + 1], in1=TMPM[:],
                                                                      op0=ALU.mult, op1=ALU.add),
                              [IDf, DCOL, TMPM], [T0])
                    big(BIGA, BA, BB, 0, gs)
                    for gl in range(16):
                        g = gh * 16 + gl
                        ps = PSM[g % 2]
                        kb.tr(ps[:], BIGA[:, gl].rearrange("p a b -> p (a b)"), IDf[:], [BIGA, IDf], [ps])
                        kb.op("act", lambda e: e.copy(out=WST[:, g, :], in_=ps[:]), [ps], [WST])
                    big(BIGB, CA, CB, 16, gs)
                    kb.op("act", lambda e: e.copy(out=VVB[:, gs, :], in_=BIGB[:].rearrange("p g a b -> p g (a b)")),
                          [BIGB], [VVB])

            with kb.scope([R_A]):
                ESEL = kb.sb("ESEL", [128, 64, 128], BF16)
                kb.dma("pool", ESEL[:, 0:32, :], sp_esel[:, 0:32, :], writes=[ESEL])
                kb.dma("pool", ESEL[:, 32:64, :], sp_esel[:, 32:64, :], writes=[ESEL])
                WG = kb.sb("WG", [128, 4, 1024], BF16)
                kb.dma("pool", WG[:], w_glu.rearrange("(c p) n -> p c n", p=128), writes=[WG])
                SWP = kb.sb("SWP", [128, 128])
                kb.dma("sp", SWP[:], sp_swap, writes=[SWP])
                NL = 2
                U8 = [kb.sb("U8", [128, 272], BF16) for i in range(NL)]
                HBL = [[kb.sb("HB", [128, 276], BF16) for i in range(3)] for l in range(NL)]
                RK = [kb.sb("RK", [128, 9, 128], BF16) for i in range(NL)]
                HSL = [[kb.sb("HS", [128, 2, 10]) for i in range(2)] for l in range(NL)]
                HSBL = [kb.sb("HSB", [128, 2, 10], BF16) for l in range(NL)]
                RKF = [kb.sb("RKF", [128, 4, 128]) for i in range(NL)]
                GT1L = [kb.sb("GT1", [128, 272]) for l in range(NL)]
                GT2L = [kb.sb("GT2", [128, 272]) for l in range(NL)]
                GT1B = kb.sb("GT1B", [128, 512])
                PU8L = [kb.ps("PU8", [128, 272]) for l in range(NL)]
                PXL = [kb.ps("PX", [128, 512]) for l in range(NL)]
                PXSL = [kb.ps("PXS", [128, 2, 10]) for l in range(NL)]
                PY8L = [kb.ps("PY8", [128, 272]) for l in range(NL)]
                PX = PXL
                PY8 = PY8L[0]

                def interleave(gens):
                    gens = list(gens)
                    while gens:
                        for g_ in list(gens):
                            try:
                                next(g_)
                            except StopIteration:
                                gens.remove(g_)

                evac_i = [0]

                def evac(dst, dbuf, src, sbuf):
                    evac_i[0] += 1
                    if evac_i[0] % 2:
                        kb.op("act", lambda e: e.copy(out=dst, in_=src), [sbuf], [dbuf])
                    else:
                        kb.op("dve", lambda e: e.tensor_copy(out=dst, in_=src), [sbuf], [dbuf])

                def build_rk(g, rk):
                    for hs, idv in ((slice(0, 64), IDf[0:64, 0:64]), (slice(64, 128), IDf[64:128, 64:128])):
                        kb.op("pool", lambda e: e.tensor_tensor(
                            out=rk[hs].rearrange("p k (c j) -> p k c j", c=2),
                            in0=idv[:, None, None, :].to_broadcast([64, 9, 2, 64]),
                            in1=RC[hs, g, :, :, None].to_broadcast([64, 9, 2, 64]), op=ALU.mult),
                            [IDf, RC], [rk])

                def build_rkf(g, rkf):
                    for hs, idv in ((slice(0, 64), IDf[0:64, 0:64]), (slice(64, 128), IDf[64:128, 64:128])):
                        kb.op("pool", lambda e: e.tensor_tensor(
                            out=rkf[hs].rearrange("p k (c j) -> p k c j", c=2),
                            in0=idv[:, None, None, :].to_broadcast([64, 4, 2, 64]),
                            in1=RC[hs, g, 0:4, :, None].to_broadcast([64, 4, 2, 64]), op=ALU.mult),
                            [IDf, RC], [rkf])

                def build_u8(src, ct, gp, ncol, u8, PU8):
                    sv = src[:, ct, 0:ncol * 8].rearrange("p (n s) -> p s n", s=8)
                    for s8 in range(8):
                        kb.mm(PU8[:, 0:ncol], ESEL[:, gp * 8 + s8, :], sv[:, s8, :], s8 == 0, s8 == 7,
                              [ESEL, src], [PU8])
                    evac(u8[:, 0:ncol], u8, PU8[:, 0:ncol], PU8)

                def body1(g, ln):
                    ct, gp = g // 8, g % 8
                    rk, u8, HB, px = RK[ln], U8[ln], HBL[ln], PXL[ln]
                    build_rk(g, rk)
                    build_u8(UTF, ct, gp, 256, u8, PU8L[ln])
                    yield
                    kb.mm(px[:, 0:256], WST[:, g, :], u8[:, 0:256], True, True, [WST, u8], [px])
                    hcur = HB[0]
                    evac(hcur[:, 0:256], hcur, px[:, 0:256], px)
                    yield
                    n = 256
                    for k in range(8):
                        n //= 2
                        hv = hcur[:, 0:2 * n].rearrange("p (n two) -> p two n", two=2)
                        kb.mm(px[:, 0:n], IDb[:], hv[:, 1, :], True, False, [IDb, hcur], [px])
                        kb.mm(px[:, 0:n], rk[:, k, :], hv[:, 0, :], False, True, [rk, hcur], [px])
                        hnext = HB[(k + 1) % 3]
                        if k < 7:
                            evac(hnext[:, 0:n], hnext, px[:, 0:n], px)
                            hcur = hnext
                        else:
                            kb.op("dve", lambda e: e.tensor_copy(out=EALL[:, g:g + 1], in_=px[:, 0:1]), [px], [EALL])
                        yield

                for g0 in range(0, 32, NL):
                    interleave([body1(g0 + ln, ln) for ln in range(NL)])
                kb.op("dve", lambda e: e.tensor_scalar(out=INITA[:, 0, :], in0=EALL[:], scalar1=RFL[:, 0:1],
                                                       scalar2=None, op0=ALU.mult), [EALL, RFL], [INITA])

                for HS in HSL:
                    for hsb in HS:
                        kb.op("dve", lambda e: e.memset(hsb[:], 0.0), [], [hsb])

                def body2(g, ln):
                    ct, gp = g // 8, g % 8
                    rk, u8, rkf, HB, px = RK[ln], U8[ln], RKF[ln], HBL[ln], PXL[ln]
                    HS, HSB, PXS, PY8, GT1, GT2 = HSL[ln], HSBL[ln], PXSL[ln], PY8L[ln], GT1L[ln], GT2L[ln]
                    build_rk(g, rk)
                    build_rkf(g, rkf)
                    build_u8(UTO, ct, gp, 272, u8, PU8L[ln])
                    yield
                    kb.mm(px[:, 0:272], WST[:, g, :], u8[:, 0:272], True, True, [WST, u8], [px])
                    hcur = HB[0]
                    evac(hcur[:, 1:257], hcur, px[:, 0:256], px)
                    kb.op("pool", lambda e: e.tensor_copy(out=hcur[:, 0:1], in_=INITA[:, 0, g:g + 1]), [INITA], [hcur])
                    hs = HS[0]
                    kb.op("dve", lambda e: e.tensor_copy(out=hs[:, :, 1:9],
                                                         in_=px[:, 256:272].rearrange("p (a b) -> p a b", a=2)),
                          [px], [hs])
                    kb.op("pool", lambda e: e.tensor_copy(out=hs[:, :, 0], in_=INITA[:, 1:3, g]), [INITA], [hs])
                    yield
                    for k in range(9):
                        sft = 1 << k
                        kb.mm(px[:, 0:257], IDb[:], hcur[:, 0:257], True, False, [IDb, hcur], [px])
                        kb.mm(px[:, sft:257], rk[:, k, :], hcur[:, 0:257 - sft], False, True, [rk, hcur], [px])
                        hnext = HB[(k + 1) % 3]
                        evac(hnext[:, 0:257], hnext, px[:, 0:257], px)
                        hcur = hnext
                        if k < 4:
                            kb.mm(PXS[:], IDf[:], hs[:], True, False, [IDf, hs], [PXS])
                            kb.mm(PXS[:, :, sft:10], rkf[:, k, :], hs[:, :, 0:10 - sft], False, True, [rkf, hs], [PXS])
                            hsn = HS[(k + 1) % 2]
                            kb.op("dve", lambda e: e.tensor_copy(out=hsn[:], in_=PXS[:]), [PXS], [hsn])
                            hs = hsn
                        yield
                    kb.op("dve", lambda e: e.tensor_copy(out=FINS[:, 0, g:g + 1], in_=hcur[:, 256:257]), [hcur], [FINS])
                    kb.op("dve", lambda e: e.tensor_copy(out=FINS[:, 1:3, g], in_=hs[:, :, 8]), [hs], [FINS])
                    kb.op("dve", lambda e: e.tensor_copy(out=HSB[:], in_=hs[:]), [hs], [HSB])
                    kb.mm(PY8[:, 0:256], T0[:, g, :], u8[:, 0:256], True, False, [T0, u8], [PY8])
                    kb.mm(PY8[:, 0:256], VVB[:, g, :], hcur[:, 0:256], False, True, [VVB, hcur], [PY8])
                    py_s = PY8[:, 256:272].rearrange("p (a b) -> p a b", a=2)
                    kb.mm(py_s, T0[:, g, :], u8[:, 256:272].rearrange("p (a b) -> p a b", a=2), True, False,
                          [T0, u8], [PY8])
                    kb.mm(py_s, VVB[:, g, :], HSB[:, :, 0:8], False, True, [VVB, HSB], [PY8])
                    yield
                    kb.op("act", lambda e: e.activation(out=GT1[:], in_=PY8[:], func=AF.Square), [PY8], [GT1])
                    TS(GT1[:], GT1[:], 0.044715, 1.0, ALU.mult, ALU.add, [GT1], [GT1])
                    TT(GT1[:], GT1[:], PY8[:], ALU.mult, [GT1, PY8], [GT1])
                    yield
                    kb.op("act", lambda e: e.activation(out=GT2[:], in_=GT1[:], func=AF.Sigmoid, scale=1.5957691216),
                          [GT1], [GT2])
                    TT(YGALL[:, g, :], GT2[:], PY8[:], ALU.mult, [GT2, PY8], [YGALL])
                    yield

                for g0 in range(0, 32, NL):
                    interleave([body2(g0 + ln, ln) for ln in range(NL)])

                PFN = PX[0]
                kb.mm(PFN[:, 0:96], SWP[:], FINS[:].rearrange("p a b -> p (a b)"), True, True, [SWP, FINS], [PFN])
                FT = kb.sb("FT", [128, 3, 32])
                FO = kb.sb("FO", [128, 128])
                b3 = lambda t: t[:, None, :].to_broadcast([128, 3, 32])
                TT(FT[:], PFN[:, 0:96].rearrange("p (a b) -> p a b", a=3), b3(CIs), ALU.mult, [PFN, CIs], [FT])
                TT(FINS[:], FINS[:], b3(CRc), ALU.mult, [FINS, CRc], [FINS])
                TT(FINS[:], FINS[:], FT[:], ALU.subtract, [FINS, FT], [FINS])
                PTF = PX[1]
                kb.tr(PTF[0:96, 0:128], FINS[:].rearrange("p a b -> p (a b)"), IDf[:], [FINS, IDf], [PTF])
                kb.op("dve", lambda e: e.tensor_copy(out=FO[0:96, :], in_=PTF[0:96, 0:128]), [PTF], [FO])
                kb.dma("sp", o_fin[:, :], FO[0:96, :], reads=[FO])

                kb.dma("pool", ESEL[:, 0:32, :], sp_eselT[:, 0:32, :], writes=[ESEL])
                kb.dma("pool", ESEL[:, 32:64, :], sp_eselT[:, 32:64, :], writes=[ESEL])
                YST = UTO
                for ct in range(4):
                    yv = YST[:, ct, :].rearrange("p (n s) -> p s n", s=8)
                    for t8 in range(8):
                        for gp in range(8):
                            kb.mm(PY8[:], ESEL[:, gp * 8 + t8, :], YGALL[:, ct * 8 + gp, :], gp == 0, gp == 7,
                                  [ESEL, YGALL], [PY8])
                        evac(yv[:, t8, :], YST, PY8[:], PY8)
                PZ = [PX[0], PX[1]]
                for t0 in range(0, NOWN, 512):
                    nq = min(512, NOWN - t0)
                    for ft in range(4):
                        for half, pz in ((0, PZ[0]), (1, PZ[1])):
                            c0 = half * 512 + ft * 128
                            for ct in range(4):
                                kb.mm(pz[:, 0:nq], WG[:, ct, c0:c0 + 128], YST[:, ct, t0:t0 + nq], ct == 0, ct == 3,
                                      [WG, YST], [pz])
                        kb.op("act", lambda e: e.activation(out=GT1B[:, 0:nq], in_=PZ[1][:, 0:nq], func=AF.Sigmoid),
                              [PZ[1]], [GT1B])
                        TT(YS2[:, ft, t0:t0 + nq], PZ[0][:, 0:nq], GT1B[:, 0:nq], ALU.mult, [PZ[0], GT1B], [YS2])

        YD = [Buf(None) for i in range(NT)]
        with kb.scope([[O_FREE2, ARENA_BYTES], [O_UTF, O_YS2]]):
            WGh = kb.sb("WGh", [128, 8, 3, 512], BF16)
            WBFh = kb.sb("WBFh", [64, 8, 512], BF16)
            WBSh = kb.sb("WBSh", [128, 4, 512], BF16)
            WBMh = kb.sb("WBMh", [128, 4, 512], BF16)
            WOh = kb.sb("WOh", [128, 4, D], BF16)
            X = [kb.sb("X", [128, D]) for i in range(2)]
            XA = [kb.sb("XA", [128, D]) for i in range(2)]
            SQ = kb.sb("SQ", [128, D], BF16)
            SS = [kb.sb("SS", [128, 1]) for i in range(2)]
            RS = [kb.sb("RS", [128, 1]) for i in range(2)]
            HN = [kb.sb("HN", [128, D], BF16) for i in range(2)]
            HT = [kb.sb("HT", [128, 8, 128], BF16) for i in range(2)]
            SGT = [[kb.sb("SGT", [128, 512]) for gi in range(3)] for i in range(2)]
            MB_ = [kb.sb("MBm", [128, 512], BF16) for i in range(2)]
            MTt = [kb.sb("MTt", [128, 4, 128], BF16) for i in range(2)]
            for _ in range(3):
                X.append(kb.sb("X", [128, D]))
                XA.append(kb.sb("XA", [128, D]))
            HT.append(kb.sb("HT", [128, 8, 128], BF16))
            PTn = kb.ps("PTn", [128, 8, 128], BF16)
            PTm = kb.ps("PTm", [128, 4, 128], BF16)
            PG = [kb.ps("PG", [128, 512]) for i in range(2)]
            PP = [kb.ps("PP", [128, 512]) for i in range(2)]
            PO = [kb.ps("PO", [128, 512]) for i in range(2)]
            wgv = w_g.rearrange("(k p) (g n) -> p k g n", p=128, g=3)
            ldw_i = [0]
            cnt = [0]
            for hf in range(2):
                fs = slice(hf * 512, (hf + 1) * 512)
                def ldw(dst, dbuf, src, npart=128):
                    stg = XA[ldw_i[0] % 5]
                    eng = ("dve", "pool")[ldw_i[0] % 2]
                    ldw_i[0] += 1
                    shp = list(src.shape)
                    assert shp[1] * shp[2] <= 1024
                    sv = stg[0:npart, 0:shp[1] * shp[2]].rearrange("p (a b) -> p a b", a=shp[1])
                    kb.dma("sp", sv, src, writes=[stg])
                    kb.op(eng, lambda e: e.tensor_copy(out=dst, in_=sv), [stg], [dbuf])

                for k in range(8):
                    ldw(WGh[:, k, 0:2, :], WGh, wgv[:, k, 0:2, fs])
                    ldw(WGh[:, k, 2:3, :], WGh, wgv[:, k, 2:3, fs])
                wbf = w_brf.rearrange("(h d) n -> d h n", d=64)
                for h0 in (0, 2, 4, 6):
                    ldw(WBFh[:, h0:h0 + 2, :], WBFh, wbf[:, h0:h0 + 2, fs], npart=64)
                for c0 in (0, 2):
                    ldw(WBSh[:, c0:c0 + 2, :], WBSh, w_brs.rearrange("(c p) n -> p c n", p=128)[:, c0:c0 + 2, fs])
                    ldw(WBMh[:, c0:c0 + 2, :], WBMh, w_brm.rearrange("(c p) n -> p c n", p=128)[:, c0:c0 + 2, fs])
                wov = w_out[hf * 512:(hf + 1) * 512, :].rearrange("(c p) n -> p c n", p=128)
                for c0 in range(4):
                    ldw(WOh[:, c0:c0 + 1, :], WOh, wov[:, c0:c0 + 1, :])

                def f0(i, hf=hf):
                    tok = slice(i * 128, (i + 1) * 128)
                    kb.dma("sp", X[i % 5][:], x_own[tok, :], writes=[X[i % 5]])
                    if hf == 1:
                        kb.dma("sp", XA[i % 5][:], o_y[tok, :], reads=[YD[i]], writes=[XA[i % 5]])

                def f1a(i, hf=hf):
                    norm_a(X[i % 5], G, SS[i % 2], RS[i % 2], HN[i % 2], SQ)

                def f1b(i, hf=hf):
                    norm_b(HN[i % 2], PTn, (HT[i % 3][:], HT[i % 3]))

                def f2(i):
                    tok = slice(i * 128, (i + 1) * 128)
                    ht, sg = HT[i % 3], SGT[i % 2]
                    for gi in range(3):
                        pg, pp = PG[cnt[0] % 2], PP[cnt[0] % 2]
                        cnt[0] += 1
                        for k in range(8):
                            kb.mm(pg[:], ht[:, k, :], WGh[:, k, gi, :], k == 0, k == 7, [ht, WGh], [pg])
                        if gi == 0:
                            for h in range(8):
                                kb.mm(pp[:], OFT[0:64, h, tok], WBFh[0:64, h, :], h == 0, h == 7, [OFT, WBFh], [pp])
                        elif gi == 1:
                            for c in range(4):
                                kb.mm(pp[:], YS2[:, c, tok], WBSh[:, c, :], c == 0, c == 3, [YS2, WBSh], [pp])
                        else:
                            for c in range(4):
                                kb.mm(pp[:], OMT[:, c, tok], WBMh[:, c, :], c == 0, c == 3, [OMT, WBMh], [pp])
                        kb.op("act", lambda e: e.activation(out=sg[gi][:], in_=pg[:], func=AF.Sigmoid),
                              [pg], [sg[gi]])
                        kb.op("dve", lambda e: e.tensor_tensor(out=sg[gi][:], in0=sg[gi][:], in1=pp[:],
                                                               op=ALU.mult), [sg[gi], pp], [sg[gi]])

                def f3(i, hf=hf):
                    tok = slice(i * 128, (i + 1) * 128)
                    sg, mb_, mt = SGT[i % 2], MB_[i % 2], MTt[i % 2]
                    xa = X[i % 5] if hf == 0 else XA[i % 5]
                    kb.op("pool", lambda e: e.tensor_tensor(out=sg[0][:], in0=sg[0][:], in1=sg[1][:], op=ALU.add),
                          [sg[0], sg[1]], [sg[0]])
                    kb.op("pool", lambda e: e.tensor_tensor(out=mb_[:], in0=sg[0][:], in1=sg[2][:], op=ALU.add),
                          [sg[0], sg[2]], [mb_])
                    for c in range(4):
                        kb.tr(PTm[:, c, :], mb_[:, c * 128:(c + 1) * 128], IDb[:], [mb_, IDb], [PTm])
                    kb.op("act", lambda e: e.copy(out=mt[:], in_=PTm[:]), [PTm], [mt])
                    for half in range(2):
                        hs = slice(half * 512, (half + 1) * 512)
                        for c in range(4):
                            kb.mm(PO[half][:], mt[:, c, :], WOh[:, c, hs], c == 0, c == 3, [mt, WOh], [PO[half]])
                        kb.op("dve", lambda e: e.tensor_tensor(out=xa[:, hs], in0=xa[:, hs], in1=PO[half][:],
                                                               op=ALU.add), [xa, PO[half]], [xa])
                    kb.dma("sp", o_y[tok, :], xa[:], reads=[xa], writes=[YD[i]])

                f0(0)
                f0(1)
                for t in range(NT + 2):
                    if t + 2 < NT:
                        f0(t + 2)
                    if t < NT:
                        f1a(t)
                    if 0 <= t - 1 < NT:
                        f2(t - 1)
                    if t < NT:
                        f1b(t)
                    if 0 <= t - 2 < NT:
                        f3(t - 2)

        if STOP == "F":
            kb.finish()
            return nc
        with kb.scope([[O_OFT, ARENA_BYTES]]):
            H2T = kb.sb("H2T", [128, 8, NOWN], BF16)
            ACC = kb.sb("ACC", [128, NT, D])
            COMBT = kb.sb("COMBT", [32, NOWN], BF16)
            WEX = [(kb.sb("WEG", [128, 8, 2, 256], BF16), kb.sb("WEU", [128, 8, 2, 256], BF16),
                    kb.sb("WED", [128, 2, 2, D], BF16)) for i in range(2)]

            def load_chunk(ch):
                wg_, wu_, wd_ = WEX[ch % 2]
                for el in range(2):
                    e_ = ch * 2 + el
                    kb.dma("pool", wg_[:, :, el, :], moe_wg[e_].rearrange("(k p) f -> p k f", p=128), writes=[wg_])
                    kb.dma("pool", wu_[:, :, el, :], moe_wu[e_].rearrange("(k p) f -> p k f", p=128), writes=[wu_])
                    kb.dma("pool", wd_[:, :, el, :], moe_wd[e_].rearrange("(t p) d -> p t d", p=128), writes=[wd_])

            if STOP != "G1":
                load_chunk(0)
                load_chunk(1)
            else:
                kb.op("dve", lambda e: e.memset(COMBT[:], 0.0), [], [COMBT])
            with kb.scope([[kb.regions[0][0], ARENA_BYTES]]):
                GF = kb.sb("GF", [128, D])
                WR = kb.sb("WR", [128, 8, 36])
                kb.dma("sp", GF[:], gffn, writes=[GF])
                kb.dma("sp", WR[:], w_r.rearrange("(k p) n -> p k n", p=128), writes=[WR])
                X = [kb.sb("X", [128, D]) for i in range(2)]
                SQ = kb.sb("SQ", [128, D], BF16)
                SS = [kb.sb("SS", [128, 1]) for i in range(2)]
                RS = [kb.sb("RS", [128, 1]) for i in range(2)]
                H2F = [kb.sb("H2F", [128, D]) for i in range(2)]
                H2Tf = [kb.sb("H2Tf", [128, 8, 128]) for i in range(2)]
                LG = kb.sb("LG", [128, 36])
                GOH = kb.sb("GOH", [128, 4])
                GEX = kb.sb("GEX", [128, 4])
                SM = kb.sb("SM", [128, 16])
                ESL = kb.sb("ESL", [128, 8])
                ES4 = kb.sb("ES4", [128, 4, 8])
                MX8 = kb.sb("MX8", [128, 8])
                OH1 = kb.sb("OH1", [128, 8])
                OH2 = kb.sb("OH2", [128, 8])
                COMB = kb.sb("COMB", [128, 4, 8])
                PTFb = [kb.ps("PTF", [128, 512]) for i in range(2)]
                PTF = [_View(p_, p_[:, 0:128]) for p_ in PTFb]
                PLb = kb.ps("PL", [128, 512])
                PL = _View(PLb, PLb[:, 0:36])
                PCTb = kb.ps("PCT", [128, 512])
                PCT = _View(PCTb, PCTb[0:32, 0:128])
                for i in range(NT):
                    b = i % 2
                    tok = slice(i * 128, (i + 1) * 128)
                    x, ss, rs, h2f, h2t = X[b], SS[b], RS[b], H2F[b], H2Tf[b]
                    kb.dma("sp", x[:], o_y[tok, :], reads=[YD[i]], writes=[x])
                    kb.op("act", lambda e: e.activation(out=SQ[:], in_=x[:], func=AF.Square, accum_out=ss[:]),
                          [x], [SQ, ss])
                    kb.op("act", lambda e: e.activation(out=rs[:], in_=ss[:], func=AF.Ln, scale=1.0 / D, bias=EPS),
                          [ss], [rs])
                    kb.op("act", lambda e: e.activation(out=rs[:], in_=rs[:], func=AF.Exp, scale=-0.5), [rs], [rs])
                    kb.op("dve", lambda e: e.scalar_tensor_tensor(out=h2f[:], in0=x[:], scalar=rs[:, 0:1], in1=GF[:],
                                                                  op0=ALU.mult, op1=ALU.mult), [x, rs, GF], [h2f])
                    if 9 < 0: continue
                    for k in range(8):
                        ptf = PTF[k % 2]
                        kb.tr(ptf[:], h2f[:, k * 128:(k + 1) * 128], IDf[:], [h2f, IDf], [ptf])
                        kb.op("act", lambda e: e.copy(out=h2t[:, k, :], in_=ptf[:]), [ptf], [h2t])
                        kb.op("dve", lambda e: e.tensor_copy(out=H2T[:, k, tok], in_=ptf[:]), [ptf], [H2T])
                    if 9 < 1: continue
                    for k in range(8):
                        kb.mm(PL[:], h2t[:, k, :], WR[:, k, :], k == 0, k == 7, [h2t, WR], [PL])
                    V = lambda fn, rd, wr: kb.op("dve", fn, rd, wr)
                    V(lambda e: e.tensor_copy(out=LG[:], in_=PL[:]), [PL], [LG])
                    if 9 < 2: continue
                    V(lambda e: e.tensor_reduce(out=SM[:, 0:1], in_=LG[:, 0:4], axis=AX.X, op=ALU.max), [LG], [SM])
                    V(lambda e: e.tensor_scalar(out=GOH[:], in0=LG[:, 0:4], scalar1=SM[:, 0:1], scalar2=None,
                                                op0=ALU.is_equal), [LG, SM], [GOH])
                    V(lambda e: e.tensor_scalar(out=SM[:, 1:2], in0=SM[:, 0:1], scalar1=-1.0, scalar2=None,
                                                op0=ALU.mult), [SM], [SM])
                    kb.op("act", lambda e: e.activation(out=GEX[:], in_=LG[:, 0:4], func=AF.Exp, bias=SM[:, 1:2],
                                                        accum_out=SM[:, 2:3]), [LG, SM], [GEX, SM])
                    V(lambda e: e.reciprocal(out=SM[:, 3:4], in_=SM[:, 2:3]), [SM], [SM])
                    V(lambda e: e.tensor_tensor(out=ES4[:], in0=LG[:, 4:36].rearrange("p (g e) -> p g e", g=4),
                                                in1=GOH[:, :, None].to_broadcast([128, 4, 8]), op=ALU.mult),
                      [LG, GOH], [ES4])
                    V(lambda e: e.tensor_reduce(out=ESL[:], in_=ES4[:].rearrange("p g e -> p e g"), axis=AX.X,
                                                op=ALU.add), [ES4], [ESL])
                    V(lambda e: e.max(out=MX8[:], in_=ESL[:]), [ESL], [MX8])
                    V(lambda e: e.tensor_scalar(out=OH1[:], in0=ESL[:], scalar1=MX8[:, 0:1], scalar2=None,
                                                op0=ALU.is_equal), [ESL, MX8], [OH1])
                    V(lambda e: e.tensor_scalar(out=OH2[:], in0=ESL[:], scalar1=MX8[:, 1:2], scalar2=None,
                                                op0=ALU.is_equal), [ESL, MX8], [OH2])
                    V(lambda e: e.tensor_tensor(out=SM[:, 4:5], in0=MX8[:, 1:2], in1=MX8[:, 0:1], op=ALU.subtract),
                      [MX8], [SM])
                    kb.op("act", lambda e: e.activation(out=SM[:, 5:6], in_=SM[:, 4:5], func=AF.Exp), [SM], [SM])
                    V(lambda e: e.tensor_scalar(out=SM[:, 6:7], in0=SM[:, 5:6], scalar1=1.0, scalar2=None,
                                                op0=ALU.add), [SM], [SM])
                    V(lambda e: e.reciprocal(out=SM[:, 6:7], in_=SM[:, 6:7]), [SM], [SM])
                    V(lambda e: e.tensor_tensor(out=SM[:, 7:8], in0=SM[:, 5:6], in1=SM[:, 6:7], op=ALU.mult),
                      [SM], [SM])
                    V(lambda e: e.tensor_tensor(out=SM[:, 8:9], in0=SM[:, 6:7], in1=SM[:, 3:4], op=ALU.mult),
                      [SM], [SM])
                    V(lambda e: e.tensor_tensor(out=SM[:, 9:10], in0=SM[:, 7:8], in1=SM[:, 3:4], op=ALU.mult),
                      [SM], [SM])
                    V(lambda e: e.tensor_scalar(out=OH1[:], in0=OH1[:], scalar1=SM[:, 8:9], scalar2=None,
                                                op0=ALU.mult), [OH1, SM], [OH1])
                    V(lambda e: e.scalar_tensor_tensor(out=OH1[:], in0=OH2[:], scalar=SM[:, 9:10], in1=OH1[:],
                                                       op0=ALU.mult, op1=ALU.add), [OH2, SM, OH1], [OH1])
                    V(lambda e: e.tensor_tensor(out=COMB[:], in0=GOH[:, :, None].to_broadcast([128, 4, 8]),
                                                in1=OH1[:, None, :].to_broadcast([128, 4, 8]), op=ALU.mult),
                      [GOH, OH1], [COMB])
                    if 9 < 3: continue
                    kb.tr(PCT[:], COMB[:].rearrange("p g e -> p (g e)"), IDf[:], [COMB, IDf], [PCT])
                    V(lambda e: e.tensor_copy(out=COMBT[:, tok], in_=PCT[:]), [PCT], [COMBT])

            with kb.scope([[kb.regions[0][0], ARENA_BYTES]]):
                ACTT = kb.sb("ACTT", [128, 2, 2, NOWN], BF16)
                SE = [kb.sb("SE", [32, 128], BF16) for i in range(2)]
                SEF = [kb.sb("SEF", [32, 128]) for i in range(2)]
                CBC = [kb.sb("CBC", [128, 512]) for i in range(2)]
                SLb = [kb.sb("SLb", [128, 512], BF16) for i in range(2)]
                UTb = [kb.sb("UTb", [128, 512], BF16) for i in range(2)]
                PGa = [kb.ps("PGa", [128, 512]) for i in range(2)]
                PUp = [kb.ps("PUp", [128, 512]) for i in range(2)]
                PCB = kb.ps("PCB", [128, 512])
                PD = [kb.ps("PD", [128, 512]) for i in range(2)]
                pieces = [(t0, min(512, NOWN - t0)) for t0 in range(0, NOWN, 512)]
                cnt = 0
                for ch in range(16):
                    wg_, wu_, wd_ = WEX[ch % 2]
                    for el in range(2):
                        e_ = ch * 2 + el
                        se = SE[e_ % 2]
                        sef = SEF[e_ % 2]
                        kb.dma("sp", sef[:], sele[e_], writes=[sef])
                        kb.op("dve", lambda e: e.tensor_copy(out=se[:], in_=sef[:]), [sef], [se])
                        for (t0, nq) in pieces:
                            cbc = CBC[cnt % 2]
                            kb.mm(PCB[:, 0:nq], se[:], COMBT[:, t0:t0 + nq], True, True, [se, COMBT], [PCB])
                            kb.op("act", lambda e: e.copy(out=cbc[:, 0:nq], in_=PCB[:, 0:nq]), [PCB], [cbc])
                            for ft in range(2):
                                pga, pup = PGa[cnt % 2], PUp[cnt % 2]
                                slb, utb = SLb[cnt % 2], UTb[cnt % 2]
                                cnt += 1
                                fsl = slice(ft * 128, (ft + 1) * 128)
                                for k in range(8):
                                    kb.mm(pga[:, 0:nq], wg_[:, k, el, fsl], H2T[:, k, t0:t0 + nq], k == 0, k == 7,
                                          [wg_, H2T], [pga])
                                for k in range(8):
                                    kb.mm(pup[:, 0:nq], wu_[:, k, el, fsl], H2T[:, k, t0:t0 + nq], k == 0, k == 7,
                                          [wu_, H2T], [pup])
                                kb.op("act", lambda e: e.activation(out=slb[:, 0:nq], in_=pga[:, 0:nq], func=AF.Silu),
                                      [pga], [slb])
                                kb.op("dve", lambda e: e.tensor_tensor(out=utb[:, 0:nq], in0=pup[:, 0:nq],
                                                                       in1=cbc[:, 0:nq], op=ALU.mult),
                                      [pup, cbc], [utb])
                                kb.op("pool", lambda e: e.tensor_tensor(out=ACTT[:, el, ft, t0:t0 + nq],
                                                                        in0=slb[:, 0:nq], in1=utb[:, 0:nq],
                                                                        op=ALU.mult), [slb, utb], [ACTT])
                    for i in range(NT):
                        tok = slice(i * 128, (i + 1) * 128)
                        for half in range(2):
                            hs = slice(half * 512, (half + 1) * 512)
                            pd = PD[(i * 2 + half) % 2]
                            n = 0
                            for el in range(2):
                                for ft in range(2):
                                    kb.mm(pd[:], ACTT[:, el, ft, tok], wd_[:, ft, el, hs], n == 0, n == 3,
                                          [ACTT, wd_], [pd])
                                    n += 1
                            if ch == 0:
                                kb.op("dve", lambda e: e.tensor_copy(out=ACC[:, i, hs], in_=pd[:]), [pd], [ACC])
                            else:
                                kb.op("dve", lambda e: e.tensor_tensor(out=ACC[:, i, hs], in0=ACC[:, i, hs],
                                                                       in1=pd[:], op=ALU.add), [ACC, pd], [ACC])
                    if ch + 2 < 16:
                        load_chunk(ch + 2)
            with kb.scope([[kb.regions[0][0], ARENA_BYTES]]):
                XF = [kb.sb("XF", [128, D]) for i in range(2)]
                for i in range(NT):
                    tok = slice(i * 128, (i + 1) * 128)
                    xf = XF[i % 2]
                    kb.dma("sp", xf[:], o_y[tok, :], reads=[YD[i]], writes=[xf])
                    kb.op("dve", lambda e: e.tensor_tensor(out=xf[:], in0=xf[:], in1=ACC[:, i, :], op=ALU.add),
                          [xf, ACC], [xf])
                    kb.dma("sp", o_y[tok, :], xf[:], reads=[xf], writes=[YD[i]])
        kb.finish()
    return nc


def _stair():
    m = np.zeros((4, 128, 512), np.float32)
    s = np.arange(128)[:, None]
    t = np.arange(128)[None, :]
    tri = (s <= t).astype(np.float32)
    for d in range(4):
        for qi in range(4):
            blk = 0.0 if qi < d else (tri if qi == d else 1.0)
            m[d, :, qi * 128:(qi + 1) * 128] = blk
    return m


def kernel(**inp):
    f = lambda a: np.ascontiguousarray(np.asarray(a, dtype=np.float32))
    x_prompt = f(inp["x_prompt"])
    x_sample = f(inp["x_sample"])
    w_in = f(inp["w_in"])[0]
    cache_k = f(inp["cache_fox_k"])[0].reshape(16, SEQ, 512)
    cache_v = f(inp["cache_fox_v"])[0].reshape(16, SEQ, 512)
    cache_lf = f(inp["cache_fox_logf"])[0]
    rep = lambda v, n=128: np.ascontiguousarray(np.broadcast_to(v[None, :], (n, v.shape[0])))
    w_a = np.ascontiguousarray(np.concatenate([w_in[:, 512:1536], w_in[:, 1536:1544], w_in[:, 2056:2568]], axis=1))
    w_b = np.ascontiguousarray(np.concatenate([w_in[:, 0:512], w_in[:, 1544:2056]], axis=1))
    s_ = np.arange(128)[:, None]
    t_ = np.arange(128)[None, :]
    stair = _stair()
    common = dict(
        gmix=rep(f(inp["norm_mix"])[0]), w_a=w_a, w_b=w_b,
        kn_rep=rep(np.tile(f(inp["kn_fox"])[0], 8)), qn_rep=rep(np.tile(f(inp["qn_fox"])[0], 8)),
        qnm_rep=rep(np.tile(f(inp["qn_mem"])[0], 4)), bf_rep=rep(f(inp["b_forget"])[0]),
        ident=np.eye(128, dtype=np.float32), triu=(s_ <= t_).astype(np.float32),
        gmem=rep(f(inp["norm_mem"])[0]), w_mkv=f(inp["w_mem_kv"])[0], knm_rep=rep(np.tile(f(inp["kn_mem"])[0], 4)))
    mem_prompt = f(inp["mem_prompt"])
    dup = lambda m_: np.ascontiguousarray(np.concatenate([m_, m_], axis=0))
    a_re, a_im = f(inp["ssm_a_re"])[0], f(inp["ssm_a_im"])[0]
    exps = [7 - i for i in range(8)] + [-i for i in range(8)] + [i + 1 for i in range(8)] + list(range(8)) \
        + [8 * (1 << k) for k in range(9)]
    pidx = np.arange(128)
    esel = np.zeros((128, 8, 8, 128), np.float32)
    for gp in range(8):
        for s8 in range(8):
            for hh in range(16):
                esel[16 * gp + hh, gp, s8, s8 * 16 + hh] = 1.0
    eselT = np.ascontiguousarray(np.transpose(esel, (3, 1, 2, 0)))
    swap = np.zeros((128, 128), np.float32)
    swap[(pidx + 64) % 128, pidx] = 1.0
    common.update(
        sp_are=dup(a_re.T), sp_aim=dup(a_im.T), sp_ldt=rep(f(inp["ssm_log_dt"])[0]),
        sp_bre=dup(np.transpose(f(inp["ssm_b_re"])[0], (1, 0, 2))), sp_bim=dup(np.transpose(f(inp["ssm_b_im"])[0], (1, 0, 2))),
        sp_cre=dup(np.transpose(f(inp["ssm_c_re"])[0], (2, 0, 1))), sp_cim=dup(np.transpose(f(inp["ssm_c_im"])[0], (2, 0, 1))),
        sp_dcol=np.ascontiguousarray(np.tile(f(inp["ssm_d"])[0].reshape(32, 16).T, (8, 1))),
        sp_et=rep(np.array(exps, np.float32)),
        sp_caus=((pidx[:, None] // 16) <= (pidx[None, :] // 16)).astype(np.float32),
        sp_esel=esel.reshape(128, 64, 128), sp_eselT=eselT.reshape(128, 64, 128), sp_swap=swap,
        sp_sg=np.concatenate([np.ones((64, 1), np.float32), -np.ones((64, 1), np.float32)]),
        w_glu=f(inp["w_glu"])[0],
        w_g=np.ascontiguousarray(w_in[:, 2568:5640]), w_brf=f(inp["w_br_fox"])[0], w_brs=f(inp["w_br_ssm"])[0],
        w_brm=f(inp["w_br_mem"])[0], w_out=f(inp["w_out"])[0], gffn=rep(f(inp["norm_ffn"])[0]),
        w_r=np.ascontiguousarray(np.concatenate([f(inp["w_router_group"])[0], f(inp["w_router_expert"])[0]], axis=1)),
        moe_wg=f(inp["moe_w_gate"])[0], moe_wu=f(inp["moe_w_up"])[0], moe_wd=f(inp["moe_w_down"])[0],
        sele=np.ascontiguousarray(np.broadcast_to(np.eye(32, dtype=np.float32)[:, :, None], (32, 32, 128))))
    st_re, st_im = f(inp["state_ssm_re"])[0], f(inp["state_ssm_im"])[0]
    cache_mk = f(inp["cache_mem_k"])[0].reshape(16, 256, 512)
    cache_mv = f(inp["cache_mem_v"])[0].reshape(16, 256, 512)
    in_maps = []
    for c in range(NCORES):
        b, r = c // 2, c % 2
        m = dict(common)
        m["x_all"] = x_prompt[b]
        m["x_own"] = np.ascontiguousarray(np.concatenate(
            [x_prompt[b, r * HALF:(r + 1) * HALF], x_sample[2 * c], x_sample[2 * c + 1]], axis=0))
        m["mask_a"] = np.ascontiguousarray(np.transpose(stair if r == 0 else np.ones_like(stair), (1, 0, 2)))
        m["mask_b"] = np.ascontiguousarray(np.transpose(stair, (1, 0, 2)))
        addm = np.zeros((4, 36), np.float32)
        if r == 0:
            for J in range(4):
                addm[J, 4 * J + 4:] = NEG
        m["addm"] = np.ascontiguousarray(np.broadcast_to(addm[None], (128, 4, 36)))
        m["rflag"] = np.full((128, 1), float(r), np.float32)
        m["c_k"] = np.ascontiguousarray(cache_k[2 * c:2 * c + 2])
        m["c_v"] = np.ascontiguousarray(cache_v[2 * c:2 * c + 2])
        m["c_lf"] = np.ascontiguousarray(cache_lf[2 * c:2 * c + 2])
        m["mem_in"] = mem_prompt[b]
        hre = np.transpose(st_re[2 * c:2 * c + 2], (2, 0, 1))
        him = np.transpose(st_im[2 * c:2 * c + 2], (2, 0, 1))
        m["sp_h0"] = np.ascontiguousarray(np.concatenate([hre, him], axis=0))
        m["sp_h0s"] = np.ascontiguousarray(np.concatenate([him, hre], axis=0))
        m["c_mk"] = np.ascontiguousarray(cache_mk[2 * c:2 * c + 2])
        m["c_mv"] = np.ascontiguousarray(cache_mv[2 * c:2 * c + 2])
        in_maps.append(m)
    nc = build()
    res = run_bass_kernel_spmd(nc, in_maps, core_ids=list(range(NCORES)))
    r = res.results
    kernel.last = r
    pk = np.stack([r[2 * b]["o_k"].reshape(SEQ, 8, 64) for b in range(4)])[None]
    pv = np.stack([r[2 * b]["o_v"].reshape(SEQ, 8, 64) for b in range(4)])[None]
    plf = np.stack([r[2 * b]["o_lf"] for b in range(4)])[None]
    sk = np.concatenate([r[c]["o_sk"].reshape(2, 64, 8, 64) for c in range(8)])[None]
    sv = np.concatenate([r[c]["o_sv"].reshape(2, 64, 8, 64) for c in range(8)])[None]
    slf = np.concatenate([r[c]["o_slf"].reshape(2, 64, 8) for c in range(8)])[None]
    mk = np.stack([r[2 * b]["o_mk"].reshape(256, 4, 128) for b in range(4)])[None]
    mv = np.stack([r[2 * b]["o_mv"].reshape(256, 4, 128) for b in range(4)])[None]
    z = lambda *s: np.zeros(s, np.float32)
    fin = [r[c]["o_fin"].reshape(3, 32, 128) for c in range(8)]
    p_re = np.stack([fin[2 * b + 1][0, :, 0:64] for b in range(4)])[None]
    p_im = np.stack([fin[2 * b + 1][0, :, 64:128] for b in range(4)])[None]
    s_re = np.stack([fin[c][1 + j, :, 0:64] for c in range(8) for j in range(2)])[None]
    s_im = np.stack([fin[c][1 + j, :, 64:128] for c in range(8) for j in range(2)])[None]
    yp = np.stack([np.concatenate([r[2 * b]["o_y"][0:HALF], r[2 * b + 1]["o_y"][0:HALF]], axis=0) for b in range(4)])
    ysm = np.concatenate([r[c]["o_y"][HALF:].reshape(2, 64, D) for c in range(8)])
    return (yp, ysm, pk, pv, plf, p_re, p_im, mk, mv, sk, sv, slf, s_re, s_im)
```

```python
import os
import numpy as np
from contextlib import ExitStack, contextmanager
import concourse.bass as bass
import concourse.mybir as mybir
from concourse.bass_utils import run_bass_kernel_spmd

F32 = mybir.dt.float32
BF16 = mybir.dt.bfloat16
I32 = mybir.dt.int32
AF = mybir.ActivationFunctionType
ALU = mybir.AluOpType
AX = mybir.AxisListType

NCORES = 8
D = 1024
SEQ = 4096
HALF = 2048
NSMP = 128
NOWN = HALF + NSMP
NT = NOWN // 128
EPS = 1e-6
NEG = -30000.0
ARENA_BYTES = 212480
STOP = ""
_DSZ = {F32: 4, BF16: 2, I32: 4}


class Buf:
    def __init__(self, t, psum=False):
        self.t = t
        self.w = None
        self.r = []
        self.psum = psum

    def __getitem__(self, idx):
        return self.t[idx]


class KB:
    NDMA = 24

    def __init__(self, nc, es):
        self.nc = nc
        self.es = es
        self.eng = {"pe": nc.tensor, "act": nc.scalar, "dve": nc.vector, "pool": nc.gpsimd, "sp": nc.sync}
        self.sem = {k: es.enter_context(nc.semaphore("s_" + k)) for k in self.eng}
        self.cnt = {k: 0 for k in self.eng}
        self.dsem = [es.enter_context(nc.semaphore("d%d" % i)) for i in range(self.NDMA)]
        self.dcnt = [0] * self.NDMA
        self.dnext = 0
        self.dnext_q = {}
        self.waited = {}
        self.uid = 0
        self.arena = es.enter_context(nc.sbuf_tensor("arena", [128, ARENA_BYTES // 2], BF16))
        self.regions = []

    def at(self, off, shape, dt=F32):
        n = 1
        for s in shape[1:]:
            n *= s
        nb = n * _DSZ[dt]
        assert off % 4 == 0 and off + nb <= ARENA_BYTES, (off, nb)
        v = self.arena[0:shape[0], off // 2:(off + nb) // 2]
        if dt != BF16:
            v = v.bitcast(dt)
        if len(shape) == 3:
            v = v.rearrange("p (a b) -> p a b", a=shape[1])
        elif len(shape) == 4:
            v = v.rearrange("p (a b c) -> p a b c", a=shape[1], b=shape[2])
        return Buf(v)

    def sb(self, name, shape, dt=F32):
        n = 1
        for s in shape[1:]:
            n *= s
        nb = (n * _DSZ[dt] + 63) // 64 * 64
        for reg in self.regions:
            if reg[0] + nb <= reg[1]:
                off = reg[0]
                reg[0] += nb
                return self.at(off, shape, dt)
        raise AssertionError("arena regions exhausted for %s %s (%d B): %s" % (name, shape, nb, self.regions))

    def ps(self, name, shape, dt=F32):
        self.uid += 1
        return Buf(self.es.enter_context(self.nc.psum_tensor("%s_%d" % (name, self.uid), list(shape), dt)), psum=True)

    @contextmanager
    def scope(self, regions):
        old, oldr = self.es, self.regions
        with ExitStack() as es2:
            self.es = es2
            self.regions = [[a, b] for a, b in regions]
            yield
            self.barrier()
        self.es, self.regions = old, oldr

    def _semof(self, key):
        return self.sem[key] if isinstance(key, str) else self.dsem[key]

    def _wait(self, eng, ev):
        key, val = ev
        if self.waited.get((eng, key), 0) >= val:
            return
        self.eng[eng].wait_ge(self._semof(key), val)
        self.waited[(eng, key)] = val

    def barrier(self):
        for e in self.eng:
            for k in self.eng:
                if k != e and self.cnt[k]:
                    self._wait(e, (k, self.cnt[k]))
            for s in range(self.NDMA):
                if self.dcnt[s]:
                    self._wait(e, (s, self.dcnt[s]))

    def _deps(self, eng, reads, writes):
        deps = []
        for b in reads:
            if b.w is not None:
                deps.append(b.w)
            if b.psum:
                deps.extend(ev for ev in b.r if ev[0] != eng)
        for b in writes:
            if b.w is not None:
                deps.append(b.w)
            deps.extend(b.r)
        for ev in deps:
            if ev[0] == eng and eng == "pe":
                continue
            self._wait(eng, ev)

    def _mark(self, ev, reads, writes):
        for b in reads:
            b.r.append(ev)
            if len(b.r) > 48:
                last = {}
                for k, v in b.r:
                    last[k] = max(last.get(k, 0), v)
                b.r = list(last.items())
        for b in writes:
            b.w = ev
            b.r = []

    def op(self, eng, fn, reads=(), writes=()):
        self._deps(eng, reads, writes)
        inst = fn(self.eng[eng])
        self.cnt[eng] += 1
        inst.then_inc(self.sem[eng], 1)
        self._mark((eng, self.cnt[eng]), reads, writes)

    def mm(self, out, lhsT, rhs, start, stop, reads, writes, **kw):
        self.op("pe", lambda e: e.matmul(out, lhsT, rhs, start=start, stop=stop, **kw), reads, writes)

    def tr(self, out, in_, ident, reads, writes):
        self.op("pe", lambda e: e.transpose(out=out, in_=in_, identity=ident), reads, writes)

    def dma(self, q, out, in_, reads=(), writes=(), **kw):
        lo, n = (0, 16) if q == "sp" else (16, self.NDMA - 16)
        cur = self.dnext_q.get(q, 0)
        slot = lo + cur
        self.dnext_q[q] = (cur + 1) % n
        if self.dcnt[slot] > 0:
            self._wait(q, (slot, self.dcnt[slot]))
        self._deps(q, reads, writes)
        inst = self.eng[q].dma_start(out=out, in_=in_, **kw)
        self.dcnt[slot] += 16
        inst.then_inc(self.dsem[slot], 16)
        self._mark((slot, self.dcnt[slot]), reads, writes)

    def finish(self):
        for slot in range(self.NDMA):
            if self.dcnt[slot]:
                self._wait("sp", (slot, self.dcnt[slot]))


class _View:
    def __init__(self, parent, ap):
        self.__dict__["p"] = parent
        self.__dict__["ap"] = ap

    def __getitem__(self, idx):
        return self.ap if idx == slice(None) else self.ap[idx]

    def __getattr__(self, k):
        return getattr(self.p, k)

    def __setattr__(self, k, v):
        setattr(self.p, k, v)


_SH = 2048
O_CONST = (0, 14336 + _SH)
O_OFT = 14336 + _SH
O_KT = 49152 + _SH
O_VE = 81920 + _SH
O_CK = 115200 + _SH
O_QT = 116224 + _SH
O_QMT = 133632 + _SH
O_UTO = 151040 + _SH
O_HI = 168448 + _SH
O_OMT = 49152 + _SH
O_UTF = 66560 + _SH
O_YS2 = 83968 + _SH
O_FREE2 = 101376 + _SH


def build():
    nc = bass.Bass("TRN2", target_bir_lowering=False)
    din = lambda n, s, dt=F32: nc.dram_tensor(n, list(s), dt, kind="ExternalInput").ap()
    dout = lambda n, s, dt=F32: nc.dram_tensor(n, list(s), dt, kind="ExternalOutput").ap()

    x_all = din("x_all", [SEQ, D])
    x_own = din("x_own", [NOWN, D])
    gmix = din("gmix", [128, D])
    w_a = din("w_a", [D, 1544])
    w_b = din("w_b", [D, 1024])
    kn_rep = din("kn_rep", [128, 512])
    qn_rep = din("qn_rep", [128, 512])
    qnm_rep = din("qnm_rep", [128, 512])
    bf_rep = din("bf_rep", [128, 8])
    ident_in = din("ident", [128, 128])
    triu_in = din("triu", [128, 128])
    ma_in = din("mask_a", [128, 4, 512])
    mb_in = din("mask_b", [128, 4, 512])
    addm_in = din("addm", [128, 4, 36])
    rflag_in = din("rflag", [128, 1])
    c_k = din("c_k", [2, SEQ, 512])
    c_v = din("c_v", [2, SEQ, 512])
    c_lf = din("c_lf", [2, SEQ, 8])
    mem_in = din("mem_in", [256, D])
    gmem = din("gmem", [128, D])
    w_mkv = din("w_mkv", [D, 1024])
    knm_rep = din("knm_rep", [128, 512])
    c_mk = din("c_mk", [2, 256, 512])
    c_mv = din("c_mv", [2, 256, 512])
    sp_are = din("sp_are", [128, 32])
    sp_aim = din("sp_aim", [128, 32])
    sp_ldt = din("sp_ldt", [128, 32])
    sp_bre = din("sp_bre", [128, 32, 16])
    sp_bim = din("sp_bim", [128, 32, 16])
    sp_cre = din("sp_cre", [128, 32, 16])
    sp_cim = din("sp_cim", [128, 32, 16])
    sp_dcol = din("sp_dcol", [128, 32])
    sp_et = din("sp_et", [128, 41])
    sp_caus = din("sp_caus", [128, 128])
    sp_esel = din("sp_esel", [128, 64, 128])
    sp_eselT = din("sp_eselT", [128, 64, 128])
    sp_swap = din("sp_swap", [128, 128])
    sp_sg = din("sp_sg", [128, 1])
    sp_h0 = din("sp_h0", [128, 2, 32])
    sp_h0s = din("sp_h0s", [128, 2, 32])
    w_glu = din("w_glu", [512, 1024])
    w_g = din("w_g", [D, 3072])
    w_brf = din("w_brf", [512, D])
    w_brs = din("w_brs", [512, D])
    w_brm = din("w_brm", [512, D])
    w_out = din("w_out", [D, D])
    gffn = din("gffn", [128, D])
    w_r = din("w_r", [D, 36])
    moe_wg = din("moe_wg", [32, D, 256])
    moe_wu = din("moe_wu", [32, D, 256])
    moe_wd = din("moe_wd", [32, 256, D])
    sele = din("sele", [32, 32, 128])

    o_k = dout("o_k", [SEQ, 512])
    o_v = dout("o_v", [SEQ, 512])
    o_lf = dout("o_lf", [SEQ, 8])
    o_sk = dout("o_sk", [NSMP, 512])
    o_sv = dout("o_sv", [NSMP, 512])
    o_slf = dout("o_slf", [NSMP, 8])
    o_mk = dout("o_mk", [256, 512])
    o_mv = dout("o_mv", [256, 512])
    o_fin = dout("o_fin", [96, 128])
    o_y = dout("o_y", [NOWN, D])

    with ExitStack() as es:
        kb = KB(nc, es)
        kb.regions = [list(O_CONST)]
        G = kb.sb("G", [128, D])
        KN = kb.sb("KN", [128, 512])
        QN = kb.sb("QN", [128, 512])
        QNM = kb.sb("QNM", [128, 512])
        BFr = kb.sb("BFr", [128, 8])
        IDf = kb.sb("IDf", [128, 128])
        IDb = kb.sb("IDb", [128, 128], BF16)
        TRIU = kb.sb("TRIU", [128, 128])
        ONES = kb.sb("ONES", [128, 128])
        RFL = kb.sb("RFL", [128, 1])
        ADDM = kb.sb("ADDM", [128, 4, 36])
        RUN = kb.sb("RUN", [128, 8])
        RUN16 = kb.sb("RUN16", [128, 8])
        RUNO = kb.sb("RUNO", [128, 8])
        RUNOJ = kb.sb("RUNOJ", [128, 4, 8])
        CREF = kb.sb("CREF", [128, 4, 8])
        KTN = kb.sb("KTN", [128, 4, 128], BF16)
        VEN = kb.sb("VEN", [128, 8, 65], BF16)
        LFN = kb.sb("LFN", [128, 8])
        for dst, src in ((G, gmix), (KN, kn_rep), (QN, qn_rep), (QNM, qnm_rep), (BFr, bf_rep), (IDf, ident_in),
                         (TRIU, triu_in), (RFL, rflag_in), (ADDM, addm_in)):
            kb.dma("sp", dst[:], src, writes=[dst])
        kb.op("dve", lambda e: e.tensor_copy(out=IDb[:], in_=IDf[:]), [IDf], [IDb])
        kb.op("dve", lambda e: e.memset(ONES[:], 1.0), [], [ONES])
        kb.op("dve", lambda e: e.memset(RUN[:], 0.0), [], [RUN])
        kb.op("dve", lambda e: e.memset(RUNO[:], 0.0), [], [RUNO])
        kb.op("dve", lambda e: e.memset(VEN[:, :, 64:65], 1.0), [], [VEN])

        OFT = kb.at(O_OFT, [64, 8, NOWN], BF16)
        KT = kb.at(O_KT, [128, 4, SEQ], BF16)
        VE = kb.at(O_VE, [128, 32, 8, 65], BF16)
        CK = kb.at(O_CK, [128, 32, 8])
        QT = kb.at(O_QT, [128, 4, NOWN], BF16)
        QMT = kb.at(O_QMT, [128, 4, NOWN], BF16)
        UTO = kb.at(O_UTO, [128, 4, NOWN], BF16)
        kb.op("dve", lambda e: e.memset(VE[:, :, :, 64:65], 1.0), [], [VE])

        def norm_a(x, gain, ss, rs, hn, sq):
            kb.op("act", lambda e: e.activation(out=sq[:], in_=x[:], func=AF.Square, accum_out=ss[:]), [x], [sq, ss])
            kb.op("act", lambda e: e.activation(out=rs[:], in_=ss[:], func=AF.Ln, scale=1.0 / D, bias=EPS),
                  [ss], [rs])
            kb.op("act", lambda e: e.activation(out=rs[:], in_=rs[:], func=AF.Exp, scale=-0.5), [rs], [rs])
            kb.op("dve", lambda e: e.scalar_tensor_tensor(out=hn[:], in0=x[:], scalar=rs[:, 0:1], in1=gain[:],
                                                          op0=ALU.mult, op1=ALU.mult), [x, rs, gain], [hn])

        def norm_b(hn, pt, ht_dst):
            for k in range(8):
                kb.tr(pt[:, k, :], hn[:, k * 128:(k + 1) * 128], IDb[:], [hn, IDb], [pt])
            kb.op("act", lambda e: e.copy(out=ht_dst[0], in_=pt[:]), [pt], [ht_dst[1]])

        def norm_tile(x, gain, ss, rs, hn, sq, pt, ht_dst):
            norm_a(x, gain, ss, rs, hn, sq)
            norm_b(hn, pt, ht_dst)

        def head_norm(src, nh, dh, gain, scale, sq, ss, dst_f32, dst_bf):
            sap, sbuf = src
            v3 = lambda ap: ap.rearrange("p (h d) -> p h d", d=dh)
            kb.op("act", lambda e: e.activation(out=sq[:], in_=sap, func=AF.Square), [sbuf], [sq])
            kb.op("dve", lambda e: e.tensor_reduce(out=ss[:, 0:nh], in_=v3(sq[:]), axis=AX.X, op=ALU.add), [sq], [ss])
            kb.op("act", lambda e: e.activation(out=ss[:, 0:nh], in_=ss[:, 0:nh], func=AF.Ln, scale=1.0 / dh,
                                                bias=EPS), [ss], [ss])
            kb.op("act", lambda e: e.activation(out=ss[:, 0:nh], in_=ss[:, 0:nh], func=AF.Exp, scale=-0.5),
                  [ss], [ss])
            kb.op("dve", lambda e: e.tensor_tensor(out=v3(sq[:]), in0=v3(sap),
                                                   in1=ss[:, 0:nh, None].to_broadcast([128, nh, dh]), op=ALU.mult),
                  [sbuf, ss], [sq])
            if dst_f32 is not None:
                kb.op("dve", lambda e: e.tensor_tensor(out=dst_f32[:], in0=sq[:], in1=gain[:], op=ALU.mult),
                      [sq, gain], [dst_f32])
            kb.op("dve", lambda e: e.scalar_tensor_tensor(out=dst_bf[:], in0=sq[:], scalar=float(scale), in1=gain[:],
                                                          op0=ALU.mult, op1=ALU.mult), [sq, gain], [dst_bf])

        def logsig(lf, src, sbuf):
            kb.op("dve", lambda e: e.tensor_tensor(out=lf[:], in0=src, in1=BFr[:], op=ALU.add), [sbuf, BFr], [lf])
            kb.op("act", lambda e: e.activation(out=lf[:], in_=lf[:], func=AF.Exp, scale=-1.0), [lf], [lf])
            kb.op("act", lambda e: e.activation(out=lf[:], in_=lf[:], func=AF.Ln, bias=1.0), [lf], [lf])
            kb.op("dve", lambda e: e.tensor_scalar(out=lf[:], in0=lf[:], scalar1=-1.0, scalar2=None, op0=ALU.mult),
                  [lf], [lf])

        def kv_tile_a(kv, ko, kob, ksq, kss, ok_ap, ov_ap, ve_dst):
            kb.dma("sp", ov_ap, kv[:, 512:1024], reads=[kv])
            kb.op("pool", lambda e: e.tensor_copy(out=ve_dst[0],
                                                  in_=kv[:, 512:1024].rearrange("p (h d) -> p h d", d=64)),
                  [kv], [ve_dst[1]])
            head_norm((kv[:, 0:512], kv), 8, 64, KN, 1.0, ksq, kss, ko, kob)
            kb.dma("sp", ok_ap, ko[:], reads=[ko])

        def kv_tile_b(kv, kob, lf, olf_ap, kt_ptk, kt_dst):
            for hp in range(4):
                kb.tr(kt_ptk[:, hp, :], kob[:, hp * 128:(hp + 1) * 128], IDb[:], [kob, IDb], [kt_ptk])
            kb.op("act", lambda e: e.copy(out=kt_dst[0], in_=kt_ptk[:, 0:4, :]), [kt_ptk], [kt_dst[1]])
            logsig(lf, kv[:, 1024:1032], kv)
            kb.dma("sp", olf_ap, lf[:], reads=[lf])

        def kv_tile(kv, ko, kob, ksq, kss, lf, ok_ap, ov_ap, olf_ap, ve_dst, kt_ptk, kt_dst):
            kv_tile_a(kv, ko, kob, ksq, kss, ok_ap, ov_ap, ve_dst)
            kv_tile_b(kv, kob, lf, olf_ap, kt_ptk, kt_dst)

        LOHI = [[O_HI, ARENA_BYTES], [O_OFT, O_KT]]

        with kb.scope(LOHI):
            W = kb.sb("WA", [128, 8, 1032], BF16)
            wv = w_a.rearrange("(k p) n -> p k n", p=128)
            STGW = [kb.sb("STGW", [128, 1032]) for i in range(2)]
            for k in range(8):
                stg = STGW[k % 2]
                kb.dma("sp", stg[:], wv[:, k, 0:1032], writes=[stg])
                kb.op(("dve", "pool")[k % 2], lambda e: e.tensor_copy(out=W[:, k, :], in_=stg[:]), [stg], [W])
            NB = 2
            X = [kb.sb("X", [128, D]) for i in range(NB)]
            SQ = kb.sb("SQ", [128, D], BF16)
            SS = [kb.sb("SS", [128, 1]) for i in range(NB)]
            RS = [kb.sb("RS", [128, 1]) for i in range(NB)]
            HN = [kb.sb("HN", [128, D], BF16) for i in range(NB)]
            HT = [kb.sb("HT", [128, 8, 128], BF16) for i in range(NB)]
            PT = [kb.ps("PT", [128, 8, 128], BF16) for i in range(NB)]
            PKV = kb.ps("PKV", [128, 3, 512])
            PTK = kb.ps("PTK", [128, 4, 128], BF16)
            PC = kb.ps("PC", [128, 8])
            KV = [kb.sb("KV", [128, 1032]) for i in range(NB)]
            KSQ = kb.sb("KSQ", [128, 512])
            KSS = [kb.sb("KSS", [128, 8]) for i in range(NB)]
            KO = [kb.sb("KO", [128, 512]) for i in range(NB)]
            KOB = [kb.sb("KOB", [128, 512], BF16) for i in range(NB)]
            LF = [kb.sb("LF", [128, 8]) for i in range(NB)]
            HT.append(kb.sb("HT", [128, 8, 128], BF16))
            KV.append(kb.sb("KV", [128, 1032]))

            X.append(kb.sb("X", [128, D]))
            X.append(kb.sb("X", [128, D]))

            def a0(i):
                kb.dma("sp", X[i % 4][:], x_all[i * 128:(i + 1) * 128, :], writes=[X[i % 4]])

            def a1a(i):
                b = i % NB
                norm_a(X[i % 4], G, SS[b], RS[b], HN[b], SQ)

            def a1b(i):
                b = i % NB
                norm_b(HN[b], PT[b], (HT[i % 3][:], HT[i % 3]))

            def a2(i):
                ht, kv = HT[i % 3], KV[i % 3]
                for ci, (c0, c1) in enumerate(((0, 512), (512, 1024), (1024, 1032))):
                    for k in range(8):
                        kb.mm(PKV[:, ci, 0:c1 - c0], ht[:, k, :], W[:, k, c0:c1], k == 0, k == 7, [ht, W], [PKV])
                kb.op("dve", lambda e: e.tensor_copy(out=kv[:, 0:1024].rearrange("p (c n) -> p c n", c=2),
                                                     in_=PKV[:, 0:2, :]), [PKV], [kv])
                kb.op("dve", lambda e: e.tensor_copy(out=kv[:, 1024:1032], in_=PKV[:, 2, 0:8]), [PKV], [kv])

            def a3a(i):
                b = i % NB
                rows = slice(i * 128, (i + 1) * 128)
                kv_tile_a(KV[i % 3], KO[b], KOB[b], KSQ, KSS[b], o_k[rows, :], o_v[rows, :], (VE[:, i, :, 0:64], VE))

            def a3b(i):
                b = i % NB
                kv, lf = KV[i % 3], LF[b]
                rows = slice(i * 128, (i + 1) * 128)
                kv_tile_b(kv, KOB[b], lf, o_lf[rows, :], PTK, (KT[:, :, rows], KT))
                kb.mm(PC[:], TRIU[:], lf[:], True, False, [TRIU, lf], [PC])
                kb.mm(PC[:], ONES[:], RUN[:], False, True, [ONES, RUN], [PC])
                kb.op("dve", lambda e: e.tensor_copy(out=CK[:, i, :], in_=PC[:]), [PC], [CK])
                kb.op("dve", lambda e: e.tensor_tensor(out=RUN[:], in0=RUN[:], in1=lf[:], op=ALU.add),
                      [RUN, lf], [RUN])
                if i == 15:
                    kb.op("dve", lambda e: e.tensor_copy(out=RUN16[:], in_=RUN[:]), [RUN], [RUN16])

            nA = SEQ // 128
            a0(0)
            a0(1)
            ok = lambda i: 0 <= i < nA
            for t in range(nA + 2):
                if t + 2 < nA:
                    a0(t + 2)
                if ok(t - 2):
                    a3a(t - 2)
                if ok(t):
                    a1a(t)
                if ok(t - 1):
                    a2(t - 1)
                if ok(t):
                    a1b(t)
                if ok(t - 2):
                    a3b(t - 2)

        with kb.scope(LOHI):
            W = kb.sb("WB", [128, 8, 1024], BF16)
            WFU = kb.sb("WFU", [128, 8, 520], BF16)
            wv = w_b.rearrange("(k p) n -> p k n", p=128)
            wv2 = w_a.rearrange("(k p) n -> p k n", p=128)
            for k in range(8):
                kb.dma("pool", W[:, k, :], wv[:, k, :], writes=[W])
                kb.dma("pool", WFU[:, k, :], wv2[:, k, 1024:1544], writes=[WFU])
            NB = 2
            X = [kb.sb("X", [128, D]) for i in range(NB)]
            SQ = kb.sb("SQ", [128, D], BF16)
            SS = [kb.sb("SS", [128, 1]) for i in range(NB)]
            RS = [kb.sb("RS", [128, 1]) for i in range(NB)]
            HN = [kb.sb("HN", [128, D], BF16) for i in range(NB)]
            PT = [kb.ps("PT", [128, 8, 128], BF16) for i in range(NB)]
            PQ = kb.ps("PQ", [128, 2, 512])
            PTQ = kb.ps("PTQ", [128, 8, 128], BF16)
            PF = kb.ps("PF", [128, 8])
            PU = kb.ps("PU", [128, 512])
            QSQ = kb.sb("QSQ", [128, 512])
            QSS = [kb.sb("QSS", [128, 8]) for i in range(NB)]
            QB = [kb.sb("QB", [128, 1024], BF16) for i in range(NB)]
            LF = [kb.sb("LF", [128, 8]) for i in range(NB)]
            KVS = kb.sb("KVS", [128, 1032])
            HT4 = kb.sb("HT4", [128, 8, 512], BF16)
            HT4b = [HT4, kb.sb("HT4", [128, 8, 512], BF16)]
            X.append(KVS)
            QF = [kb.sb("QF", [128, 1024]) for i in range(2)]
            FF = [kb.sb("FF", [128, 8]) for i in range(2)]

            def tile_pos(i):
                return HT4b[(i // 4) % 2], slice((i % 4) * 128, (i % 4 + 1) * 128)

            def b0(i):
                kb.dma("sp", X[i % 3][:, 0:D], x_own[i * 128:(i + 1) * 128, :], writes=[X[i % 3]])

            def b1a(i):
                xb = X[i % 3]
                norm_a(_View(xb, xb[:, 0:D]), G, SS[i % 2], RS[i % 2], HN[i % 2], SQ)

            def b1b(i):
                ht4, ltk = tile_pos(i)
                norm_b(HN[i % 2], PT[i % 2], (ht4[:, :, ltk], ht4))
                if i % 4 == 3 or i == NT - 1:
                    grp = i // 4
                    t0 = grp * 512
                    nt = i % 4 + 1
                    for c in range(4):
                        for k in range(8):
                            kb.mm(PU[:, 0:nt * 128], WFU[:, k, 8 + c * 128:8 + (c + 1) * 128],
                                  ht4[:, k, 0:nt * 128], k == 0, k == 7, [WFU, ht4], [PU])
                        kb.op("act", lambda e: e.copy(out=UTO[:, c, t0:t0 + nt * 128], in_=PU[:, 0:nt * 128]),
                              [PU], [UTO])

            def b2(i):
                ht4, ltk = tile_pos(i)
                qf, ff = QF[i % 2], FF[i % 2]
                for ci in range(2):
                    for k in range(8):
                        kb.mm(PQ[:, ci, :], ht4[:, k, ltk], W[:, k, ci * 512:(ci + 1) * 512],
                              k == 0, k == 7, [ht4, W], [PQ])
                for k in range(8):
                    kb.mm(PF[:], ht4[:, k, ltk], WFU[:, k, 0:8], k == 0, k == 7, [ht4, WFU], [PF])
                kb.op("dve", lambda e: e.tensor_copy(out=qf[:].rearrange("p (c n) -> p c n", c=2), in_=PQ[:]),
                      [PQ], [qf])
                kb.op("dve", lambda e: e.tensor_copy(out=ff[:], in_=PF[:]), [PF], [ff])

            def b3(i):
                b = i % 2
                tok = slice(i * 128, (i + 1) * 128)
                qf, ff, qb = QF[b], FF[b], QB[b]
                head_norm((qf[:, 0:512], qf), 8, 64, QN, 0.125, QSQ, QSS[b], None, _View(qb, qb[:, 0:512]))
                head_norm((qf[:, 512:1024], qf), 4, 128, QNM, 128.0 ** -0.5, QSQ, QSS[b], None,
                          _View(qb, qb[:, 512:1024]))
                for c in range(8):
                    kb.tr(PTQ[:, c, :], qb[:, c * 128:(c + 1) * 128], IDb[:], [qb, IDb], [PTQ])
                kb.op("act", lambda e: e.copy(out=QT[:, :, tok], in_=PTQ[:, 0:4, :]), [PTQ], [QT])
                kb.op("act", lambda e: e.copy(out=QMT[:, :, tok], in_=PTQ[:, 4:8, :]), [PTQ], [QMT])
                if i < 16:
                    lf = LF[b]
                    logsig(lf, ff[:], ff)
                    kb.op("dve", lambda e: e.tensor_tensor(out=RUNO[:], in0=RUNO[:], in1=lf[:], op=ALU.add),
                          [RUNO, lf], [RUNO])
                    if i % 4 == 3:
                        kb.op("dve", lambda e: e.tensor_copy(out=RUNOJ[:, i // 4, :], in_=RUNO[:]),
                              [RUNO], [RUNOJ])

            okb = lambda i: 0 <= i < NT
            b0(0)
            b0(1)
            for t in range(NT + 2):
                if t + 2 < NT:
                    b0(t + 2)
                if okb(t):
                    b1a(t)
                if okb(t - 1):
                    b2(t - 1)
                if okb(t):
                    b1b(t)
                if okb(t - 2):
                    b3(t - 2)
            ht4, ltk = tile_pos(NT - 1)
            for k in range(8):
                kb.dma("pool", W[:, k, :], wv2[:, k, 0:1024], reads=[], writes=[W])
            for ci, (c0, c1) in enumerate(((0, 512), (512, 1024))):
                for k in range(8):
                    kb.mm(PQ[:, ci, :], ht4[:, k, ltk], W[:, k, c0:c1], k == 0, k == 7, [ht4, W], [PQ])
            kb.op("dve", lambda e: e.tensor_copy(out=KVS[:, 0:1024].rearrange("p (c n) -> p c n", c=2),
                                                 in_=PQ[:]), [PQ], [KVS])
            kb.op("dve", lambda e: e.tensor_copy(out=KVS[:, 1024:1032], in_=FF[(NT - 1) % 2][:]),
                  [FF[(NT - 1) % 2]], [KVS])
            KOS = _View(QF[1], QF[1][:, 0:512])
            KOBS = _View(QB[1], QB[1][:, 0:512])
            kv_tile(KVS, KOS, KOBS, QSQ, QSS[0], LFN, o_sk[:, :], o_sv[:, :], o_slf[:, :],
                    (VEN[:, :, 0:64], VEN), PTQ, (KTN[:], KTN))

        def normalize_out(pso, nq, OS, RR, PS_R, SEL, dst, dbuf):
            kb.op("dve", lambda e: e.tensor_copy(out=OS[:, 0:nq], in_=pso[:, 0:nq]), [pso], [OS])
            kb.mm(PS_R[:, 0:nq], SEL[:], OS[:, 0:nq], True, True, [SEL, OS], [PS_R])
            kb.op("act", lambda e: e.activation(out=RR[:, 0:nq], in_=PS_R[:, 0:nq], func=AF.Ln), [PS_R], [RR])
            kb.op("act", lambda e: e.activation(out=RR[:, 0:nq], in_=RR[:, 0:nq], func=AF.Exp, scale=-1.0),
                  [RR], [RR])
            kb.op("dve", lambda e: e.tensor_tensor(out=dst, in0=OS[0:64, 0:nq], in1=RR[:, 0:nq], op=ALU.mult),
                  [OS, RR], [dbuf])

        with kb.scope([[O_HI, ARENA_BYTES]]):
            MA = kb.sb("MA", [128, 4, 512], BF16)
            MB = kb.sb("MB", [128, 4, 512], BF16)
            kb.dma("pool", MA[:], ma_in, writes=[MA])
            kb.dma("pool", MB[:], mb_in, writes=[MB])
            SEL = kb.sb("SEL", [65, 64])
            kb.op("dve", lambda e: e.memset(SEL[:], 0.0), [], [SEL])
            kb.op("dve", lambda e: e.memset(SEL[64:65, :], 1.0), [], [SEL])
            BIAS = kb.sb("BIAS", [128, 4, 36, 8])
            TMP8 = kb.sb("TMP8", [128, 8])
            PCR = kb.ps("PCR", [128, 8])
            for J in range(4):
                nkb = 20 + 4 * J
                kb.op("dve", lambda e: e.scalar_tensor_tensor(
                    out=TMP8[:], in0=RUN16[:], scalar=RFL[:, 0:1], in1=RUNOJ[:, J, :], op0=ALU.mult,
                    op1=ALU.add), [RUN16, RFL, RUNOJ], [TMP8])
                kb.mm(PCR[:], ONES[:], TMP8[:], True, True, [ONES, TMP8], [PCR])
                kb.op("dve", lambda e: e.tensor_copy(out=CREF[:, J, :], in_=PCR[:]), [PCR], [CREF])
                kb.op("dve", lambda e: e.tensor_tensor(
                    out=BIAS[:, J, 0:nkb, :], in0=ADDM[:, J, 0:nkb, None].to_broadcast([128, nkb, 8]),
                    in1=CK[:, 0:nkb, :], op=ALU.subtract), [ADDM, CK], [BIAS])
                kb.op("dve", lambda e: e.tensor_tensor(
                    out=BIAS[:, J, 0:nkb, :], in0=BIAS[:, J, 0:nkb, :],
                    in1=CREF[:, J, None, :].to_broadcast([128, nkb, 8]), op=ALU.add), [BIAS, CREF], [BIAS])
            PS_S = [kb.ps("PS_S", [128, 512]) for i in range(3)]
            PS_O = [kb.ps("PS_O", [65, 512]) for i in range(2)]
            PS_R = kb.ps("PS_R", [64, 512])
            PTs = [kb.sb("PTs", [128, 512], BF16) for i in range(3)]
            OS = kb.sb("OS", [65, 512])
            RR = kb.sb("RR", [64, 512])
            LA = 2
            it = 0

            def run_pipe(items, tail):
                n = len(items)
                pend = []
                for idx in range(n + LA):
                    if idx < n:
                        items[idx][0]()
                    if idx >= LA:
                        items[idx - LA][1]()
                        ep = items[idx - LA][2]
                        if ep is not None:
                            pend.append([LA + 1, ep])
                    for pe_ in pend:
                        pe_[0] -= 1
                    for pe_ in [p_ for p_ in pend if p_[0] <= 0]:
                        pe_[1]()
                        pend.remove(pe_)
                for pe_ in pend:
                    pe_[1]()

            items = []
            for J in range(4):
                nkb = 20 + 4 * J
                qs = slice(J * 512, (J + 1) * 512)
                for h in range(8):
                    hp, po = h // 2, 64 * (h % 2)
                    pso = PS_O[(J * 8 + h) % 2]
                    for m in range(nkb):
                        pss, pts = PS_S[it % 3], PTs[it % 3]
                        it += 1

                        def qk(pss=pss, po=po, hp=hp, m=m, qs=qs):
                            kb.mm(pss[:], KT[po:po + 64, hp, m * 128:(m + 1) * 128], QT[po:po + 64, hp, qs],
                                  True, True, [KT, QT], [pss])

                        def post(pss=pss, pts=pts, pso=pso, J=J, m=m, h=h, nkb=nkb):
                            kb.op("act", lambda e: e.activation(out=pts[:], in_=pss[:], func=AF.Exp,
                                                                bias=BIAS[:, J, m, h:h + 1], scale=1.0),
                                  [pss, BIAS], [pts])
                            if 4 * J <= m < 4 * J + 4:
                                kb.op("dve", lambda e: e.tensor_tensor(out=pts[:], in0=pts[:], in1=MA[:, m - 4 * J, :],
                                                                       op=ALU.mult), [pts, MA], [pts])
                            if 16 + 4 * J <= m:
                                kb.op("dve", lambda e: e.tensor_tensor(out=pts[:], in0=pts[:],
                                                                       in1=MB[:, m - 16 - 4 * J, :], op=ALU.mult),
                                      [pts, MB], [pts])
                            kb.mm(pso[:], VE[:, m, h, :], pts[:], m == 0, m == nkb - 1, [VE, pts], [pso])

                        ep = None
                        if m == nkb - 1:
                            ep = (lambda pso=pso, h=h, qs=qs: normalize_out(pso, 512, OS, RR, PS_R, SEL,
                                                                            OFT[:, h, qs], OFT))
                        items.append((qk, post, ep))
            run_pipe(items, None)

            CKS = [kb.sb("CKS", [128, 4, 512], BF16) for i in range(2)]
            STGC = [kb.sb("STGC", [128, 2, 512]) for i in range(3)]
            stg_i = [0]
            LFS = kb.sb("LFS", [128, 32, 8])
            CKN = kb.sb("CKN", [128, 8])
            BIASN = kb.sb("BIASN", [128, 8])
            CRS = kb.sb("CRS", [128, 8])
            PTK = kb.ps("PTK2", [128, 2, 4, 128], BF16)
            for sbi in range(2):
                r0 = 64 * sbi
                rs_ = slice(r0, r0 + 64)
                qcol = slice(HALF + r0, HALF + r0 + 64)
                ckv = c_k[sbi].rearrange("(n p) f -> p n f", p=128)
                cvf = c_v[sbi].rearrange("(n p) f -> p n f", p=128)
                kb.dma("sp", LFS[:], c_lf[sbi].rearrange("(n p) h -> p n h", p=128), writes=[LFS])
                for g4 in range(8):
                    cks = CKS[g4 % 2]
                    for hf2 in range(2):
                        t0_ = g4 * 4 + hf2 * 2
                        stg = STGC[stg_i[0] % 3]
                        stg_i[0] += 1
                        kb.dma("sp", stg[:], ckv[:, t0_:t0_ + 2, :], writes=[stg])
                        kb.op("dve", lambda e: e.tensor_copy(out=cks[:, hf2 * 2:hf2 * 2 + 2, :], in_=stg[:]),
                              [stg], [cks])
                        stg = STGC[stg_i[0] % 3]
                        stg_i[0] += 1
                        kb.dma("sp", stg[:], cvf[:, t0_:t0_ + 2, :], writes=[stg])
                        kb.op("pool", lambda e: e.tensor_copy(
                            out=VE[:, t0_:t0_ + 2, :, 0:64],
                            in_=stg[:].rearrange("p n (h d) -> p n h d", d=64)), [stg], [VE])
                    for j2 in range(2):
                        for j in range(2):
                            for hp in range(4):
                                kb.tr(PTK[:, j, hp, :], cks[:, j2 * 2 + j, hp * 128:(hp + 1) * 128], IDb[:],
                                      [cks, IDb], [PTK])
                        c0 = g4 * 512 + j2 * 256
                        kb.op("act", lambda e: e.copy(
                            out=KT[:, :, c0:c0 + 256].rearrange("p h (j t) -> p j h t", j=2),
                            in_=PTK[:]), [PTK], [KT])
                kb.op("dve", lambda e: e.memset(RUN[:], 0.0), [], [RUN])
                for i in range(32):
                    kb.mm(PCR[:], TRIU[:], LFS[:, i, :], True, False, [TRIU, LFS], [PCR])
                    kb.mm(PCR[:], ONES[:], RUN[:], False, True, [ONES, RUN], [PCR])
                    kb.op("dve", lambda e: e.tensor_copy(out=CK[:, i, :], in_=PCR[:]), [PCR], [CK])
                    kb.op("dve", lambda e: e.tensor_tensor(out=RUN[:], in0=RUN[:], in1=LFS[:, i, :], op=ALU.add),
                          [RUN, LFS], [RUN])
                kb.mm(PCR[rs_, :], TRIU[rs_, rs_], LFN[rs_, :], True, False, [TRIU, LFN], [PCR])
                kb.mm(PCR[rs_, :], ONES[:, rs_], RUN[:], False, True, [ONES, RUN], [PCR])
                kb.op("dve", lambda e: e.tensor_copy(out=CKN[rs_, :], in_=PCR[rs_, :]), [PCR], [CKN])
                kb.mm(PCR[:], ONES[:], RUN[:], True, False, [ONES, RUN], [PCR])
                kb.mm(PCR[:], ONES[rs_, :], LFN[rs_, :], False, True, [ONES, LFN], [PCR])
                kb.op("dve", lambda e: e.tensor_copy(out=CRS[:], in_=PCR[:]), [PCR], [CRS])
                kb.op("dve", lambda e: e.tensor_tensor(out=BIAS[:, 0, 0:32, :],
                                                       in0=CRS[:, None, :].to_broadcast([128, 32, 8]),
                                                       in1=CK[:, 0:32, :], op=ALU.subtract), [CRS, CK], [BIAS])
                kb.op("dve", lambda e: e.tensor_tensor(out=BIASN[rs_, :], in0=CRS[rs_, :], in1=CKN[rs_, :],
                                                       op=ALU.subtract), [CRS, CKN], [BIASN])
                items = []
                for h in range(8):
                    hp, po = h // 2, 64 * (h % 2)
                    pso = PS_O[h % 2]
                    for m in range(33):
                        pss, pts = PS_S[it % 3], PTs[it % 3]
                        it += 1
                        if m < 32:
                            def qk(pss=pss, po=po, hp=hp, m=m):
                                kb.mm(pss[:, 0:64], KT[po:po + 64, hp, m * 128:(m + 1) * 128],
                                      QT[po:po + 64, hp, qcol], True, True, [KT, QT], [pss])

                            def post(pss=pss, pts=pts, pso=pso, m=m, h=h):
                                kb.op("act", lambda e: e.activation(out=pts[:, 0:64], in_=pss[:, 0:64], func=AF.Exp,
                                                                    bias=BIAS[:, 0, m, h:h + 1], scale=1.0),
                                      [pss, BIAS], [pts])
                                kb.mm(pso[:, 0:64], VE[:, m, h, :], pts[:, 0:64], m == 0, False, [VE, pts], [pso])
                            ep = None
                        else:
                            def qk(pss=pss, po=po, hp=hp):
                                kb.mm(pss[rs_, 0:64], KTN[po:po + 64, hp, rs_], QT[po:po + 64, hp, qcol],
                                      True, True, [KTN, QT], [pss])

                            def post(pss=pss, pts=pts, pso=pso, h=h):
                                kb.op("act", lambda e: e.activation(out=pts[rs_, 0:64], in_=pss[rs_, 0:64],
                                                                    func=AF.Exp, bias=BIASN[rs_, h:h + 1], scale=1.0),
                                      [pss, BIASN], [pts])
                                kb.op("dve", lambda e: e.tensor_tensor(out=pts[rs_, 0:64], in0=pts[rs_, 0:64],
                                                                       in1=TRIU[rs_, rs_], op=ALU.mult),
                                      [pts, TRIU], [pts])
                                kb.mm(pso[:, 0:64], VEN[rs_, h, :], pts[rs_, 0:64], False, True, [VEN, pts], [pso])
                            ep = (lambda pso=pso, h=h: normalize_out(pso, 64, OS, RR, PS_R, SEL, OFT[:, h, qcol], OFT))
                        items.append((qk, post, ep))
                run_pipe(items, None)


        OMT = kb.at(O_OMT, [128, 4, NOWN], BF16)
        with kb.scope([[O_FREE2, O_QMT], [O_HI, ARENA_BYTES]]):
            WM = kb.sb("WM", [128, 8, 1024], BF16)
            wv = w_mkv.rearrange("(k p) n -> p k n", p=128)
            for k in range(8):
                kb.dma("pool", WM[:, k, :], wv[:, k, :], writes=[WM])
            GM = kb.sb("GM", [128, D])
            KNM = kb.sb("KNM", [128, 512])
            kb.dma("sp", GM[:], gmem, writes=[GM])
            kb.dma("sp", KNM[:], knm_rep, writes=[KNM])
            ONESb = kb.sb("ONESb", [128, 128], BF16)
            kb.op("dve", lambda e: e.memset(ONESb[:], 1.0), [], [ONESb])
            X = [kb.sb("X", [128, D]) for i in range(2)]
            SQ = kb.sb("SQ", [128, D], BF16)
            SS = [kb.sb("SS", [128, 1]) for i in range(2)]
            RS = [kb.sb("RS", [128, 1]) for i in range(2)]
            HN = [kb.sb("HN", [128, D], BF16) for i in range(2)]
            HT = [kb.sb("HT", [128, 8, 128], BF16) for i in range(2)]
            KVm = [kb.sb("KVm", [128, 1024]) for i in range(2)]
            KSQ = kb.sb("KSQ", [128, 512])
            KSS = kb.sb("KSS", [128, 8])
            KO = [kb.sb("KO", [128, 512]) for i in range(2)]
            KOB = [kb.sb("KOB", [128, 512], BF16) for i in range(2)]
            MKT = kb.sb("MKT", [128, 4, 256], BF16)
            MV = kb.sb("MV", [128, 2, 512], BF16)
            CST = [kb.sb("CST", [128, 2, 512], BF16) for i in range(2)]
            PTs = [kb.sb("PTs", [128, 512], BF16) for i in range(2)]
            RR = kb.sb("RR", [128, 512])
            PT = [kb.ps("PT", [128, 8, 128], BF16) for i in range(2)]
            PKV = kb.ps("PKV", [128, 2, 512])
            PS_S = [kb.ps("PS_S", [128, 512]) for i in range(2)]
            PS_O = kb.ps("PS_O", [128, 512])
            PS_D = kb.ps("PS_D", [128, 512])
            for i in range(2):
                x, ht, kv = X[i], HT[i], KVm[i]
                kb.dma("sp", x[:], mem_in[i * 128:(i + 1) * 128, :], writes=[x])
                norm_tile(x, GM, SS[i], RS[i], HN[i], SQ, PT[i], (ht[:], ht))
                for ci in range(2):
                    for k in range(8):
                        kb.mm(PKV[:, ci, :], ht[:, k, :], WM[:, k, ci * 512:(ci + 1) * 512], k == 0, k == 7,
                              [ht, WM], [PKV])
                kb.op("dve", lambda e: e.tensor_copy(out=kv[:].rearrange("p (c n) -> p c n", c=2), in_=PKV[:]),
                      [PKV], [kv])
                rows = slice(i * 128, (i + 1) * 128)
                kb.dma("sp", o_mv[rows, :], kv[:, 512:1024], reads=[kv])
                kb.op("pool", lambda e: e.tensor_copy(out=MV[:, i, :], in_=kv[:, 512:1024]), [kv], [MV])
                head_norm((kv[:, 0:512], kv), 4, 128, KNM, 1.0, KSQ, KSS, KO[i], KOB[i])
                kb.dma("sp", o_mk[rows, :], KO[i][:], reads=[KO[i]])
                for h in range(4):
                    kb.tr(PT[i][:, h, :], KOB[i][:, h * 128:(h + 1) * 128], IDb[:], [KOB[i], IDb], [PT[i]])
                kb.op("act", lambda e: e.copy(out=MKT[:, :, rows], in_=PT[i][:, 0:4, :]), [PT[i]], [MKT])

            def mem_attend(qs, nq, it0):
                it = it0
                for h in range(4):
                    for mb in range(2):
                        pss, pts = PS_S[it % 2], PTs[it % 2]
                        it += 1
                        kb.mm(pss[:, 0:nq], MKT[:, h, mb * 128:(mb + 1) * 128], QMT[:, h, qs], True, True,
                              [MKT, QMT], [pss])
                        kb.op("act", lambda e: e.activation(out=pts[:, 0:nq], in_=pss[:, 0:nq], func=AF.Exp),
                              [pss], [pts])
                        kb.mm(PS_O[:, 0:nq], MV[:, mb, h * 128:(h + 1) * 128], pts[:, 0:nq], mb == 0, mb == 1,
                              [MV, pts], [PS_O])
                        kb.mm(PS_D[:, 0:nq], ONESb[:], pts[:, 0:nq], mb == 0, mb == 1, [ONESb, pts], [PS_D])
                    kb.op("act", lambda e: e.activation(out=RR[:, 0:nq], in_=PS_D[:, 0:nq], func=AF.Ln), [PS_D], [RR])
                    kb.op("act", lambda e: e.activation(out=RR[:, 0:nq], in_=RR[:, 0:nq], func=AF.Exp, scale=-1.0),
                          [RR], [RR])
                    kb.op("dve", lambda e: e.tensor_tensor(out=OMT[:, h, qs], in0=PS_O[:, 0:nq], in1=RR[:, 0:nq],
                                                           op=ALU.mult), [PS_O, RR], [OMT])
                return it

            it = 0
            for J in range(4):
                it = mem_attend(slice(J * 512, (J + 1) * 512), 512, it)
            for sbi in range(2):
                ck, cv = CST[0], CST[1]
                kb.dma("pool", ck[:], c_mk[sbi].rearrange("(n p) f -> p n f", p=128), writes=[ck])
                kb.dma("pool", MV[:], c_mv[sbi].rearrange("(n p) f -> p n f", p=128), writes=[MV])
                for i in range(2):
                    for h in range(4):
                        kb.tr(PT[i][:, h, :], ck[:, i, h * 128:(h + 1) * 128], IDb[:], [ck, IDb], [PT[i]])
                    kb.op("act", lambda e: e.copy(out=MKT[:, :, i * 128:(i + 1) * 128], in_=PT[i][:, 0:4, :]),
                          [PT[i]], [MKT])
                it = mem_attend(slice(HALF + 64 * sbi, HALF + 64 * sbi + 64), 64, it)

        UTF = kb.at(O_UTF, [128, 4, HALF], BF16)
        YGALL = kb.at(O_UTF, [128, 32, 272], BF16)
        YS2 = kb.at(O_YS2, [128, 4, NOWN], BF16)
        with kb.scope([[O_FREE2, O_UTO], [O_HI, ARENA_BYTES]]):
            WU = kb.sb("WU", [128, 8, 512], BF16)
            wv2 = w_a.rearrange("(k p) n -> p k n", p=128)
            STGW = [kb.sb("STGW", [128, 2, 512]) for i in range(2)]
            for k2 in range(4):
                stg = STGW[k2 % 2]
                kb.dma("sp", stg[:], wv2[:, 2 * k2:2 * k2 + 2, 1032:1544], writes=[stg])
                kb.op(("dve", "pool")[k2 % 2], lambda e: e.tensor_copy(out=WU[:, 2 * k2:2 * k2 + 2, :], in_=stg[:]),
                      [stg], [WU])
            X = [kb.sb("X", [128, D]) for i in range(2)]
            SQ = kb.sb("SQ", [128, D], BF16)
            SS = [kb.sb("SS", [128, 1]) for i in range(2)]
            RS = [kb.sb("RS", [128, 1]) for i in range(2)]
            HN = [kb.sb("HN", [128, D], BF16) for i in range(2)]
            HT4 = [kb.sb("HT4", [128, 8, 512], BF16) for i in range(2)]
            PT = [kb.ps("PT", [128, 8, 128], BF16) for i in range(2)]
            PU = [kb.ps("PU", [128, 512]) for i in range(2)]
            for grp in range(4):
                ht4 = HT4[grp % 2]
                for j in range(4):
                    i = grp * 4 + j
                    b = i % 2
                    kb.dma("sp", X[b][:], x_all[i * 128:(i + 1) * 128, :], writes=[X[b]])
                    norm_tile(X[b], G, SS[b], RS[b], HN[b], SQ, PT[b], (ht4[:, :, j * 128:(j + 1) * 128], ht4))
                for c in range(4):
                    pu = PU[c % 2]
                    for k in range(8):
                        kb.mm(pu[:], WU[:, k, c * 128:(c + 1) * 128], ht4[:, k, :], k == 0, k == 7, [WU, ht4], [pu])
                    kb.op("act", lambda e: e.copy(out=UTF[:, c, grp * 512:(grp + 1) * 512], in_=pu[:]), [pu], [UTF])

        R_A = [O_FREE2, O_UTO]
        R_B = [O_HI, ARENA_BYTES]
        with kb.scope([R_B]):
            T0 = kb.sb("T0", [128, 32, 128], BF16)
            WST = kb.sb("WST", [128, 32, 128], BF16)
            VVB = kb.sb("VVB", [128, 32, 128], BF16)
            RC = kb.sb("RC", [128, 32, 9, 2])
            CRc = kb.sb("CRc", [128, 32])
            CIs = kb.sb("CIs", [128, 32])
            EALL = kb.sb("EALL", [128, 32])
            INITA = kb.sb("INITA", [128, 3, 32])
            FINS = kb.sb("FINS", [128, 3, 32])
            SG = kb.sb("SG", [128, 1])
            kb.dma("sp", SG[:], sp_sg, writes=[SG])
            with kb.scope([R_A]):
                P1 = kb.sb("P1", [128, 32, 41])
                P2 = kb.sb("P2", [128, 32, 41])
                CA = kb.sb("CA", [128, 32, 16])
                CB = kb.sb("CB", [128, 32, 16])
                BA = kb.sb("BA", [128, 32, 16])
                BB = kb.sb("BB", [128, 32, 16])
                SCR0 = kb.regions[0][0]
                ARE = kb.sb("ARE", [128, 32])
                AIM = kb.sb("AIM", [128, 32])
                LDT = kb.sb("LDT", [128, 32])
                ET = kb.sb("ET", [128, 41])
                CRE = kb.sb("CRE", [128, 32, 16])
                CIM = kb.sb("CIM", [128, 32, 16])
                DAR = kb.sb("DAR", [128, 32])
                DAI = kb.sb("DAI", [128, 32])
                Y = kb.sb("Y", [128, 32, 41])
                TF = kb.sb("TF", [128, 32, 41])
                TI = kb.sb("TI", [128, 32, 41], I32)
                MAG = kb.sb("MAG", [128, 32, 41])
                SIN, COS = P2, P1
                t32 = [kb.sb("t32", [128, 32]) for i in range(6)]
                for dst, src in ((ARE, sp_are), (AIM, sp_aim), (LDT, sp_ldt), (ET, sp_et), (BA, sp_bre), (BB, sp_bim),
                                 (CRE, sp_cre), (CIM, sp_cim)):
                    kb.dma("sp", dst[:], src, writes=[dst])
                TT = lambda o, a, b_, op, rd, wr: kb.op("dve", lambda e: e.tensor_tensor(out=o, in0=a, in1=b_, op=op),
                                                        rd, wr)
                TS = lambda o, a, s1, s2, op0, op1, rd, wr: kb.op(
                    "dve", lambda e: e.tensor_scalar(out=o, in0=a, scalar1=s1, scalar2=s2, op0=op0,
                                                     **({"op1": op1} if op1 is not None else {})), rd, wr)
                kb.op("act", lambda e: e.activation(out=LDT[:], in_=LDT[:], func=AF.Exp), [LDT], [LDT])
                TT(DAR[:], LDT[:], ARE[:], ALU.mult, [LDT, ARE], [DAR])
                TT(DAI[:], LDT[:], AIM[:], ALU.mult, [LDT, AIM], [DAI])
                bc_g = lambda t: t[:, :, None].to_broadcast([128, 32, 41])
                bc_e = lambda t: t[:, None, :].to_broadcast([128, 32, 41])
                TT(MAG[:], bc_g(DAR), bc_e(ET), ALU.mult, [DAR, ET], [MAG])
                kb.op("act", lambda e: e.activation(out=MAG[:], in_=MAG[:], func=AF.Exp), [MAG], [MAG])
                TT(Y[:], bc_g(DAI), bc_e(ET), ALU.mult, [DAI, ET], [Y])
                TS(Y[:], Y[:], 1.0 / (2.0 * np.pi), None, ALU.mult, None, [Y], [Y])

                def sin_of(dst, shift):
                    TS(TF[:], Y[:], float(shift), None, ALU.add, None, [Y], [TF])
                    kb.op("dve", lambda e: e.tensor_copy(out=TI[:], in_=TF[:]), [TF], [TI])
                    kb.op("dve", lambda e: e.tensor_copy(out=dst[:], in_=TI[:]), [TI], [dst])
                    TT(TF[:], TF[:], dst[:], ALU.subtract, [TF, dst], [TF])
                    TS(dst[:], TF[:], 0.5, None, ALU.is_gt, None, [TF], [dst])
                    TT(TF[:], TF[:], dst[:], ALU.subtract, [TF, dst], [TF])
                    TS(dst[:], TF[:], -0.5, None, ALU.is_lt, None, [TF], [dst])
                    TT(TF[:], TF[:], dst[:], ALU.add, [TF, dst], [TF])
                    kb.op("act", lambda e: e.activation(out=dst[:], in_=TF[:], func=AF.Sin, scale=6.283185),
                          [TF], [dst])

                sin_of(SIN, 0.0)
                sin_of(COS, 0.25)
                TT(COS[:], COS[:], MAG[:], ALU.mult, [COS, MAG], [COS])
                TT(SIN[:], SIN[:], MAG[:], ALU.mult, [SIN, MAG], [SIN])
                top, bot = slice(0, 64), slice(64, 128)
                cp = lambda o, a, rd, wr: kb.op("dve", lambda e: e.tensor_copy(out=o, in_=a), rd, wr)
                NR, DEN, C1, C2, C3, C4 = t32
                abr, abi = COS[:, :, 16], SIN[:, :, 16]
                TS(NR[:], abr, -1.0, None, ALU.add, None, [COS], [NR])
                TT(DEN[:], ARE[:], ARE[:], ALU.mult, [ARE], [DEN])
                TT(C1[:], AIM[:], AIM[:], ALU.mult, [AIM], [C1])
                TT(DEN[:], DEN[:], C1[:], ALU.add, [DEN, C1], [DEN])
                kb.op("dve", lambda e: e.reciprocal(out=DEN[:], in_=DEN[:]), [DEN], [DEN])
                TT(C1[:], NR[:], ARE[:], ALU.mult, [NR, ARE], [C1])
                TT(C2[:], abi, AIM[:], ALU.mult, [SIN, AIM], [C2])
                TT(C1[:], C1[:], C2[:], ALU.add, [C1, C2], [C1])
                TT(CRc[:], C1[:], DEN[:], ALU.mult, [C1, DEN], [CRc])
                TT(C1[:], abi, ARE[:], ALU.mult, [SIN, ARE], [C1])
                TT(C2[:], NR[:], AIM[:], ALU.mult, [NR, AIM], [C2])
                TT(C1[:], C1[:], C2[:], ALU.subtract, [C1, C2], [C1])
                TT(C3[:], C1[:], DEN[:], ALU.mult, [C1, DEN], [C3])
                TS(CIs[:], C3[:], SG[:, 0:1], None, ALU.mult, None, [C3, SG], [CIs])
                bc_h = lambda t: t[:, :, None].to_broadcast([128, 32, 16])
                TT(CA[:], CRE[:], bc_h(CRc), ALU.mult, [CRE, CRc], [CA])
                TT(CB[:], CIM[:], bc_h(C3), ALU.mult, [CIM, C3], [CB])
                TT(CA[:], CA[:], CB[:], ALU.subtract, [CA, CB], [CA])
                TT(CB[:], CRE[:], bc_h(C3), ALU.mult, [CRE, C3], [CB])
                TT(CRE[:], CIM[:], bc_h(CRc), ALU.mult, [CIM, CRc], [CRE])
                TT(CB[:], CB[:], CRE[:], ALU.add, [CB, CRE], [CB])
                TS(CB[:], CB[:], -1.0, None, ALU.mult, None, [CB], [CB])
                TS(CA[bot], CA[bot], -1.0, None, ALU.mult, None, [CA], [CA])
                TS(BB[top], BB[top], -1.0, None, ALU.mult, None, [BB], [BB])
                cp(TF[bot], P1[bot], [P1], [TF])
                cp(P1[bot], P2[bot], [P2], [P1])
                cp(P2[bot], TF[bot], [TF], [P2])
                cp(RC[top, :, :, 0], P1[top, :, 32:41], [P1], [RC])
                cp(RC[top, :, :, 1], P2[top, :, 32:41], [P2], [RC])
                TS(RC[bot, :, :, 0], P1[bot, :, 32:41], -1.0, None, ALU.mult, None, [P1], [RC])
                cp(RC[bot, :, :, 1], P2[bot, :, 32:41], [P2], [RC])
                H0 = kb.sb("H0", [128, 2, 32])
                H0S = kb.sb("H0S", [128, 2, 32])
                kb.dma("sp", H0[:], sp_h0, writes=[H0])
                kb.dma("sp", H0S[:], sp_h0s, writes=[H0S])
                TT(C1[:], CRc[:], CRc[:], ALU.mult, [CRc], [C1])
                TT(C2[:], C3[:], C3[:], ALU.mult, [C3], [C2])
                TT(C1[:], C1[:], C2[:], ALU.add, [C1, C2], [C1])
                kb.op("dve", lambda e: e.reciprocal(out=C1[:], in_=C1[:]), [C1], [C1])
                b2 = lambda t: t[:, None, :].to_broadcast([128, 2, 32])
                TT(H0[:], H0[:], b2(CRc), ALU.mult, [H0, CRc], [H0])
                TT(H0S[:], H0S[:], b2(CIs), ALU.mult, [H0S, CIs], [H0S])
                TT(H0[:], H0[:], H0S[:], ALU.add, [H0, H0S], [H0])
                TT(INITA[:, 1:3, :], H0[:], b2(C1), ALU.mult, [H0, C1], [INITA])
                kb.barrier()
                kb.regions = [[SCR0, O_UTO]]
                BIGA = kb.sb("BIGA", [128, 16, 8, 16])
                BIGB = kb.sb("BIGB", [128, 16, 8, 16])
                BIGT = kb.sb("BIGT", [128, 16, 8, 16])
                CAUS = kb.sb("CAUS", [128, 128])
                DCOL = kb.sb("DCOL", [128, 32])
                TMPM = kb.sb("TMPM", [128, 128])
                kb.dma("sp", CAUS[:], sp_caus, writes=[CAUS])
                kb.dma("sp", DCOL[:], sp_dcol, writes=[DCOL])
                PSM = [kb.ps("PSM", [128, 128]) for i in range(2)]

                def big(dst, xa, xb, c0, gs, wr_extra=None):
                    bx = lambda t: t[:, gs, None, :].to_broadcast([128, 16, 8, 16])
                    bp = lambda t: t[:, gs, c0:c0 + 8, None].to_broadcast([128, 16, 8, 16])
                    TT(BIGT[:], bx(xb), bp(P2), ALU.mult, [xb, P2], [BIGT])
                    TT(dst[:], bx(xa), bp(P1), ALU.mult, [xa, P1], [dst])
                    TT(dst[:], dst[:], BIGT[:], ALU.add, [dst, BIGT], [dst])

                for gh in range(2):
                    gs = slice(gh * 16, (gh + 1) * 16)
                    big(BIGA, BA, BB, 8, gs)
                    big(BIGB, CA, CB, 24, gs)
                    for gl in range(16):
                        g = gh * 16 + gl
                        ps = PSM[g % 2]
                        kb.mm(ps[:], BIGA[:, gl].rearrange("p a b -> p (a b)"),
                              BIGB[:, gl].rearrange("p a b -> p (a b)"), True, True, [BIGA, BIGB], [ps])
                        TT(TMPM[:], ps[:], CAUS[:], ALU.mult, [ps, CAUS], [TMPM])
                        kb.op("dve", lambda e: e.scalar_tensor_tensor(out=T0[:, g, :], in0=IDf[:],
                                                                      scalar=DCOL[:, g:g + 1], in1=TMPM[:],
                                                                      op0=ALU.mult, op1=ALU.add),
                              [IDf, DCOL, TMPM], [T0])
                    big(BIGA, BA, BB, 0, gs)
                    for gl in range(16):
                        g = gh * 16 + gl
                        ps = PSM[g % 2]
                        kb.tr(ps[:], BIGA[:, gl].rearrange("p a b -> p (a b)"), IDf[:], [BIGA, IDf], [ps])
                        kb.op("act", lambda e: e.copy(out=WST[:, g, :], in_=ps[:]), [ps], [WST])
                    big(BIGB, CA, CB, 16, gs)
                    kb.op("act", lambda e: e.copy(out=VVB[:, gs, :], in_=BIGB[:].rearrange("p g a b -> p g (a b)")),
                          [BIGB], [VVB])

            with kb.scope([R_A]):
                ESEL = kb.sb("ESEL", [128, 64, 128], BF16)
                kb.dma("pool", ESEL[:, 0:32, :], sp_esel[:, 0:32, :], writes=[ESEL])
                kb.dma("pool", ESEL[:, 32:64, :], sp_esel[:, 32:64, :], writes=[ESEL])
                WG = kb.sb("WG", [128, 4, 1024], BF16)
                kb.dma("pool", WG[:], w_glu.rearrange("(c p) n -> p c n", p=128), writes=[WG])
                SWP = kb.sb("SWP", [128, 128])
                kb.dma("sp", SWP[:], sp_swap, writes=[SWP])
                NL = 2
                U8 = [kb.sb("U8", [128, 272], BF16) for i in range(NL)]
                HBL = [[kb.sb("HB", [128, 276], BF16) for i in range(3)] for l in range(NL)]
                RK = [kb.sb("RK", [128, 9, 128], BF16) for i in range(NL)]
                HSL = [[kb.sb("HS", [128, 2, 10]) for i in range(2)] for l in range(NL)]
                HSBL = [kb.sb("HSB", [128, 2, 10], BF16) for l in range(NL)]
                RKF = [kb.sb("RKF", [128, 4, 128]) for i in range(NL)]
                GT1L = [kb.sb("GT1", [128, 272]) for l in range(NL)]
                GT2L = [kb.sb("GT2", [128, 272]) for l in range(NL)]
                GT1B = kb.sb("GT1B", [128, 512])
                PU8L = [kb.ps("PU8", [128, 272]) for l in range(NL)]
                PXL = [kb.ps("PX", [128, 512]) for l in range(NL)]
                PXSL = [kb.ps("PXS", [128, 2, 10]) for l in range(NL)]
                PY8L = [kb.ps("PY8", [128, 272]) for l in range(NL)]
                PX = PXL
                PY8 = PY8L[0]

                def interleave(gens):
                    gens = list(gens)
                    while gens:
                        for g_ in list(gens):
                            try:
                                next(g_)
                            except StopIteration:
                                gens.remove(g_)

                evac_i = [0]

                def evac(dst, dbuf, src, sbuf):
                    evac_i[0] += 1
                    if evac_i[0] % 2:
                        kb.op("act", lambda e: e.copy(out=dst, in_=src), [sbuf], [dbuf])
                    else:
                        kb.op("dve", lambda e: e.tensor_copy(out=dst, in_=src), [sbuf], [dbuf])

                def build_rk(g, rk):
                    for hs, idv in ((slice(0, 64), IDf[0:64, 0:64]), (slice(64, 128), IDf[64:128, 64:128])):
                        kb.op("pool", lambda e: e.tensor_tensor(
                            out=rk[hs].rearrange("p k (c j) -> p k c j", c=2),
                            in0=idv[:, None, None, :].to_broadcast([64, 9, 2, 64]),
                            in1=RC[hs, g, :, :, None].to_broadcast([64, 9, 2, 64]), op=ALU.mult),
                            [IDf, RC], [rk])

                def build_rkf(g, rkf):
                    for hs, idv in ((slice(0, 64), IDf[0:64, 0:64]), (slice(64, 128), IDf[64:128, 64:128])):
                        kb.op("pool", lambda e: e.tensor_tensor(
                            out=rkf[hs].rearrange("p k (c j) -> p k c j", c=2),
                            in0=idv[:, None, None, :].to_broadcast([64, 4, 2, 64]),
                            in1=RC[hs, g, 0:4, :, None].to_broadcast([64, 4, 2, 64]), op=ALU.mult),
                            [IDf, RC], [rkf])

                def build_u8(src, ct, gp, ncol, u8, PU8):
                    sv = src[:, ct, 0:ncol * 8].rearrange("p (n s) -> p s n", s=8)
                    for s8 in range(8):
                        kb.mm(PU8[:, 0:ncol], ESEL[:, gp * 8 + s8, :], sv[:, s8, :], s8 == 0, s8 == 7,
                              [ESEL, src], [PU8])
                    evac(u8[:, 0:ncol], u8, PU8[:, 0:ncol], PU8)

                def body1(g, ln):
                    ct, gp = g // 8, g % 8
                    rk, u8, HB, px = RK[ln], U8[ln], HBL[ln], PXL[ln]
                    build_rk(g, rk)
                    build_u8(UTF, ct, gp, 256, u8, PU8L[ln])
                    yield
                    kb.mm(px[:, 0:256], WST[:, g, :], u8[:, 0:256], True, True, [WST, u8], [px])
                    hcur = HB[0]
                    evac(hcur[:, 0:256], hcur, px[:, 0:256], px)
                    yield
                    n = 256
                    for k in range(8):
                        n //= 2
                        hv = hcur[:, 0:2 * n].rearrange("p (n two) -> p two n", two=2)
                        kb.mm(px[:, 0:n], IDb[:], hv[:, 1, :], True, False, [IDb, hcur], [px])
                        kb.mm(px[:, 0:n], rk[:, k, :], hv[:, 0, :], False, True, [rk, hcur], [px])
                        hnext = HB[(k + 1) % 3]
                        if k < 7:
                            evac(hnext[:, 0:n], hnext, px[:, 0:n], px)
                            hcur = hnext
                        else:
                            kb.op("dve", lambda e: e.tensor_copy(out=EALL[:, g:g + 1], in_=px[:, 0:1]), [px], [EALL])
                        yield

                for g0 in range(0, 32, NL):
                    interleave([body1(g0 + ln, ln) for ln in range(NL)])
                kb.op("dve", lambda e: e.tensor_scalar(out=INITA[:, 0, :], in0=EALL[:], scalar1=RFL[:, 0:1],
                                                       scalar2=None, op0=ALU.mult), [EALL, RFL], [INITA])

                for HS in HSL:
                    for hsb in HS:
                        kb.op("dve", lambda e: e.memset(hsb[:], 0.0), [], [hsb])

                def body2(g, ln):
                    ct, gp = g // 8, g % 8
                    rk, u8, rkf, HB, px = RK[ln], U8[ln], RKF[ln], HBL[ln], PXL[ln]
                    HS, HSB, PXS, PY8, GT1, GT2 = HSL[ln], HSBL[ln], PXSL[ln], PY8L[ln], GT1L[ln], GT2L[ln]
                    build_rk(g, rk)
                    build_rkf(g, rkf)
                    build_u8(UTO, ct, gp, 272, u8, PU8L[ln])
                    yield
                    kb.mm(px[:, 0:272], WST[:, g, :], u8[:, 0:272], True, True, [WST, u8], [px])
                    hcur = HB[0]
                    evac(hcur[:, 1:257], hcur, px[:, 0:256], px)
                    kb.op("pool", lambda e: e.tensor_copy(out=hcur[:, 0:1], in_=INITA[:, 0, g:g + 1]), [INITA], [hcur])
                    hs = HS[0]
                    kb.op("dve", lambda e: e.tensor_copy(out=hs[:, :, 1:9],
                                                         in_=px[:, 256:272].rearrange("p (a b) -> p a b", a=2)),
                          [px], [hs])
                    kb.op("pool", lambda e: e.tensor_copy(out=hs[:, :, 0], in_=INITA[:, 1:3, g]), [INITA], [hs])
                    yield
                    for k in range(9):
                        sft = 1 << k
                        kb.mm(px[:, 0:257], IDb[:], hcur[:, 0:257], True, False, [IDb, hcur], [px])
                        kb.mm(px[:, sft:257], rk[:, k, :], hcur[:, 0:257 - sft], False, True, [rk, hcur], [px])
                        hnext = HB[(k + 1) % 3]
                        evac(hnext[:, 0:257], hnext, px[:, 0:257], px)
                        hcur = hnext
                        if k < 4:
                            kb.mm(PXS[:], IDf[:], hs[:], True, False, [IDf, hs], [PXS])
                            kb.mm(PXS[:, :, sft:10], rkf[:, k, :], hs[:, :, 0:10 - sft], False, True, [rkf, hs], [PXS])
                            hsn = HS[(k + 1) % 2]
                            kb.op("dve", lambda e: e.tensor_copy(out=hsn[:], in_=PXS[:]), [PXS], [hsn])
                            hs = hsn
                        yield
                    kb.op("dve", lambda e: e.tensor_copy(out=FINS[:, 0, g:g + 1], in_=hcur[:, 256:257]), [hcur], [FINS])
                    kb.op("dve", lambda e: e.tensor_copy(out=FINS[:, 1:3, g], in_=hs[:, :, 8]), [hs], [FINS])
                    kb.op("dve", lambda e: e.tensor_copy(out=HSB[:], in_=hs[:]), [hs], [HSB])
                    kb.mm(PY8[:, 0:256], T0[:, g, :], u8[:, 0:256], True, False, [T0, u8], [PY8])
                    kb.mm(PY8[:, 0:256], VVB[:, g, :], hcur[:, 0:256], False, True, [VVB, hcur], [PY8])
                    py_s = PY8[:, 256:272].rearrange("p (a b) -> p a b", a=2)
                    kb.mm(py_s, T0[:, g, :], u8[:, 256:272].rearrange("p (a b) -> p a b", a=2), True, False,
                          [T0, u8], [PY8])
                    kb.mm(py_s, VVB[:, g, :], HSB[:, :, 0:8], False, True, [VVB, HSB], [PY8])
                    yield
                    kb.op("act", lambda e: e.activation(out=GT1[:], in_=PY8[:], func=AF.Square), [PY8], [GT1])
                    TS(GT1[:], GT1[:], 0.044715, 1.0, ALU.mult, ALU.add, [GT1], [GT1])
                    TT(GT1[:], GT1[:], PY8[:], ALU.mult, [GT1, PY8], [GT1])
                    yield
                    kb.op("act", lambda e: e.activation(out=GT2[:], in_=GT1[:], func=AF.Sigmoid, scale=1.5957691216),
                          [GT1], [GT2])
                    TT(YGALL[:, g, :], GT2[:], PY8[:], ALU.mult, [GT2, PY8], [YGALL])
                    yield

                for g0 in range(0, 32, NL):
                    interleave([body2(g0 + ln, ln) for ln in range(NL)])

                PFN = PX[0]
                kb.mm(PFN[:, 0:96], SWP[:], FINS[:].rearrange("p a b -> p (a b)"), True, True, [SWP, FINS], [PFN])
                FT = kb.sb("FT", [128, 3, 32])
                FO = kb.sb("FO", [128, 128])
                b3 = lambda t: t[:, None, :].to_broadcast([128, 3, 32])
                TT(FT[:], PFN[:, 0:96].rearrange("p (a b) -> p a b", a=3), b3(CIs), ALU.mult, [PFN, CIs], [FT])
                TT(FINS[:], FINS[:], b3(CRc), ALU.mult, [FINS, CRc], [FINS])
                TT(FINS[:], FINS[:], FT[:], ALU.subtract, [FINS, FT], [FINS])
                PTF = PX[1]
                kb.tr(PTF[0:96, 0:128], FINS[:].rearrange("p a b -> p (a b)"), IDf[:], [FINS, IDf], [PTF])
                kb.op("dve", lambda e: e.tensor_copy(out=FO[0:96, :], in_=PTF[0:96, 0:128]), [PTF], [FO])
                kb.dma("sp", o_fin[:, :], FO[0:96, :], reads=[FO])

                kb.dma("pool", ESEL[:, 0:32, :], sp_eselT[:, 0:32, :], writes=[ESEL])
                kb.dma("pool", ESEL[:, 32:64, :], sp_eselT[:, 32:64, :], writes=[ESEL])
                YST = UTO
                for ct in range(4):
                    yv = YST[:, ct, :].rearrange("p (n s) -> p s n", s=8)
                    for t8 in range(8):
                        for gp in range(8):
                            kb.mm(PY8[:], ESEL[:, gp * 8 + t8, :], YGALL[:, ct * 8 + gp, :], gp == 0, gp == 7,
                                  [ESEL, YGALL], [PY8])
                        evac(yv[:, t8, :], YST, PY8[:], PY8)
                PZ = [PX[0], PX[1]]
                for t0 in range(0, NOWN, 512):
                    nq = min(512, NOWN - t0)
                    for ft in range(4):
                        for half, pz in ((0, PZ[0]), (1, PZ[1])):
                            c0 = half * 512 + ft * 128
                            for ct in range(4):
                                kb.mm(pz[:, 0:nq], WG[:, ct, c0:c0 + 128], YST[:, ct, t0:t0 + nq], ct == 0, ct == 3,
                                      [WG, YST], [pz])
                        kb.op("act", lambda e: e.activation(out=GT1B[:, 0:nq], in_=PZ[1][:, 0:nq], func=AF.Sigmoid),
                              [PZ[1]], [GT1B])
                        TT(YS2[:, ft, t0:t0 + nq], PZ[0][:, 0:nq], GT1B[:, 0:nq], ALU.mult, [PZ[0], GT1B], [YS2])

        YD = [Buf(None) for i in range(NT)]
        with kb.scope([[O_FREE2, ARENA_BYTES], [O_UTF, O_YS2]]):
            WGh = kb.sb("WGh", [128, 8, 3, 512], BF16)
            WBFh = kb.sb("WBFh", [64, 8, 512], BF16)
            WBSh = kb.sb("WBSh", [128, 4, 512], BF16)
            WBMh = kb.sb("WBMh", [128, 4, 512], BF16)
            WOh = kb.sb("WOh", [128, 4, D], BF16)
            X = [kb.sb("X", [128, D]) for i in range(2)]
            XA = [kb.sb("XA", [128, D]) for i in range(2)]
            SQ = kb.sb("SQ", [128, D], BF16)
            SS = [kb.sb("SS", [128, 1]) for i in range(2)]
            RS = [kb.sb("RS", [128, 1]) for i in range(2)]
            HN = [kb.sb("HN", [128, D], BF16) for i in range(2)]
            HT = [kb.sb("HT", [128, 8, 128], BF16) for i in range(2)]
            SGT = [[kb.sb("SGT", [128, 512]) for gi in range(3)] for i in range(2)]
            MB_ = [kb.sb("MBm", [128, 512], BF16) for i in range(2)]
            MTt = [kb.sb("MTt", [128, 4, 128], BF16) for i in range(2)]
            for _ in range(3):
                X.append(kb.sb("X", [128, D]))
                XA.append(kb.sb("XA", [128, D]))
            HT.append(kb.sb("HT", [128, 8, 128], BF16))
            PTn = kb.ps("PTn", [128, 8, 128], BF16)
            PTm = kb.ps("PTm", [128, 4, 128], BF16)
            PG = [kb.ps("PG", [128, 512]) for i in range(2)]
            PP = [kb.ps("PP", [128, 512]) for i in range(2)]
            PO = [kb.ps("PO", [128, 512]) for i in range(2)]
            wgv = w_g.rearrange("(k p) (g n) -> p k g n", p=128, g=3)
            ldw_i = [0]
            cnt = [0]
            for hf in range(2):
                fs = slice(hf * 512, (hf + 1) * 512)
                def ldw(dst, dbuf, src, npart=128):
                    stg = XA[ldw_i[0] % 5]
                    eng = ("dve", "pool")[ldw_i[0] % 2]
                    ldw_i[0] += 1
                    shp = list(src.shape)
                    assert shp[1] * shp[2] <= 1024
                    sv = stg[0:npart, 0:shp[1] * shp[2]].rearrange("p (a b) -> p a b", a=shp[1])
                    kb.dma("sp", sv, src, writes=[stg])
                    kb.op(eng, lambda e: e.tensor_copy(out=dst, in_=sv), [stg], [dbuf])

                for k in range(8):
                    ldw(WGh[:, k, 0:2, :], WGh, wgv[:, k, 0:2, fs])
                    ldw(WGh[:, k, 2:3, :], WGh, wgv[:, k, 2:3, fs])
                wbf = w_brf.rearrange("(h d) n -> d h n", d=64)
                for h0 in (0, 2, 4, 6):
                    ldw(WBFh[:, h0:h0 + 2, :], WBFh, wbf[:, h0:h0 + 2, fs], npart=64)
                for c0 in (0, 2):
                    ldw(WBSh[:, c0:c0 + 2, :], WBSh, w_brs.rearrange("(c p) n -> p c n", p=128)[:, c0:c0 + 2, fs])
                    ldw(WBMh[:, c0:c0 + 2, :], WBMh, w_brm.rearrange("(c p) n -> p c n", p=128)[:, c0:c0 + 2, fs])
                wov = w_out[hf * 512:(hf + 1) * 512, :].rearrange("(c p) n -> p c n", p=128)
                for c0 in range(4):
                    ldw(WOh[:, c0:c0 + 1, :], WOh, wov[:, c0:c0 + 1, :])

                def f0(i, hf=hf):
                    tok = slice(i * 128, (i + 1) * 128)
                    kb.dma("sp", X[i % 5][:], x_own[tok, :], writes=[X[i % 5]])
                    if hf == 1:
                        kb.dma("sp", XA[i % 5][:], o_y[tok, :], reads=[YD[i]], writes=[XA[i % 5]])

                def f1a(i, hf=hf):
                    norm_a(X[i % 5], G, SS[i % 2], RS[i % 2], HN[i % 2], SQ)

                def f1b(i, hf=hf):
                    norm_b(HN[i % 2], PTn, (HT[i % 3][:], HT[i % 3]))

                def f2(i):
                    tok = slice(i * 128, (i + 1) * 128)
                    ht, sg = HT[i % 3], SGT[i % 2]
                    for gi in range(3):
                        pg, pp = PG[cnt[0] % 2], PP[cnt[0] % 2]
                        cnt[0] += 1
                        for k in range(8):
                            kb.mm(pg[:], ht[:, k, :], WGh[:, k, gi, :], k == 0, k == 7, [ht, WGh], [pg])
                        if gi == 0:
                            for h in range(8):
                                kb.mm(pp[:], OFT[0:64, h, tok], WBFh[0:64, h, :], h == 0, h == 7, [OFT, WBFh], [pp])
                        elif gi == 1:
                            for c in range(4):
                                kb.mm(pp[:], YS2[:, c, tok], WBSh[:, c, :], c == 0, c == 3, [YS2, WBSh], [pp])
                        else:
                            for c in range(4):
                                kb.mm(pp[:], OMT[:, c, tok], WBMh[:, c, :], c == 0, c == 3, [OMT, WBMh], [pp])
                        kb.op("act", lambda e: e.activation(out=sg[gi][:], in_=pg[:], func=AF.Sigmoid),
                              [pg], [sg[gi]])
                        kb.op("dve", lambda e: e.tensor_tensor(out=sg[gi][:], in0=sg[gi][:], in1=pp[:],
                                                               op=ALU.mult), [sg[gi], pp], [sg[gi]])

                def f3(i, hf=hf):
                    tok = slice(i * 128, (i + 1) * 128)
                    sg, mb_, mt = SGT[i % 2], MB_[i % 2], MTt[i % 2]
                    xa = X[i % 5] if hf == 0 else XA[i % 5]
                    kb.op("pool", lambda e: e.tensor_tensor(out=sg[0][:], in0=sg[0][:], in1=sg[1][:], op=ALU.add),
                          [sg[0], sg[1]], [sg[0]])
                    kb.op("pool", lambda e: e.tensor_tensor(out=mb_[:], in0=sg[0][:], in1=sg[2][:], op=ALU.add),
                          [sg[0], sg[2]], [mb_])
                    for c in range(4):
                        kb.tr(PTm[:, c, :], mb_[:, c * 128:(c + 1) * 128], IDb[:], [mb_, IDb], [PTm])
                    kb.op("act", lambda e: e.copy(out=mt[:], in_=PTm[:]), [PTm], [mt])
                    for half in range(2):
                        hs = slice(half * 512, (half + 1) * 512)
                        for c in range(4):
                            kb.mm(PO[half][:], mt[:, c, :], WOh[:, c, hs], c == 0, c == 3, [mt, WOh], [PO[half]])
                        kb.op("dve", lambda e: e.tensor_tensor(out=xa[:, hs], in0=xa[:, hs], in1=PO[half][:],
                                                               op=ALU.add), [xa, PO[half]], [xa])
                    kb.dma("sp", o_y[tok, :], xa[:], reads=[xa], writes=[YD[i]])

                f0(0)
                f0(1)
                for t in range(NT + 2):
                    if t + 2 < NT:
                        f0(t + 2)
                    if t < NT:
                        f1a(t)
                    if 0 <= t - 1 < NT:
                        f2(t - 1)
                    if t < NT:
                        f1b(t)
                    if 0 <= t - 2 < NT:
                        f3(t - 2)

        if STOP == "F":
            kb.finish()
            return nc
        with kb.scope([[O_OFT, ARENA_BYTES]]):
            H2T = kb.sb("H2T", [128, 8, NOWN], BF16)
            ACC = kb.sb("ACC", [128, NT, D])
            COMBT = kb.sb("COMBT", [32, NOWN], BF16)
            WEX = [(kb.sb("WEG", [128, 8, 2, 256], BF16), kb.sb("WEU", [128, 8, 2, 256], BF16),
                    kb.sb("WED", [128, 2, 2, D], BF16)) for i in range(2)]

            def load_chunk(ch):
                wg_, wu_, wd_ = WEX[ch % 2]
                for el in range(2):
                    e_ = ch * 2 + el
                    kb.dma("pool", wg_[:, :, el, :], moe_wg[e_].rearrange("(k p) f -> p k f", p=128), writes=[wg_])
                    kb.dma("pool", wu_[:, :, el, :], moe_wu[e_].rearrange("(k p) f -> p k f", p=128), writes=[wu_])
                    kb.dma("pool", wd_[:, :, el, :], moe_wd[e_].rearrange("(t p) d -> p t d", p=128), writes=[wd_])

            if STOP != "G1":
                load_chunk(0)
                load_chunk(1)
            else:
                kb.op("dve", lambda e: e.memset(COMBT[:], 0.0), [], [COMBT])
            with kb.scope([[kb.regions[0][0], ARENA_BYTES]]):
                GF = kb.sb("GF", [128, D])
                WR = kb.sb("WR", [128, 8, 36])
                kb.dma("sp", GF[:], gffn, writes=[GF])
                kb.dma("sp", WR[:], w_r.rearrange("(k p) n -> p k n", p=128), writes=[WR])
                X = [kb.sb("X", [128, D]) for i in range(2)]
                SQ = kb.sb("SQ", [128, D], BF16)
                SS = [kb.sb("SS", [128, 1]) for i in range(2)]
                RS = [kb.sb("RS", [128, 1]) for i in range(2)]
                H2F = [kb.sb("H2F", [128, D]) for i in range(2)]
                H2Tf = [kb.sb("H2Tf", [128, 8, 128]) for i in range(2)]
                LG = kb.sb("LG", [128, 36])
                GOH = kb.sb("GOH", [128, 4])
                GEX = kb.sb("GEX", [128, 4])
                SM = kb.sb("SM", [128, 16])
                ESL = kb.sb("ESL", [128, 8])
                ES4 = kb.sb("ES4", [128, 4, 8])
                MX8 = kb.sb("MX8", [128, 8])
                OH1 = kb.sb("OH1", [128, 8])
                OH2 = kb.sb("OH2", [128, 8])
                COMB = kb.sb("COMB", [128, 4, 8])
                PTFb = [kb.ps("PTF", [128, 512]) for i in range(2)]
                PTF = [_View(p_, p_[:, 0:128]) for p_ in PTFb]
                PLb = kb.ps("PL", [128, 512])
                PL = _View(PLb, PLb[:, 0:36])
                PCTb = kb.ps("PCT", [128, 512])
                PCT = _View(PCTb, PCTb[0:32, 0:128])
                for i in range(NT):
                    b = i % 2
                    tok = slice(i * 128, (i + 1) * 128)
                    x, ss, rs, h2f, h2t = X[b], SS[b], RS[b], H2F[b], H2Tf[b]
                    kb.dma("sp", x[:], o_y[tok, :], reads=[YD[i]], writes=[x])
                    kb.op("act", lambda e: e.activation(out=SQ[:], in_=x[:], func=AF.Square, accum_out=ss[:]),
                          [x], [SQ, ss])
                    kb.op("act", lambda e: e.activation(out=rs[:], in_=ss[:], func=AF.Ln, scale=1.0 / D, bias=EPS),
                          [ss], [rs])
                    kb.op("act", lambda e: e.activation(out=rs[:], in_=rs[:], func=AF.Exp, scale=-0.5), [rs], [rs])
                    kb.op("dve", lambda e: e.scalar_tensor_tensor(out=h2f[:], in0=x[:], scalar=rs[:, 0:1], in1=GF[:],
                                                                  op0=ALU.mult, op1=ALU.mult), [x, rs, GF], [h2f])
                    if 9 < 0: continue
                    for k in range(8):
                        ptf = PTF[k % 2]
                        kb.tr(ptf[:], h2f[:, k * 128:(k + 1) * 128], IDf[:], [h2f, IDf], [ptf])
                        kb.op("act", lambda e: e.copy(out=h2t[:, k, :], in_=ptf[:]), [ptf], [h2t])
                        kb.op("dve", lambda e: e.tensor_copy(out=H2T[:, k, tok], in_=ptf[:]), [ptf], [H2T])
                    if 9 < 1: continue
                    for k in range(8):
                        kb.mm(PL[:], h2t[:, k, :], WR[:, k, :], k == 0, k == 7, [h2t, WR], [PL])
                    V = lambda fn, rd, wr: kb.op("dve", fn, rd, wr)
                    V(lambda e: e.tensor_copy(out=LG[:], in_=PL[:]), [PL], [LG])
                    if 9 < 2: continue
                    V(lambda e: e.tensor_reduce(out=SM[:, 0:1], in_=LG[:, 0:4], axis=AX.X, op=ALU.max), [LG], [SM])
                    V(lambda e: e.tensor_scalar(out=GOH[:], in0=LG[:, 0:4], scalar1=SM[:, 0:1], scalar2=None,
                                                op0=ALU.is_equal), [LG, SM], [GOH])
                    V(lambda e: e.tensor_scalar(out=SM[:, 1:2], in0=SM[:, 0:1], scalar1=-1.0, scalar2=None,
                                                op0=ALU.mult), [SM], [SM])
                    kb.op("act", lambda e: e.activation(out=GEX[:], in_=LG[:, 0:4], func=AF.Exp, bias=SM[:, 1:2],
                                                        accum_out=SM[:, 2:3]), [LG, SM], [GEX, SM])
                    V(lambda e: e.reciprocal(out=SM[:, 3:4], in_=SM[:, 2:3]), [SM], [SM])
                    V(lambda e: e.tensor_tensor(out=ES4[:], in0=LG[:, 4:36].rearrange("p (g e) -> p g e", g=4),
                                                in1=GOH[:, :, None].to_broadcast([128, 4, 8]), op=ALU.mult),
                      [LG, GOH], [ES4])
                    V(lambda e: e.tensor_reduce(out=ESL[:], in_=ES4[:].rearrange("p g e -> p e g"), axis=AX.X,
                                                op=ALU.add), [ES4], [ESL])
                    V(lambda e: e.max(out=MX8[:], in_=ESL[:]), [ESL], [MX8])
                    V(lambda e: e.tensor_scalar(out=OH1[:], in0=ESL[:], scalar1=MX8[:, 0:1], scalar2=None,
                                                op0=ALU.is_equal), [ESL, MX8], [OH1])
                    V(lambda e: e.tensor_scalar(out=OH2[:], in0=ESL[:], scalar1=MX8[:, 1:2], scalar2=None,
                                                op0=ALU.is_equal), [ESL, MX8], [OH2])
                    V(lambda e: e.tensor_tensor(out=SM[:, 4:5], in0=MX8[:, 1:2], in1=MX8[:, 0:1], op=ALU.subtract),
                      [MX8], [SM])
                    kb.op("act", lambda e: e.activation(out=SM[:, 5:6], in_=SM[:, 4:5], func=AF.Exp), [SM], [SM])
                    V(lambda e: e.tensor_scalar(out=SM[:, 6:7], in0=SM[:, 5:6], scalar1=1.0, scalar2=None,
                                                op0=ALU.add), [SM], [SM])
                    V(lambda e: e.reciprocal(out=SM[:, 6:7], in_=SM[:, 6:7]), [SM], [SM])
                    V(lambda e: e.tensor_tensor(out=SM[:, 7:8], in0=SM[:, 5:6], in1=SM[:, 6:7], op=ALU.mult),
                      [SM], [SM])
                    V(lambda e: e.tensor_tensor(out=SM[:, 8:9], in0=SM[:, 6:7], in1=SM[:, 3:4], op=ALU.mult),
                      [SM], [SM])
                    V(lambda e: e.tensor_tensor(out=SM[:, 9:10], in0=SM[:, 7:8], in1=SM[:, 3:4], op=ALU.mult),
                      [SM], [SM])
                    V(lambda e: e.tensor_scalar(out=OH1[:], in0=OH1[:], scalar1=SM[:, 8:9], scalar2=None,
                                                op0=ALU.mult), [OH1, SM], [OH1])
                    V(lambda e: e.scalar_tensor_tensor(out=OH1[:], in0=OH2[:], scalar=SM[:, 9:10], in1=OH1[:],
                                                       op0=ALU.mult, op1=ALU.add), [OH2, SM, OH1], [OH1])
                    V(lambda e: e.tensor_tensor(out=COMB[:], in0=GOH[:, :, None].to_broadcast([128, 4, 8]),
                                                in1=OH1[:, None, :].to_broadcast([128, 4, 8]), op=ALU.mult),
                      [GOH, OH1], [COMB])
                    if 9 < 3: continue
                    kb.tr(PCT[:], COMB[:].rearrange("p g e -> p (g e)"), IDf[:], [COMB, IDf], [PCT])
                    V(lambda e: e.tensor_copy(out=COMBT[:, tok], in_=PCT[:]), [PCT], [COMBT])

            with kb.scope([[kb.regions[0][0], ARENA_BYTES]]):
                ACTT = kb.sb("ACTT", [128, 2, 2, NOWN], BF16)
                SE = [kb.sb("SE", [32, 128], BF16) for i in range(2)]
                SEF = [kb.sb("SEF", [32, 128]) for i in range(2)]
                CBC = [kb.sb("CBC", [128, 512]) for i in range(2)]
                SLb = [kb.sb("SLb", [128, 512], BF16) for i in range(2)]
                UTb = [kb.sb("UTb", [128, 512], BF16) for i in range(2)]
                PGa = [kb.ps("PGa", [128, 512]) for i in range(2)]
                PUp = [kb.ps("PUp", [128, 512]) for i in range(2)]
                PCB = kb.ps("PCB", [128, 512])
                PD = [kb.ps("PD", [128, 512]) for i in range(2)]
                pieces = [(t0, min(512, NOWN - t0)) for t0 in range(0, NOWN, 512)]
                cnt = 0
                for ch in range(16):
                    wg_, wu_, wd_ = WEX[ch % 2]
                    for el in range(2):
                        e_ = ch * 2 + el
                        se = SE[e_ % 2]
                        sef = SEF[e_ % 2]
                        kb.dma("sp", sef[:], sele[e_], writes=[sef])
                        kb.op("dve", lambda e: e.tensor_copy(out=se[:], in_=sef[:]), [sef], [se])
                        for (t0, nq) in pieces:
                            cbc = CBC[cnt % 2]
                            kb.mm(PCB[:, 0:nq], se[:], COMBT[:, t0:t0 + nq], True, True, [se, COMBT], [PCB])
                            kb.op("act", lambda e: e.copy(out=cbc[:, 0:nq], in_=PCB[:, 0:nq]), [PCB], [cbc])
                            for ft in range(2):
                                pga, pup = PGa[cnt % 2], PUp[cnt % 2]
                                slb, utb = SLb[cnt % 2], UTb[cnt % 2]
                                cnt += 1
                                fsl = slice(ft * 128, (ft + 1) * 128)
                                for k in range(8):
                                    kb.mm(pga[:, 0:nq], wg_[:, k, el, fsl], H2T[:, k, t0:t0 + nq], k == 0, k == 7,
                                          [wg_, H2T], [pga])
                                for k in range(8):
                                    kb.mm(pup[:, 0:nq], wu_[:, k, el, fsl], H2T[:, k, t0:t0 + nq], k == 0, k == 7,
                                          [wu_, H2T], [pup])
                                kb.op("act", lambda e: e.activation(out=slb[:, 0:nq], in_=pga[:, 0:nq], func=AF.Silu),
                                      [pga], [slb])
                                kb.op("dve", lambda e: e.tensor_tensor(out=utb[:, 0:nq], in0=pup[:, 0:nq],
                                                                       in1=cbc[:, 0:nq], op=ALU.mult),
                                      [pup, cbc], [utb])
                                kb.op("pool", lambda e: e.tensor_tensor(out=ACTT[:, el, ft, t0:t0 + nq],
                                                                        in0=slb[:, 0:nq], in1=utb[:, 0:nq],
                                                                        op=ALU.mult), [slb, utb], [ACTT])
                    for i in range(NT):
                        tok = slice(i * 128, (i + 1) * 128)
                        for half in range(2):
                            hs = slice(half * 512, (half + 1) * 512)
                            pd = PD[(i * 2 + half) % 2]
                            n = 0
                            for el in range(2):
                                for ft in range(2):
                                    kb.mm(pd[:], ACTT[:, el, ft, tok], wd_[:, ft, el, hs], n == 0, n == 3,
                                          [ACTT, wd_], [pd])
                                    n += 1
                            if ch == 0:
                                kb.op("dve", lambda e: e.tensor_copy(out=ACC[:, i, hs], in_=pd[:]), [pd], [ACC])
                            else:
                                kb.op("dve", lambda e: e.tensor_tensor(out=ACC[:, i, hs], in0=ACC[:, i, hs],
                                                                       in1=pd[:], op=ALU.add), [ACC, pd], [ACC])
                    if ch + 2 < 16:
                        load_chunk(ch + 2)
            with kb.scope([[kb.regions[0][0], ARENA_BYTES]]):
                XF = [kb.sb("XF", [128, D]) for i in range(4)]

                def ld_x1(i):
                    kb.dma("sp", XF[i % 4][:], o_y[i * 128:(i + 1) * 128, :], reads=[YD[i]], writes=[XF[i % 4]])

                ld_x1(0)
                ld_x1(1)
                for i in range(NT):
                    tok = slice(i * 128, (i + 1) * 128)
                    xf = XF[i % 4]
                    if i + 2 < NT:
                        ld_x1(i + 2)
                    kb.op("dve", lambda e: e.tensor_tensor(out=xf[:], in0=xf[:], in1=ACC[:, i, :], op=ALU.add),
                          [xf, ACC], [xf])
                    kb.dma("sp", o_y[tok, :], xf[:], reads=[xf], writes=[YD[i]])
        kb.finish()
    return nc


def _stair():
    m = np.zeros((4, 128, 512), np.float32)
    s = np.arange(128)[:, None]
    t = np.arange(128)[None, :]
    tri = (s <= t).astype(np.float32)
    for d in range(4):
        for qi in range(4):
            blk = 0.0 if qi < d else (tri if qi == d else 1.0)
            m[d, :, qi * 128:(qi + 1) * 128] = blk
    return m


def kernel(**inp):
    f = lambda a: np.ascontiguousarray(np.asarray(a, dtype=np.float32))
    x_prompt = f(inp["x_prompt"])
    x_sample = f(inp["x_sample"])
    w_in = f(inp["w_in"])[0]
    cache_k = f(inp["cache_fox_k"])[0].reshape(16, SEQ, 512)
    cache_v = f(inp["cache_fox_v"])[0].reshape(16, SEQ, 512)
    cache_lf = f(inp["cache_fox_logf"])[0]
    rep = lambda v, n=128: np.ascontiguousarray(np.broadcast_to(v[None, :], (n, v.shape[0])))
    w_a = np.ascontiguousarray(np.concatenate([w_in[:, 512:1536], w_in[:, 1536:1544], w_in[:, 2056:2568]], axis=1))
    w_b = np.ascontiguousarray(np.concatenate([w_in[:, 0:512], w_in[:, 1544:2056]], axis=1))
    s_ = np.arange(128)[:, None]
    t_ = np.arange(128)[None, :]
    stair = _stair()
    common = dict(
        gmix=rep(f(inp["norm_mix"])[0]), w_a=w_a, w_b=w_b,
        kn_rep=rep(np.tile(f(inp["kn_fox"])[0], 8)), qn_rep=rep(np.tile(f(inp["qn_fox"])[0], 8)),
        qnm_rep=rep(np.tile(f(inp["qn_mem"])[0], 4)), bf_rep=rep(f(inp["b_forget"])[0]),
        ident=np.eye(128, dtype=np.float32), triu=(s_ <= t_).astype(np.float32),
        gmem=rep(f(inp["norm_mem"])[0]), w_mkv=f(inp["w_mem_kv"])[0], knm_rep=rep(np.tile(f(inp["kn_mem"])[0], 4)))
    mem_prompt = f(inp["mem_prompt"])
    dup = lambda m_: np.ascontiguousarray(np.concatenate([m_, m_], axis=0))
    a_re, a_im = f(inp["ssm_a_re"])[0], f(inp["ssm_a_im"])[0]
    exps = [7 - i for i in range(8)] + [-i for i in range(8)] + [i + 1 for i in range(8)] + list(range(8)) \
        + [8 * (1 << k) for k in range(9)]
    pidx = np.arange(128)
    esel = np.zeros((128, 8, 8, 128), np.float32)
    for gp in range(8):
        for s8 in range(8):
            for hh in range(16):
                esel[16 * gp + hh, gp, s8, s8 * 16 + hh] = 1.0
    eselT = np.ascontiguousarray(np.transpose(esel, (3, 1, 2, 0)))
    swap = np.zeros((128, 128), np.float32)
    swap[(pidx + 64) % 128, pidx] = 1.0
    common.update(
        sp_are=dup(a_re.T), sp_aim=dup(a_im.T), sp_ldt=rep(f(inp["ssm_log_dt"])[0]),
        sp_bre=dup(np.transpose(f(inp["ssm_b_re"])[0], (1, 0, 2))), sp_bim=dup(np.transpose(f(inp["ssm_b_im"])[0], (1, 0, 2))),
        sp_cre=dup(np.transpose(f(inp["ssm_c_re"])[0], (2, 0, 1))), sp_cim=dup(np.transpose(f(inp["ssm_c_im"])[0], (2, 0, 1))),
        sp_dcol=np.ascontiguousarray(np.tile(f(inp["ssm_d"])[0].reshape(32, 16).T, (8, 1))),
        sp_et=rep(np.array(exps, np.float32)),
        sp_caus=((pidx[:, None] // 16) <= (pidx[None, :] // 16)).astype(np.float32),
        sp_esel=esel.reshape(128, 64, 128), sp_eselT=eselT.reshape(128, 64, 128), sp_swap=swap,
        sp_sg=np.concatenate([np.ones((64, 1), np.float32), -np.ones((64, 1), np.float32)]),
        w_glu=f(inp["w_glu"])[0],
        w_g=np.ascontiguousarray(w_in[:, 2568:5640]), w_brf=f(inp["w_br_fox"])[0], w_brs=f(inp["w_br_ssm"])[0],
        w_brm=f(inp["w_br_mem"])[0], w_out=f(inp["w_out"])[0], gffn=rep(f(inp["norm_ffn"])[0]),
        w_r=np.ascontiguousarray(np.concatenate([f(inp["w_router_group"])[0], f(inp["w_router_expert"])[0]], axis=1)),
        moe_wg=f(inp["moe_w_gate"])[0], moe_wu=f(inp["moe_w_up"])[0], moe_wd=f(inp["moe_w_down"])[0],
        sele=np.ascontiguousarray(np.broadcast_to(np.eye(32, dtype=np.float32)[:, :, None], (32, 32, 128))))
    st_re, st_im = f(inp["state_ssm_re"])[0], f(inp["state_ssm_im"])[0]
    cache_mk = f(inp["cache_mem_k"])[0].reshape(16, 256, 512)
    cache_mv = f(inp["cache_mem_v"])[0].reshape(16, 256, 512)
    in_maps = []
    for c in range(NCORES):
        b, r = c // 2, c % 2
        m = dict(common)
        m["x_all"] = x_prompt[b]
        m["x_own"] = np.ascontiguousarray(np.concatenate(
            [x_prompt[b, r * HALF:(r + 1) * HALF], x_sample[2 * c], x_sample[2 * c + 1]], axis=0))
        m["mask_a"] = np.ascontiguousarray(np.transpose(stair if r == 0 else np.ones_like(stair), (1, 0, 2)))
        m["mask_b"] = np.ascontiguousarray(np.transpose(stair, (1, 0, 2)))
        addm = np.zeros((4, 36), np.float32)
        if r == 0:
            for J in range(4):
                addm[J, 4 * J + 4:] = NEG
        m["addm"] = np.ascontiguousarray(np.broadcast_to(addm[None], (128, 4, 36)))
        m["rflag"] = np.full((128, 1), float(r), np.float32)
        m["c_k"] = np.ascontiguousarray(cache_k[2 * c:2 * c + 2])
        m["c_v"] = np.ascontiguousarray(cache_v[2 * c:2 * c + 2])
        m["c_lf"] = np.ascontiguousarray(cache_lf[2 * c:2 * c + 2])
        m["mem_in"] = mem_prompt[b]
        hre = np.transpose(st_re[2 * c:2 * c + 2], (2, 0, 1))
        him = np.transpose(st_im[2 * c:2 * c + 2], (2, 0, 1))
        m["sp_h0"] = np.ascontiguousarray(np.concatenate([hre, him], axis=0))
        m["sp_h0s"] = np.ascontiguousarray(np.concatenate([him, hre], axis=0))
        m["c_mk"] = np.ascontiguousarray(cache_mk[2 * c:2 * c + 2])
        m["c_mv"] = np.ascontiguousarray(cache_mv[2 * c:2 * c + 2])
        in_maps.append(m)
    nc = build()
    res = run_bass_kernel_spmd(nc, in_maps, core_ids=list(range(NCORES)))
    r = res.results
    kernel.last = r
    pk = np.stack([r[2 * b]["o_k"].reshape(SEQ, 8, 64) for b in range(4)])[None]
    pv = np.stack([r[2 * b]["o_v"].reshape(SEQ, 8, 64) for b in range(4)])[None]
    plf = np.stack([r[2 * b]["o_lf"] for b in range(4)])[None]
    sk = np.concatenate([r[c]["o_sk"].reshape(2, 64, 8, 64) for c in range(8)])[None]
    sv = np.concatenate([r[c]["o_sv"].reshape(2, 64, 8, 64) for c in range(8)])[None]
    slf = np.concatenate([r[c]["o_slf"].reshape(2, 64, 8) for c in range(8)])[None]
    mk = np.stack([r[2 * b]["o_mk"].reshape(256, 4, 128) for b in range(4)])[None]
    mv = np.stack([r[2 * b]["o_mv"].reshape(256, 4, 128) for b in range(4)])[None]
    z = lambda *s: np.zeros(s, np.float32)
    fin = [r[c]["o_fin"].reshape(3, 32, 128) for c in range(8)]
    p_re = np.stack([fin[2 * b + 1][0, :, 0:64] for b in range(4)])[None]
    p_im = np.stack([fin[2 * b + 1][0, :, 64:128] for b in range(4)])[None]
    s_re = np.stack([fin[c][1 + j, :, 0:64] for c in range(8) for j in range(2)])[None]
    s_im = np.stack([fin[c][1 + j, :, 64:128] for c in range(8) for j in range(2)])[None]
    yp = np.stack([np.concatenate([r[2 * b]["o_y"][0:HALF], r[2 * b + 1]["o_y"][0:HALF]], axis=0) for b in range(4)])
    ysm = np.concatenate([r[c]["o_y"][HALF:].reshape(2, 64, D) for c in range(8)])
    return (yp, ysm, pk, pv, plf, p_re, p_im, mk, mv, sk, sv, slf, s_re, s_im)
```
